# Optimizing a Trainium2 kernel written in Bass

```python
import math
import jax
import jax.numpy as jnp
from jax import lax
import numpy as np

D_MODEL = 1024
BATCH = 1
SEQ = 16384
DEPTH = 4

N_A_LAYERS = DEPTH // 2
N_B_LAYERS = DEPTH - N_A_LAYERS

HEAD_DIM = 64
Q_BLOCK = 128
LN_EPS = 1e-5
NEG_INF = -1e30
TINY = 1e-30
F32 = jnp.float32

A_WINDOWS = (128, 512, 2048)
A_DILATIONS = (1, 4, 16)
A_N_GROUPS = len(A_WINDOWS)
A_HEADS = D_MODEL // HEAD_DIM
A_WIDTH = A_HEADS * HEAD_DIM
A_IN_COLS = A_N_GROUPS * 3 * A_WIDTH

B_HEADS = D_MODEL // HEAD_DIM
B_KV_GROUPS = 4
B_HPG = B_HEADS // B_KV_GROUPS
B_WIDTH = B_HEADS * HEAD_DIM
B_N_BRANCH = 3
B_Q_COLS = B_WIDTH + B_N_BRANCH * B_HEADS
B_KV_COLS = 2 * B_N_BRANCH * B_KV_GROUPS * HEAD_DIM
CMP_LEN = 32
CMP_STRIDE = 16
CMP_HIDDEN = 256
SLC_BLOCK = 64
N_SELECT = 16
WIN = 512
FORCE_SCORE = 1e4

N_EXPERT_GROUPS = 4
EXPERTS_PER_GROUP = 4
N_EXPERTS = N_EXPERT_GROUPS * EXPERTS_PER_GROUP
TOP_K_EXPERTS = 2
D_EXPERT = 256
MOE_CHUNK = 2048

DEEPNORM_ALPHA = (2.0 * DEPTH) ** 0.25
DEEPNORM_BETA = (8.0 * DEPTH) ** -0.25

kernel_name = 'yoco_dilated_nsa_hmoe_deepnorm'


def _alibi_slopes(n):
    return jnp.asarray(2.0 ** (-8.0 * np.arange(1, n + 1) / n), dtype=F32)


def layer_norm(x, g, b):
    xf = x.astype(F32)
    mu = jnp.mean(xf, axis=-1, keepdims=True)
    var = jnp.mean(jnp.square(xf - mu), axis=-1, keepdims=True)
    return ((xf - mu) * lax.rsqrt(var + LN_EPS) * g + b).astype(x.dtype)


def _masked_softmax(s, mask):
    s = jnp.where(mask, s, NEG_INF)
    m = jnp.max(s, axis=-1, keepdims=True)
    p = jnp.where(mask, jnp.exp(s - m), 0.0)
    return p / jnp.maximum(jnp.sum(p, axis=-1, keepdims=True), TINY)


def dilated_attention(h, w_in, w_out):
    bsz, seq, _ = h.shape
    qkv = jnp.einsum('bsd,dc->bsc', h, w_in).reshape(bsz, seq, A_N_GROUPS, 3, A_HEADS, HEAD_DIM)
    q_groups = [qkv[:, :, g, 0] for g in range(A_N_GROUPS)]
    k_groups = [qkv[:, :, g, 1] for g in range(A_N_GROUPS)]
    v_groups = [qkv[:, :, g, 2] for g in range(A_N_GROUPS)]
    slopes = _alibi_slopes(A_HEADS)[:, None, None]
    scale = HEAD_DIM ** -0.5

    def block(i):
        t0 = i * Q_BLOCK
        t = t0 + jnp.arange(Q_BLOCK)
        outs, lses = [], []
        for g in range(A_N_GROUPS):
            dil = A_DILATIONS[g]
            dist = jnp.arange(A_WINDOWS[g] // dil + 1) * dil
            idx = t[:, None] - dist[None, :]
            valid = idx >= 0
            idx = jnp.maximum(idx, 0)
            qb = lax.dynamic_slice_in_dim(q_groups[g], t0, Q_BLOCK, axis=1)
            kb = jnp.take(k_groups[g], idx, axis=1)
            vb = jnp.take(v_groups[g], idx, axis=1)
            s = jnp.einsum('bqhd,bqkhd->bhqk', qb, kb).astype(F32) * scale - slopes * dist.astype(F32)
            s = jnp.where(valid, s, NEG_INF)
            m = jnp.max(s, axis=-1, keepdims=True)
            p = jnp.exp(s - m)
            l = jnp.sum(p, axis=-1, keepdims=True)
            outs.append(jnp.einsum('bhqk,bqkhd->bqhd', p / l, vb))
            lses.append(jnp.transpose((m + jnp.log(l))[..., 0], (0, 2, 1)))
        w = jax.nn.softmax(jnp.stack(lses, axis=0), axis=0)
        o = w[0][..., None] * outs[0]
        for g in range(1, A_N_GROUPS):
            o = o + w[g][..., None] * outs[g]
        return o

    o = lax.map(block, jnp.arange(seq // Q_BLOCK))
    o = jnp.transpose(o, (1, 0, 2, 3, 4)).reshape(bsz, seq, A_WIDTH)
    return jnp.einsum('bsc,cd->bsd', o, w_out).astype(h.dtype)


def nsa_shared_kv(h, w_kv, cmp_pos, cmp_w1, cmp_b1, cmp_w2, cmp_b2):
    bsz, seq, _ = h.shape
    kv = jnp.einsum('bsd,dc->bsc', h, w_kv).reshape(bsz, seq, 2 * B_N_BRANCH, B_KV_GROUPS, HEAD_DIM)
    r = CMP_LEN // CMP_STRIDE
    n_chunks = seq // CMP_STRIDE
    n_cmp = n_chunks - r + 1

    def compress(u, j):
        c = u.reshape(bsz, n_chunks, CMP_STRIDE, B_KV_GROUPS, HEAD_DIM)
        blocks = jnp.concatenate([c[:, o:o + n_cmp] for o in range(r)], axis=2)
        blocks = blocks + cmp_pos[j][:, None, :]
        flat = jnp.transpose(blocks, (0, 1, 3, 2, 4)).reshape(bsz, n_cmp, B_KV_GROUPS, CMP_LEN * HEAD_DIM)
        hid = jax.nn.gelu(flat @ cmp_w1[j] + cmp_b1[j])
        return hid @ cmp_w2[j] + cmp_b2[j]

    k_cmp = compress(kv[:, :, 0], 0)
    v_cmp = compress(kv[:, :, 1], 1)
    return (k_cmp, v_cmp, kv[:, :, 2], kv[:, :, 3], kv[:, :, 4], kv[:, :, 5])


def nsa_attention(h, w_q, b_q, w_out, k_cmp, v_cmp, k_slc, v_slc, k_win, v_win):
    bsz, seq, _ = h.shape
    proj = jnp.einsum('bsd,dc->bsc', h, w_q) + b_q
    q = proj[..., :B_WIDTH].reshape(bsz, seq, B_KV_GROUPS, B_HPG, HEAD_DIM)
    gates = jax.nn.sigmoid(proj[..., B_WIDTH:].astype(F32)).reshape(bsz, seq, B_N_BRANCH, B_KV_GROUPS, B_HPG)
    slopes = _alibi_slopes(B_HEADS).reshape(B_KV_GROUPS, B_HPG, 1, 1)
    scale = HEAD_DIM ** -0.5
    n_cmp = k_cmp.shape[1]
    cmp_end = jnp.arange(n_cmp) * CMP_STRIDE + (CMP_LEN - 1)
    n_slc = seq // SLC_BLOCK
    n_sel = min(N_SELECT, n_slc)
    n_keys_sel = n_sel * SLC_BLOCK
    r_s = SLC_BLOCK // CMP_STRIDE
    r_c = CMP_LEN // CMP_STRIDE
    slc_ids = jnp.arange(n_slc)
    map_idx = (r_s * slc_ids[:, None, None] + jnp.arange(r_s)[None, :, None]
               - jnp.arange(r_c)[None, None, :]).reshape(n_slc, r_s * r_c)
    map_valid = ((map_idx >= 0) & (map_idx < n_cmp)).astype(F32)
    map_idx = jnp.clip(map_idx, 0, n_cmp - 1)
    b_ids = jnp.arange(bsz)[:, None, None]
    g_ids = jnp.arange(B_KV_GROUPS)[None, :, None]
    k_win_p = jnp.pad(k_win, ((0, 0), (WIN, 0), (0, 0), (0, 0)))
    v_win_p = jnp.pad(v_win, ((0, 0), (WIN, 0), (0, 0), (0, 0)))

    def block(i):
        t0 = i * Q_BLOCK
        t = t0 + jnp.arange(Q_BLOCK)
        qb = lax.dynamic_slice_in_dim(q, t0, Q_BLOCK, axis=1)
        d_c = t[:, None] - cmp_end[None, :]
        s_c = jnp.einsum('bqgrd,bcgd->bgrqc', qb, k_cmp).astype(F32) * scale - slopes * d_c.astype(F32)
        p_c = _masked_softmax(s_c, d_c >= 0)
        o_c = jnp.einsum('bgrqc,bcgd->bqgrd', p_c, v_cmp)
        imp = jnp.sum(p_c, axis=2)
        p_slc = jnp.sum(jnp.take(imp, map_idx, axis=-1) * map_valid, axis=-1)
        cur = (t // SLC_BLOCK)[:, None]
        forced = (slc_ids[None, :] == 0) | (slc_ids[None, :] == cur) | (slc_ids[None, :] == cur - 1)
        score = jnp.where(forced, FORCE_SCORE, p_slc)
        score = jnp.where(slc_ids[None, :] <= cur, score, -FORCE_SCORE)
        _, sel = lax.top_k(score, n_sel)
        tok = (sel[..., None] * SLC_BLOCK + jnp.arange(SLC_BLOCK)).reshape(bsz, B_KV_GROUPS, Q_BLOCK * n_keys_sel)
        k_s = k_slc[b_ids, tok, g_ids].reshape(bsz, B_KV_GROUPS, Q_BLOCK, n_keys_sel, HEAD_DIM)
        v_s = v_slc[b_ids, tok, g_ids].reshape(bsz, B_KV_GROUPS, Q_BLOCK, n_keys_sel, HEAD_DIM)
        d_s = t[None, None, :, None] - tok.reshape(bsz, B_KV_GROUPS, Q_BLOCK, n_keys_sel)
        s_s = jnp.einsum('bqgrd,bgqkd->bgrqk', qb, k_s).astype(F32) * scale - slopes * d_s[:, :, None].astype(F32)
        p_s = _masked_softmax(s_s, (d_s >= 0)[:, :, None])
        o_s = jnp.einsum('bgrqk,bgqkd->bqgrd', p_s, v_s)
        k_w = lax.dynamic_slice_in_dim(k_win_p, t0, WIN + Q_BLOCK, axis=1)
        v_w = lax.dynamic_slice_in_dim(v_win_p, t0, WIN + Q_BLOCK, axis=1)
        pos_w = t0 - WIN + jnp.arange(WIN + Q_BLOCK)
        d_w = t[:, None] - pos_w[None, :]
        mask_w = (d_w >= 0) & (d_w < WIN) & (pos_w[None, :] >= 0)
        s_w = jnp.einsum('bqgrd,bkgd->bgrqk', qb, k_w).astype(F32) * scale - slopes * d_w.astype(F32)
        p_w = _masked_softmax(s_w, mask_w)
        o_w = jnp.einsum('bgrqk,bkgd->bqgrd', p_w, v_w)
        gb = lax.dynamic_slice_in_dim(gates, t0, Q_BLOCK, axis=1)[..., None]
        return gb[:, :, 0] * o_c + gb[:, :, 1] * o_s + gb[:, :, 2] * o_w

    o = lax.map(block, jnp.arange(seq // Q_BLOCK))
    o = jnp.transpose(o, (1, 0, 2, 3, 4, 5)).reshape(bsz, seq, B_WIDTH)
    return jnp.einsum('bsc,cd->bsd', o, w_out).astype(h.dtype)


def hier_moe(h, w_router, b_router, w_gate_up, w_down):
    bsz, seq, d = h.shape
    n_tok = bsz * seq
    xt = h.reshape(n_tok, d)
    logits = (xt @ w_router + b_router).astype(F32)
    g_logits = logits[:, :N_EXPERT_GROUPS]
    e_logits = logits[:, N_EXPERT_GROUPS:].reshape(n_tok, N_EXPERT_GROUPS, EXPERTS_PER_GROUP)
    p_group = jax.nn.softmax(g_logits, axis=-1)
    g_idx = jnp.argmax(g_logits, axis=-1)
    g_w = jnp.take_along_axis(p_group, g_idx[:, None], axis=1)
    e_in = jnp.take_along_axis(e_logits, g_idx[:, None, None], axis=1)[:, 0]
    e_val, e_idx = lax.top_k(e_in, TOP_K_EXPERTS)
    e_w = jax.nn.softmax(e_val, axis=-1) * g_w
    expert_id = g_idx[:, None] * EXPERTS_PER_GROUP + e_idx
    combine = jnp.sum(jax.nn.one_hot(expert_id, N_EXPERTS, dtype=F32) * e_w[..., None], axis=1)
    chunk = math.gcd(n_tok, MOE_CHUNK)

    def run(args):
        xc, cc = args
        gu = jnp.einsum('td,edf->tef', xc, w_gate_up)
        hid = jax.nn.silu(gu[..., :D_EXPERT]) * gu[..., D_EXPERT:] * cc[..., None].astype(gu.dtype)
        return jnp.einsum('tef,efd->td', hid, w_down)

    y = lax.map(run, (xt.reshape(-1, chunk, d), combine.reshape(-1, chunk, N_EXPERTS)))
    return y.reshape(bsz, seq, d).astype(h.dtype)


def setup_inputs(seed: int = 0) -> dict:
    key = jax.random.key(seed)
    ks = jax.random.split(key, 20)

    def dense(k, shape, fan_in, gain=1.0):
        return jax.random.normal(k, shape, F32) * (gain * fan_in ** -0.5)

    def small(k, shape, s):
        return s * jax.random.normal(k, shape, F32)

    x = jax.random.normal(ks[0], (BATCH, SEQ, D_MODEL), F32)
    v_scale_a = jnp.asarray([1.0, 1.0, DEEPNORM_BETA], F32)[:, None]
    a_w_in = (dense(ks[1], (N_A_LAYERS, D_MODEL, A_N_GROUPS, 3, A_WIDTH), D_MODEL) * v_scale_a).reshape(N_A_LAYERS, D_MODEL, A_IN_COLS)
    a_w_out = dense(ks[2], (N_A_LAYERS, A_WIDTH, D_MODEL), A_WIDTH, DEEPNORM_BETA)
    v_scale_b = jnp.asarray([1.0, DEEPNORM_BETA] * B_N_BRANCH, F32)[:, None]
    b_w_kv = (dense(ks[3], (D_MODEL, 2 * B_N_BRANCH, B_KV_GROUPS * HEAD_DIM), D_MODEL) * v_scale_b).reshape(D_MODEL, B_KV_COLS)
    b_cmp_pos = small(ks[4], (2, CMP_LEN, HEAD_DIM), 0.1)
    b_cmp_w1 = dense(ks[5], (2, CMP_LEN * HEAD_DIM, CMP_HIDDEN), CMP_LEN * HEAD_DIM)
    b_cmp_b1 = small(ks[6], (2, CMP_HIDDEN), 0.01)
    b_cmp_w2 = dense(ks[7], (2, CMP_HIDDEN, HEAD_DIM), CMP_HIDDEN)
    b_cmp_b2 = small(ks[8], (2, HEAD_DIM), 0.01)
    b_w_q = dense(ks[9], (N_B_LAYERS, D_MODEL, B_Q_COLS), D_MODEL)
    b_b_q = small(ks[10], (N_B_LAYERS, B_Q_COLS), 0.01)
    b_w_out = dense(ks[11], (N_B_LAYERS, B_WIDTH, D_MODEL), B_WIDTH, DEEPNORM_BETA)
    moe_w_router = dense(ks[12], (DEPTH, D_MODEL, N_EXPERT_GROUPS + N_EXPERTS), D_MODEL)
    moe_b_router = small(ks[13], (DEPTH, N_EXPERT_GROUPS + N_EXPERTS), 0.01)
    moe_w_gate_up = dense(ks[14], (DEPTH, N_EXPERTS, D_MODEL, 2 * D_EXPERT), D_MODEL)
    moe_w_down = dense(ks[15], (DEPTH, N_EXPERTS, D_EXPERT, D_MODEL), D_EXPERT, DEEPNORM_BETA)
    ln_mix_g = 1.0 + small(ks[16], (DEPTH, D_MODEL), 0.05)
    ln_mix_b = small(ks[17], (DEPTH, D_MODEL), 0.02)
    ln_ffn_g = 1.0 + small(ks[18], (DEPTH, D_MODEL), 0.05)
    ln_ffn_b = small(ks[19], (DEPTH, D_MODEL), 0.02)
    return {'x': x, 'a_w_in': a_w_in, 'a_w_out': a_w_out, 'b_w_kv': b_w_kv,
            'b_cmp_pos': b_cmp_pos, 'b_cmp_w1': b_cmp_w1, 'b_cmp_b1': b_cmp_b1,
            'b_cmp_w2': b_cmp_w2, 'b_cmp_b2': b_cmp_b2, 'b_w_q': b_w_q, 'b_b_q': b_b_q,
            'b_w_out': b_w_out, 'moe_w_router': moe_w_router, 'moe_b_router': moe_b_router,
            'moe_w_gate_up': moe_w_gate_up, 'moe_w_down': moe_w_down,
            'ln_mix_g': ln_mix_g, 'ln_mix_b': ln_mix_b, 'ln_ffn_g': ln_ffn_g, 'ln_ffn_b': ln_ffn_b}


def reference(x, a_w_in, a_w_out, b_w_kv, b_cmp_pos, b_cmp_w1, b_cmp_b1, b_cmp_w2, b_cmp_b2,
              b_w_q, b_b_q, b_w_out, moe_w_router, moe_b_router, moe_w_gate_up, moe_w_down,
              ln_mix_g, ln_mix_b, ln_ffn_g, ln_ffn_b):
    h = x
    shared_kv = None
    for layer in range(DEPTH):
        if layer < N_A_LAYERS:
            mix = dilated_attention(h, a_w_in[layer], a_w_out[layer])
        else:
            if layer == N_A_LAYERS:
                shared_kv = nsa_shared_kv(h, b_w_kv, b_cmp_pos, b_cmp_w1, b_cmp_b1, b_cmp_w2, b_cmp_b2)
            i = layer - N_A_LAYERS
            mix = nsa_attention(h, b_w_q[i], b_b_q[i], b_w_out[i], *shared_kv)
        h = layer_norm(DEEPNORM_ALPHA * h + mix, ln_mix_g[layer], ln_mix_b[layer])
        ffn = hier_moe(h, moe_w_router[layer], moe_b_router[layer], moe_w_gate_up[layer], moe_w_down[layer])
        h = layer_norm(DEEPNORM_ALPHA * h + ffn, ln_ffn_g[layer], ln_ffn_b[layer])
    return h
```

```python
import ml_dtypes
import contextlib
import numpy as np
import concourse.bass as bass
import concourse.mybir as mybir
from concourse.bass_utils import run_bass_kernel_spmd

F32 = mybir.dt.float32
BF16 = mybir.dt.bfloat16
I32 = mybir.dt.int32
U32 = mybir.dt.uint32
AF = mybir.ActivationFunctionType
ALU = mybir.AluOpType
AX = mybir.AxisListType

DMA_RING = 6


class Sched:
    def __init__(self, nc, same_engine_sync=None):
        if same_engine_sync is None:
            same_engine_sync = True
        self.nc = nc
        self.es = contextlib.ExitStack()
        self.engs = {"pe": nc.tensor, "dve": nc.vector, "act": nc.scalar,
                     "pool": nc.gpsimd, "sp": nc.sync}
        self.sem = {}
        self.cnt = {}
        for e in self.engs:
            self.sem[e] = self.es.enter_context(nc.semaphore("s_" + e))
            self.cnt[e] = 0
        self.dsem = {}
        self.dcnt = {}
        self.dissued = {}
        for q in ("sp", "act", "pool"):
            self.dsem[q] = [self.es.enter_context(nc.semaphore("d_%s%d" % (q, i)))
                            for i in range(DMA_RING)]
            self.dcnt[q] = [0] * DMA_RING
            self.dissued[q] = 0
        self.waited = {}
        self.lastw = {}
        self.readers = {}
        self.same = same_engine_sync
        self.excl = set()
        self.ninst = 0

    def sb(self, name, shape, dt):
        return self.es.enter_context(self.nc.sbuf_tensor(name, shape, dt))

    def ps(self, name, shape, dt=F32, key=None):
        self.excl.add(key if key is not None else name)
        return self.es.enter_context(self.nc.psum_tensor(name, shape, dt))

    def _semobj(self, key):
        if isinstance(key, tuple):
            return self.dsem[key[0]][key[1]]
        return self.sem[key]

    def _wait(self, eng, key, val):
        if key == eng and (not self.same or eng == "pe"):
            return
        w = self.waited.get((eng, key), 0)
        if val > w:
            self.engs[eng].wait_ge(self._semobj(key), val)
            self.waited[(eng, key)] = val

    def _deps(self, eng, reads, writes):
        for t in reads:
            lw = self.lastw.get(t)
            if lw is not None:
                self._wait(eng, lw[0], lw[1])
        for t in writes:
            lw = self.lastw.get(t)
            if lw is not None:
                self._wait(eng, lw[0], lw[1])
            for r in self.readers.get(t, ()):
                self._wait(eng, r[0], r[1])

    def _record(self, stamp, reads, writes):
        for t in writes:
            self.lastw[t] = stamp
            self.readers[t] = []
        for t in reads:
            self.readers.setdefault(t, []).append(stamp)

    def op(self, eng, fn, reads=(), writes=()):
        ex = [t for t in reads if t in self.excl]
        if ex:
            reads = [t for t in reads if t not in self.excl]
            writes = list(writes) + ex
        self._deps(eng, reads, writes)
        ins = fn(self.engs[eng])
        self.cnt[eng] += 1
        ins.then_inc(self.sem[eng], 1)
        self._record((eng, self.cnt[eng]), reads, writes)
        self.ninst += 1
        return ins

    def dma(self, q, out, in_, reads=(), writes=(), **kw):
        slot = self.dissued[q] % DMA_RING
        self.dissued[q] += 1
        key = (q, slot)
        if self.dcnt[q][slot] > 0:
            self._wait(q, key, self.dcnt[q][slot])
        self._deps(q, reads, writes)
        ins = self.engs[q].dma_start(out=out, in_=in_, **kw)
        self.dcnt[q][slot] += 16
        ins.then_inc(self.dsem[q][slot], 16)
        self._record((key, self.dcnt[q][slot]), reads, writes)
        self.ninst += 1
        return ins

    def finish(self, out_tiles):
        for t in out_tiles:
            lw = self.lastw.get(t)
            if lw is not None:
                self._wait("sp", lw[0], lw[1])

    def close(self):
        self.es.close()


def build_proj(NT, N, out_bf16=True, has_bias=False):
    nc = bass.Bass("TRN2", target_bir_lowering=False)
    T = NT * 128
    xT = nc.dram_tensor("xT", [1024, T], F32, kind="ExternalInput").ap()
    w = nc.dram_tensor("w", [1024, N], F32, kind="ExternalInput").ap()
    odt = BF16 if out_bf16 else F32
    y = nc.dram_tensor("y", [T, N], odt, kind="ExternalOutput").ap()
    if has_bias:
        bias = nc.dram_tensor("bias", [N], F32, kind="ExternalInput").ap()
    s = Sched(nc)
    if has_bias:
        bias_bc = s.sb("bias_bc", [128, N], F32)
        s.dma("sp", bias_bc[:, :], bias.partition_broadcast(128), writes=["bias_bc"])
    x_f = s.sb("x_f", [128, 8, 512], F32)
    x_b = s.sb("x_b", [128, 8, T], BF16)
    xTv = xT.rearrange("(kc p) t -> p kc t", p=128)
    for c in range(T // 512):
        s.dma("sp", x_f[:, :, :], xTv[:, :, c * 512:(c + 1) * 512], writes=["x_f"])
        s.op("dve", lambda e: e.tensor_copy(out=x_b[:, :, c * 512:(c + 1) * 512], in_=x_f[:, :, :]),
             reads=["x_f"], writes=[("x_b", c)])
    CW = 512
    nch = (N + CW - 1) // CW
    w_f = [s.sb("w_f%d" % i, [128, 8, CW], F32) for i in range(2)]
    w_b = [s.sb("w_b%d" % i, [128, 8, CW], BF16) for i in range(2)]
    pt = [s.ps("pt%d" % i, [128, CW], key=("pt", i)) for i in range(4)]
    ot = [s.sb("ot%d" % i, [128, CW], odt) for i in range(4)]
    wv = w.rearrange("(kc p) n -> p kc n", p=128)
    k = 0

    def load_chunk(ch):
        n0 = ch * CW
        cw = min(CW, N - n0)
        b = ch % 2
        s.dma("sp", w_f[b][:, :, :cw], wv[:, :, n0:n0 + cw], writes=[("w_f", b)])
        s.op("pool", lambda e: e.tensor_copy(out=w_b[b][:, :, :cw], in_=w_f[b][:, :, :cw]),
             reads=[("w_f", b)], writes=[("w_b", b)])

    load_chunk(0)
    for ch in range(nch):
        n0 = ch * CW
        cw = min(CW, N - n0)
        b = ch % 2
        if ch + 1 < nch:
            load_chunk(ch + 1)
        for t in range(NT):
            pb = k % 4
            k += 1
            for kc in range(8):
                s.op("pe", lambda e: e.matmul(pt[pb][:, :cw], lhsT=x_b[:, kc, t * 128:(t + 1) * 128],
                                              rhs=w_b[b][:, kc, :cw], start=(kc == 0), stop=(kc == 7)),
                     reads=[("x_b", t // 4), ("w_b", b)], writes=[("pt", pb)])
            if has_bias:
                s.op("dve", lambda e: e.tensor_tensor(out=ot[pb][:, :cw], in0=pt[pb][:, :cw], in1=bias_bc[:, n0:n0 + cw], op=ALU.add),
                     reads=[("pt", pb), "bias_bc"], writes=[("ot", pb)])
            elif pb % 2 == 0:
                s.op("act", lambda e: e.copy(out=ot[pb][:, :cw], in_=pt[pb][:, :cw]),
                     reads=[("pt", pb)], writes=[("ot", pb)])
            else:
                s.op("dve", lambda e: e.tensor_copy(out=ot[pb][:, :cw], in_=pt[pb][:, :cw]),
                     reads=[("pt", pb)], writes=[("ot", pb)])
            s.dma("sp" if pb % 2 else "pool", y[t * 128:(t + 1) * 128, n0:n0 + cw], ot[pb][:, :cw],
                  reads=[("ot", pb)], writes=[("y", t, ch)])
    s.finish([("y", t, ch) for t in range(NT) for ch in range(nch)])
    print("ninst", s.ninst)
    s.close()
    return nc


A_DIL = (1, 4, 16)


def atta_bias_table():
    slopes = (2.0 ** (-8.0 * np.arange(1, 17) / 16)).astype(np.float32)
    k = np.arange(128)[:, None]
    q = np.arange(128)[None, :]
    tab = np.zeros((3, 128, 2, 16, 128), np.float32)
    for g, d in enumerate(A_DIL):
        for ch in range(2):
            dist = (q - k + (128 if ch == 0 else 0))
            valid = (dist >= 0) & (dist <= 128)
            for hh in range(16):
                b = -(slopes[hh] * (dist * d).astype(np.float32))
                tab[g, :, ch, hh, :] = np.where(valid, b, -1.0e30)
    return tab


def build_atta(units, nslot):
    nc = bass.Bass("TRN2", target_bir_lowering=False)
    U = len(units)
    qT = nc.dram_tensor("qT", [U, 64, 16, 128], BF16, kind="ExternalInput").ap()
    kT = nc.dram_tensor("kT", [U, 64, 16, 256], BF16, kind="ExternalInput").ap()
    v = nc.dram_tensor("v", [U, 2, 128, 16, 64], BF16, kind="ExternalInput").ap()
    tab = nc.dram_tensor("tab", [nslot, 128, 2, 16, 128], F32, kind="ExternalInput").ap()
    o = nc.dram_tensor("o", [U, 128, 16 * 65], F32, kind="ExternalOutput").ap()
    s = Sched(nc)
    tb = s.sb("tb", [128, 2, 16, 128], F32)
    q_sb = [s.sb("q_sb%d" % i, [64, 16, 128], BF16) for i in range(2)]
    k_sb = [s.sb("k_sb%d" % i, [64, 16, 256], BF16) for i in range(2)]
    v_sb = [s.sb("v_sb%d" % i, [128, 2, 16, 65], BF16) for i in range(2)]
    tmp = [s.sb("tmp%d" % i, [128, 2, 512], F32) for i in range(2)]
    pT = [s.sb("pT%d" % i, [128, 2, 512], BF16) for i in range(2)]
    o_sb = [s.sb("o_sb%d" % i, [128, 16, 65], F32) for i in range(2)]
    ps_s = [[s.ps("ps_s%d_%d" % (i, c), [128, 512], key=("ps_s", i, c)) for c in range(2)] for i in range(2)]
    ps_o = [s.ps("ps_o%d" % i, [128, 4, 128], key=("ps_o", i)) for i in range(2)]
    for i in range(2):
        s.op("pool", lambda e: e.memset(v_sb[i][:, :, :, 64:65], 1.0), writes=[("v_sb", i)])
    okeys = []
    state = {"cur_g": -1}
    items = [(u, hg) for u in range(U) for hg in range(4)]

    def S(n):
        u, hg = items[n]
        g = units[u]
        b = u % 2
        i = n % 2
        if hg == 0:
            if g != state["cur_g"]:
                s.dma("sp", tb[:, :, :, :], tab[g], writes=["tb"])
                state["cur_g"] = g
            s.dma("sp", q_sb[b][:, :, :], qT[u], writes=[("q_sb", b)])
            s.dma("sp", k_sb[b][:, :, :], kT[u], writes=[("k_sb", b)])
            for c in range(2):
                s.dma("sp", v_sb[b][:, c, :, 0:64], v[u, c], writes=[("v_sb", b)], reads=[("v_sb", b)])
        for c in range(2):
            for hh in range(4):
                hd = hg * 4 + hh
                s.op("pe", lambda e: e.matmul(ps_s[i][c][:, hh * 128:(hh + 1) * 128],
                                              lhsT=k_sb[b][:, hd, c * 128:(c + 1) * 128], rhs=q_sb[b][:, hd, :],
                                              start=True, stop=True),
                     reads=[("k_sb", b), ("q_sb", b)], writes=[("ps_s", i, c)])
            s.op("dve", lambda e: e.scalar_tensor_tensor(
                out=tmp[i][:, c, :], in0=ps_s[i][c][:, :], scalar=0.125,
                in1=tb[:, c, hg * 4:hg * 4 + 4, :].rearrange("p a b -> p (a b)"),
                op0=ALU.mult, op1=ALU.add),
                reads=[("ps_s", i, c), "tb"], writes=[("tmp", i, c)])
            s.op("act", lambda e: e.activation(out=pT[i][:, c, :], in_=tmp[i][:, c, :], func=AF.Exp),
                 reads=[("tmp", i, c)], writes=[("pT", i, c)])

    def PV(n):
        u, hg = items[n]
        b = u % 2
        i = n % 2
        for hh in range(4):
            hd = hg * 4 + hh
            for c in range(2):
                s.op("pe", lambda e: e.matmul(ps_o[i][:, hh, 0:65], lhsT=pT[i][:, c, hh * 128:(hh + 1) * 128],
                                              rhs=v_sb[b][:, c, hd, :], start=(c == 0), stop=(c == 1)),
                     reads=[("pT", i, c), ("v_sb", b)], writes=[("ps_o", i)])
        s.op("dve" if hg % 2 else "act", (lambda e: e.tensor_copy(out=o_sb[b][:, hg * 4:hg * 4 + 4, :], in_=ps_o[i][:, :, 0:65])) if hg % 2
             else (lambda e: e.copy(out=o_sb[b][:, hg * 4:hg * 4 + 4, :], in_=ps_o[i][:, :, 0:65])),
             reads=[("ps_o", i)], writes=[("o_sb", b)])
        if hg == 3:
            s.dma("sp", o[u], o_sb[b][:, :, :].rearrange("p a b -> p (a b)"), reads=[("o_sb", b)], writes=[("o", u)])
            okeys.append(("o", u))

    S(0)
    for n in range(len(items)):
        if n + 1 < len(items):
            S(n + 1)
        PV(n)
    s.finish(okeys)
    print("atta ninst", s.ninst)
    s.close()
    return nc


def atta_host_prep(qkv):
    S = qkv.shape[0]
    x = qkv.reshape(S, 3, 3, 16, 64)
    qTs, kTs, vs, meta = [], [], [], []
    for g, d in enumerate(A_DIL):
        L = S // d
        nb = L // 128
        def perm(a):
            return a.reshape(L, d, 16, 64).transpose(1, 0, 2, 3)
        q = perm(x[:, g, 0]).reshape(d, nb, 128, 16, 64)
        k = perm(x[:, g, 1])
        vv = perm(x[:, g, 2])
        z = np.zeros((d, 128, 16, 64), qkv.dtype)
        kp = np.concatenate([z, k], axis=1).reshape(d, nb + 1, 128, 16, 64)
        vp = np.concatenate([z, vv], axis=1).reshape(d, nb + 1, 128, 16, 64)
        qT = q.transpose(0, 1, 4, 3, 2).reshape(d * nb, 64, 16, 128)
        k2 = np.stack([kp[:, :-1], kp[:, 1:]], axis=2)
        kT = k2.transpose(0, 1, 5, 4, 2, 3).reshape(d * nb, 64, 16, 256)
        v2 = np.stack([vp[:, :-1], vp[:, 1:]], axis=2).reshape(d * nb, 2, 128, 16, 64)
        qTs.append(np.ascontiguousarray(qT))
        kTs.append(np.ascontiguousarray(kT))
        vs.append(np.ascontiguousarray(v2))
        meta.append([(g, (i % nb) == 0) for i in range(d * nb)])
    return qTs, kTs, vs, meta


def atta_host_post(o_groups, S):
    out = []
    for g, d in enumerate(A_DIL):
        L = S // d
        a = o_groups[g].reshape(d, L, 1040).transpose(1, 0, 2).reshape(S, 1040)
        out.append(a)
    return np.stack(out)


ALPHA = 8.0 ** 0.25
LN_EPS = 1e-5
BIG = 1.0e30


def build_post(T, modeA):
    nc = bass.Bass("TRN2", target_bir_lowering=False)
    D = 1024
    NTL = T // 128
    SG = min(1024, T)
    NSG = T // SG
    TPS = SG // 128

    def din(name, shape, dt=F32):
        return nc.dram_tensor(name, shape, dt, kind="ExternalInput").ap()

    h = din("h", [T, D])
    if modeA:
        og = din("og", [3, T, 16 * 65])
    else:
        og = din("og", [3, T, 16 * 65])
        gl = din("gl", [T, 48], BF16)
    wout = din("wout", [D, D])
    lnp = din("lnp", [4, D])
    wr = din("wr", [D, 20])
    br = din("br", [20])
    wgu = din("wgu", [16, D, 512])
    wd = din("wd", [16, 256, D])
    ident = din("ident", [128, 128])
    sel = din("sel", [128, 16 * 128])
    hout = nc.dram_tensor("hout", [T, D], F32, kind="ExternalOutput").ap()

    s = Sched(nc)
    ident_f = s.sb("ident_f", [128, 128], F32)
    ident_b = s.sb("ident_b", [128, 128], BF16)
    sel_f = s.sb("sel_f", [128, 16 * 128], F32)
    lnbc = s.sb("lnbc", [128, 4, D], F32)
    br_bc = s.sb("br_bc", [128, 20], F32)
    wr_f = s.sb("wr_f", [128, 8, 20], F32)
    wout_b = s.sb("wout_b", [128, 8, D], BF16)
    eps_t = s.sb("eps_t", [128, 1], F32)
    stage = [s.sb("stage%d" % i, [128, 4096], F32) for i in range(2)]
    wgu_b = [s.sb("wgu_b%d" % i, [128, 8, 512], BF16) for i in range(2)]
    wd_b = [s.sb("wd_b%d" % i, [128, 2, D], BF16) for i in range(2)]
    nstage = [0]

    def stage_load(src_ap, n_inner, dst_ap, dkey):
        b = nstage[0] % 2
        nstage[0] += 1
        sv = stage[b][:, :src_ap.shape[1] * src_ap.shape[2]].rearrange("p (a b) -> p a b", a=src_ap.shape[1])
        s.dma("sp", sv, src_ap, writes=[("stage", b)])
        s.op("pool", lambda e: e.tensor_copy(out=dst_ap, in_=sv), reads=[("stage", b)], writes=[dkey])

    s.dma("sp", ident_f[:, :], ident, writes=["ident_f"])
    s.op("dve", lambda e: e.tensor_copy(out=ident_b[:, :], in_=ident_f[:, :]), reads=["ident_f"], writes=["ident_b"])
    s.dma("sp", sel_f[:, :], sel, writes=["sel_f"])
    for i in range(4):
        s.dma("sp", lnbc[:, i, :], lnp[i].partition_broadcast(128), writes=[("lnbc", i)])
    s.dma("sp", br_bc[:, :], br.partition_broadcast(128), writes=["br_bc"])
    s.dma("sp", wr_f[:, :, :], wr.rearrange("(kc p) n -> p kc n", p=128), writes=["wr_f"])
    s.op("dve", lambda e: e.memset(eps_t[:, :], LN_EPS), writes=["eps_t"])
    woutv = wout.rearrange("(kc p) n -> p kc n", p=128)
    for i in range(2):
        stage_load(woutv[:, 4 * i:4 * i + 4, :], None, wout_b[:, 4 * i:4 * i + 4, :], ("wout_b", i))

    h_t = s.sb("h_t", [128, D], F32)
    if modeA:
        og_t = s.sb("og_t", [128, 3, 16 * 65], F32)
        acc = s.sb("acc", [128, 16, 65], F32)
        rl = s.sb("rl", [128, 16], F32)
    else:
        og_t = s.sb("og_t", [128, 3, 16 * 65], F32)
        gl_t = s.sb("gl_t", [128, 48], BF16)
        gate = s.sb("gate", [128, 3, 16], F32)
        rl3 = s.sb("rl3", [128, 3, 16], F32)

    o_b = s.sb("o_b", [128, D], BF16)
    oT = s.sb("oT", [128, 8, 128], BF16)
    r = s.sb("r", [128, D], F32)
    xn = s.sb("xn", [128, D], F32)
    tA = r[:, :].rearrange("p (a b) -> p a b", a=16)
    tB = xn[:, :].rearrange("p (a b) -> p a b", a=16)
    h1 = s.sb("h1", [128, D], F32)
    st = s.sb("st", [128, 2, 6], F32)
    mv = s.sb("mv", [128, 2], F32)
    rstd = s.sb("rstd", [128, 1], F32)
    yacc = s.sb("yacc", [128, TPS, D], F32)
    h1T_b = s.sb("h1T_b", [128, 8, SG], BF16)
    h1T_f = s.sb("h1T_f", [128, 8, 128], F32)
    combT = s.sb("combT", [128, SG], F32)
    L = s.sb("L", [128, 20], F32)
    sm = s.sb("sm", [128, 16], F32)
    goh = s.sb("goh", [128, 4], F32)
    em = s.sb("em", [128, 4, 4], F32)
    top8 = s.sb("top8", [128, 8], F32)
    c0 = s.sb("c0", [128, 16], F32)
    c1 = s.sb("c1", [128, 16], F32)
    comb = s.sb("comb", [128, 128], F32)
    s.op("dve", lambda e: e.memset(comb[:, :], 0.0), writes=["comb"])
    sg = [s.sb("sg%d" % i, [128, 512], F32) for i in range(2)]
    tt_ = [s.sb("tt%d" % i, [128, 512], F32) for i in range(2)]
    hid2 = [[s.sb("hid%d_%d" % (p, i), [128, 512], BF16) for i in range(2)] for p in range(2)]
    cb2 = [s.sb("cb2_%d" % p, [128, 512], F32) for p in range(2)]
    gu = [s.ps("gu%d" % i, [128, 512], key=("gu", i)) for i in range(4)]
    pcb = s.ps("pcb", [128, 512])
    py = [s.ps("py%d" % i, [128, 512], key=("py", i)) for i in range(2)]
    pT_b = s.ps("pT_b", [128, 1024], BF16)

    def layer_norm(src, src_key, gi, dst, dst_key):
        for i in range(2):
            s.op("dve", lambda e: e.bn_stats(out=st[:, i, :], in_=src[:, i * 512:(i + 1) * 512]),
                 reads=[src_key], writes=[("st", i)])
        s.op("dve", lambda e: e.bn_aggr(out=mv[:, :], in_=st[:, :, :]), reads=[("st", 0), ("st", 1)], writes=["mv"])
        s.op("act", lambda e: e.activation(out=rstd[:, :], in_=mv[:, 1:2], func=AF.Sqrt, bias=eps_t[:, :], scale=1.0),
             reads=["mv", "eps_t"], writes=["rstd"])
        s.op("dve", lambda e: e.reciprocal(out=rstd[:, :], in_=rstd[:, :]), reads=["rstd"], writes=["rstd"])
        s.op("dve", lambda e: e.tensor_scalar(out=xn[:, :], in0=src[:, :], scalar1=mv[:, 0:1], scalar2=rstd[:, 0:1],
                                              op0=ALU.subtract, op1=ALU.mult),
             reads=[src_key, "mv", "rstd"], writes=["xn"])
        s.op("pool", lambda e: e.tensor_tensor(out=xn[:, :], in0=xn[:, :], in1=lnbc[:, gi, :], op=ALU.mult),
             reads=["xn", ("lnbc", gi)], writes=["xn"])
        s.op("pool", lambda e: e.tensor_tensor(out=dst, in0=xn[:, :], in1=lnbc[:, gi + 1, :], op=ALU.add),
             reads=["xn", ("lnbc", gi + 1)], writes=[dst_key])

    STOP = 99

    class StopBuild(Exception):
        pass

    def ck(n):
        if STOP == n:
            raise StopBuild()

    out_keys = []
    try:
      ck(1)
      for sgi in range(NSG):
          for ti in range(TPS):
              gt = sgi * TPS + ti
              rows = slice(gt * 128, (gt + 1) * 128)
              s.dma("sp", h_t[:, :], h[rows, :], writes=["h_t"])
              if modeA:
                  s.dma("sp", og_t[:, :, :], og[:, rows, :].rearrange("g t c -> t g c"), writes=["og_t"])
                  accf = acc[:, :, :].rearrange("p a b -> p (a b)")
                  s.op("dve", lambda e: e.tensor_tensor(out=accf, in0=og_t[:, 0, :], in1=og_t[:, 1, :], op=ALU.add),
                       reads=["og_t"], writes=["acc"])
                  s.op("dve", lambda e: e.tensor_tensor(out=accf, in0=accf, in1=og_t[:, 2, :], op=ALU.add),
                       reads=["og_t", "acc"], writes=["acc"])
                  s.op("dve", lambda e: e.reciprocal(out=rl[:, :], in_=acc[:, :, 64]), reads=["acc"], writes=["rl"])
                  s.op("dve", lambda e: e.tensor_tensor(out=o_b[:, :].rearrange("p (a b) -> p a b", a=16),
                                                        in0=acc[:, :, 0:64],
                                                        in1=rl[:, :].unsqueeze(2).to_broadcast([128, 16, 64]),
                                                        op=ALU.mult),
                       reads=["acc", "rl"], writes=["o_b"])
              else:
                  s.dma("sp", og_t[:, :, :], og[:, rows, :].rearrange("g t c -> t g c"), writes=["og_t"])
                  s.dma("sp", gl_t[:, :], gl[rows, :], writes=["gl_t"])
                  s.op("act", lambda e: e.activation(out=gate[:, :, :].rearrange("p a b -> p (a b)"), in_=gl_t[:, :], func=AF.Sigmoid),
                       reads=["gl_t"], writes=["gate"])
                  ogv = og_t[:, :, :].rearrange("p g (h c) -> p g h c", c=65)
                  s.op("dve", lambda e: e.tensor_scalar(out=rl3[:, :, :], in0=ogv[:, :, :, 64], scalar1=1.0e-30, scalar2=None, op0=ALU.max),
                       reads=["og_t"], writes=["rl3"])
                  s.op("dve", lambda e: e.reciprocal(out=rl3[:, :, :], in_=rl3[:, :, :]), reads=["rl3"], writes=["rl3"])
                  s.op("dve", lambda e: e.tensor_tensor(out=rl3[:, :, :], in0=rl3[:, :, :], in1=gate[:, :, :], op=ALU.mult),
                       reads=["rl3", "gate"], writes=["rl3"])
                  s.op("dve", lambda e: e.tensor_tensor(out=tA[:, :, :], in0=ogv[:, 0, :, 0:64],
                                                        in1=rl3[:, 0, :].unsqueeze(2).to_broadcast([128, 16, 64]), op=ALU.mult),
                       reads=["og_t", "rl3"], writes=["r"])
                  s.op("dve", lambda e: e.tensor_tensor(out=tB[:, :, :], in0=ogv[:, 1, :, 0:64],
                                                        in1=rl3[:, 1, :].unsqueeze(2).to_broadcast([128, 16, 64]), op=ALU.mult),
                       reads=["og_t", "rl3"], writes=["xn"])
                  s.op("dve", lambda e: e.tensor_tensor(out=tA[:, :, :], in0=tA[:, :, :], in1=tB[:, :, :], op=ALU.add),
                       reads=["r", "xn"], writes=["r"])
                  s.op("dve", lambda e: e.tensor_tensor(out=tB[:, :, :], in0=ogv[:, 2, :, 0:64],
                                                        in1=rl3[:, 2, :].unsqueeze(2).to_broadcast([128, 16, 64]), op=ALU.mult),
                       reads=["og_t", "rl3"], writes=["xn"])
                  s.op("dve", lambda e: e.tensor_tensor(out=o_b[:, :].rearrange("p (a b) -> p a b", a=16), in0=tA[:, :, :], in1=tB[:, :, :], op=ALU.add),
                       reads=["r", "xn"], writes=["o_b"])
              for kc in range(8):
                  s.op("pe", lambda e: e.transpose(out=pT_b[:, kc * 128:(kc + 1) * 128],
                                                   in_=o_b[:, kc * 128:(kc + 1) * 128], identity=ident_b[:, :]),
                       reads=["o_b", "ident_b"], writes=["pT_b"])
              s.op("act", lambda e: e.copy(out=oT[:, :, :].rearrange("p a b -> p (a b)"), in_=pT_b[:, :]),
                   reads=["pT_b"], writes=["oT"])
              for hh in range(2):
                  for kc in range(8):
                      s.op("pe", lambda e: e.matmul(gu[hh][:, :], lhsT=oT[:, kc, :],
                                                    rhs=wout_b[:, kc, hh * 512:(hh + 1) * 512],
                                                    start=(kc == 0), stop=(kc == 7)),
                           reads=["oT", ("wout_b", kc // 4)], writes=[("gu", hh)])
                  s.op("dve", lambda e: e.scalar_tensor_tensor(out=r[:, hh * 512:(hh + 1) * 512],
                                                               in0=h_t[:, hh * 512:(hh + 1) * 512], scalar=ALPHA,
                                                               in1=gu[hh][:, :], op0=ALU.mult, op1=ALU.add),
                       reads=["h_t", ("gu", hh)], writes=["r"])
              ck(2)
              layer_norm(r, "r", 0, h1[:, :], "h1")
              ck(3)
              s.op("act", lambda e: e.mul(out=yacc[:, ti, :], in_=h1[:, :], mul=ALPHA),
                   reads=["h1"], writes=[("yacc", ti)])
              ck(31)
              for q in range(2):
                  for j in range(4):
                      kc = q * 4 + j
                      s.op("pe", lambda e: e.transpose(out=gu[2 + q][:, j * 128:(j + 1) * 128],
                                                       in_=h1[:, kc * 128:(kc + 1) * 128], identity=ident_f[:, :]),
                           reads=["h1", "ident_f"], writes=[("gu", 2 + q)])
                  ck(32)
                  s.op("act", lambda e: e.copy(out=h1T_f[:, q * 4:q * 4 + 4, :],
                                               in_=gu[2 + q][:, :].rearrange("p (a b) -> p a b", a=4)),
                       reads=[("gu", 2 + q)], writes=[("h1T_f", q)])
                  ck(33)
                  s.op("dve", lambda e: e.tensor_copy(out=h1T_b[:, q * 4:q * 4 + 4, ti * 128:(ti + 1) * 128],
                                                      in_=gu[2 + q][:, :].rearrange("p (a b) -> p a b", a=4)),
                       reads=[("gu", 2 + q)], writes=[("h1T_b", ti)])
              ck(4)
              for kc in range(8):
                  s.op("pe", lambda e: e.matmul(pcb[:, 0:20], lhsT=h1T_f[:, kc, :], rhs=wr_f[:, kc, :],
                                                start=(kc == 0), stop=(kc == 7)),
                       reads=[("h1T_f", kc // 4), "wr_f"], writes=["pcb"])
              s.op("dve", lambda e: e.tensor_tensor(out=L[:, :], in0=pcb[:, 0:20], in1=br_bc[:, :], op=ALU.add),
                   reads=["pcb", "br_bc"], writes=["L"])
              ck(5)
              V = lambda eng, fn, rd, wr_: s.op(eng, fn, reads=rd, writes=wr_)
              V("dve", lambda e: e.reduce_max(out=sm[:, 0:1], in_=L[:, 0:4], axis=AX.X), ["L"], ["sm"])
              V("dve", lambda e: e.tensor_scalar(out=sm[:, 1:2], in0=sm[:, 0:1], scalar1=-1.0, scalar2=None, op0=ALU.mult),
                ["sm"], ["sm"])
              V("act", lambda e: e.activation(out=goh[:, :], in_=L[:, 0:4], func=AF.Exp, bias=sm[:, 1:2], scale=1.0),
                ["L", "sm"], ["goh"])
              V("dve", lambda e: e.reduce_sum(out=sm[:, 2:3], in_=goh[:, :], axis=AX.X), ["goh"], ["sm"])
              V("dve", lambda e: e.reciprocal(out=sm[:, 3:4], in_=sm[:, 2:3]), ["sm"], ["sm"])
              V("dve", lambda e: e.tensor_scalar(out=goh[:, :], in0=L[:, 0:4], scalar1=sm[:, 0:1], scalar2=None,
                                                 op0=ALU.is_equal), ["L", "sm", "goh"], ["goh"])
              V("dve", lambda e: e.tensor_scalar(out=goh[:, :], in0=goh[:, :], scalar1=BIG, scalar2=BIG,
                                                 op0=ALU.mult, op1=ALU.subtract), ["goh"], ["goh"])
              V("dve", lambda e: e.tensor_tensor(out=em[:, :, :], in0=L[:, 4:20].rearrange("p (a b) -> p a b", a=4),
                                                 in1=goh[:, :].unsqueeze(2).to_broadcast([128, 4, 4]), op=ALU.add),
                ["L", "goh"], ["em"])
              emf = em[:, :, :].rearrange("p a b -> p (a b)")
              V("dve", lambda e: e.max(out=top8[:, :], in_=emf), ["em"], ["top8"])
              V("dve", lambda e: e.tensor_tensor(out=sm[:, 4:5], in0=top8[:, 1:2], in1=top8[:, 0:1], op=ALU.subtract),
                ["top8", "sm"], ["sm"])
              V("act", lambda e: e.activation(out=sm[:, 5:6], in_=sm[:, 4:5], func=AF.Exp), ["sm"], ["sm"])
              V("dve", lambda e: e.tensor_scalar(out=sm[:, 6:7], in0=sm[:, 5:6], scalar1=1.0, scalar2=None, op0=ALU.add),
                ["sm"], ["sm"])
              V("dve", lambda e: e.reciprocal(out=sm[:, 6:7], in_=sm[:, 6:7]), ["sm"], ["sm"])
              V("dve", lambda e: e.tensor_tensor(out=sm[:, 7:8], in0=sm[:, 6:7], in1=sm[:, 3:4], op=ALU.mult), ["sm"], ["sm"])
              V("dve", lambda e: e.tensor_tensor(out=sm[:, 8:9], in0=sm[:, 7:8], in1=sm[:, 5:6], op=ALU.mult), ["sm"], ["sm"])
              V("dve", lambda e: e.tensor_scalar(out=c0[:, :], in0=emf, scalar1=top8[:, 0:1], scalar2=sm[:, 7:8],
                                                 op0=ALU.is_equal, op1=ALU.mult), ["em", "top8", "sm"], ["c0"])
              V("dve", lambda e: e.tensor_scalar(out=c1[:, :], in0=emf, scalar1=top8[:, 1:2], scalar2=sm[:, 8:9],
                                                 op0=ALU.is_equal, op1=ALU.mult), ["em", "top8", "sm"], ["c1"])
              V("dve", lambda e: e.tensor_tensor(out=comb[:, 0:16], in0=c0[:, :], in1=c1[:, :], op=ALU.add),
                ["c0", "c1"], ["comb"])
              ck(6)
              V("pe", lambda e: e.transpose(out=pcb[:, 128:256], in_=comb[:, :], identity=ident_f[:, :]),
                ["comb", "ident_f"], ["pcb"])
              V("act", lambda e: e.copy(out=combT[:, ti * 128:(ti + 1) * 128], in_=pcb[:, 128:256]),
                ["pcb"], [("combT", ti)])

          ck(7)
          NSUB = SG // 512
          units = [(ex, sub) for ex in range(16) for sub in range(NSUB)]

          def load_w(ex):
              wb = ex % 2
              stage_load(wgu[ex].rearrange("(kc p) n -> p kc n", p=128), None, wgu_b[wb][:, :, :], ("wgu_b", wb))
              stage_load(wd[ex].rearrange("(fc p) n -> p fc n", p=128), None, wd_b[wb][:, :, :], ("wd_b", wb))

          def GU(n, fc):
              ex, sub = units[n]
              wb = ex % 2
              tok = slice(sub * 512, (sub + 1) * 512)
              tkeys = [("h1T_b", sub * 4 + i) for i in range(4)]
              for j in (fc, 2 + fc):
                  for kc in range(8):
                      s.op("pe", lambda e: e.matmul(gu[j][:, :], lhsT=wgu_b[wb][:, kc, j * 128:(j + 1) * 128],
                                                    rhs=h1T_b[:, kc, tok], start=(kc == 0), stop=(kc == 7)),
                           reads=[("wgu_b", wb)] + tkeys, writes=[("gu", j)])

          def CB(n):
              ex, sub = units[n]
              tok = slice(sub * 512, (sub + 1) * 512)
              s.op("pe", lambda e: e.matmul(pcb[:, :], lhsT=sel_f[:, ex * 128:(ex + 1) * 128], rhs=combT[:, tok],
                                            start=True, stop=True),
                   reads=["sel_f"] + [("combT", sub * 4 + i) for i in range(4)], writes=["pcb"])
              s.op("act", lambda e: e.copy(out=cb2[n % 2][:, :], in_=pcb[:, :]), reads=["pcb"], writes=[("cb", n % 2)])

          def EW(n, fc):
              p = n % 2
              s.op("act", lambda e: e.activation(out=sg[fc][:, :], in_=gu[fc][:, :], func=AF.Silu),
                   reads=[("gu", fc)], writes=[("sg", fc)])
              s.op("dve", lambda e: e.tensor_tensor(out=tt_[fc][:, :], in0=gu[2 + fc][:, :], in1=cb2[p][:, :], op=ALU.mult),
                   reads=[("gu", 2 + fc), ("cb", p)], writes=[("tt", fc)])
              s.op("pool", lambda e: e.tensor_tensor(out=hid2[p][fc][:, :], in0=sg[fc][:, :], in1=tt_[fc][:, :], op=ALU.mult),
                   reads=[("sg", fc), ("tt", fc)], writes=[("hid", p, fc)])

          def DOWN(n):
              ex, sub = units[n]
              wb = ex % 2
              p = n % 2
              for t4 in range(4):
                  ti = sub * 4 + t4
                  for hh in range(2):
                      for fc in range(2):
                          s.op("pe", lambda e: e.matmul(py[hh][:, :], lhsT=hid2[p][fc][:, t4 * 128:(t4 + 1) * 128],
                                                        rhs=wd_b[wb][:, fc, hh * 512:(hh + 1) * 512],
                                                        start=(fc == 0), stop=(fc == 1)),
                               reads=[("hid", p, fc), ("wd_b", wb)], writes=[("py", hh)])
                      s.op("dve", lambda e: e.tensor_tensor(out=yacc[:, ti, hh * 512:(hh + 1) * 512],
                                                            in0=yacc[:, ti, hh * 512:(hh + 1) * 512],
                                                            in1=py[hh][:, :], op=ALU.add),
                           reads=[("yacc", ti), ("py", hh)], writes=[("yacc", ti)])

          load_w(0)
          GU(0, 0); CB(0); EW(0, 0); GU(0, 1); EW(0, 1)
          for n in range(len(units)):
              ex, sub = units[n]
              if sub == 0 and ex + 1 < 16:
                  load_w(ex + 1)
              if n + 1 < len(units):
                  GU(n + 1, 0); CB(n + 1); EW(n + 1, 0)
              DOWN(n)
              if n + 1 < len(units):
                  GU(n + 1, 1); EW(n + 1, 1)
          ck(8)
          for ti in range(TPS):
              gt = sgi * TPS + ti
              layer_norm(yacc[:, ti, :], ("yacc", ti), 2, h1[:, :], "h1")
              s.dma("sp", hout[gt * 128:(gt + 1) * 128, :], h1[:, :], reads=["h1"], writes=[("hout", gt)])
              out_keys.append(("hout", gt))
    except StopBuild:
        pass
    s.finish(out_keys)
    print("post ninst", s.ninst)
    s.close()
    return nc


def post_consts():
    ident = np.eye(128, dtype=np.float32)
    sel = np.zeros((128, 16 * 128), np.float32)
    for e in range(16):
        sel[e, e * 128:(e + 1) * 128] = 1.0
    return {"ident": ident, "sel": sel}


def build_cmp():
    nc = bass.Bass("TRN2", target_bir_lowering=False)

    def din(name, shape, dt=F32):
        return nc.dram_tensor(name, shape, dt, kind="ExternalInput").ap()
    xT = din("xT", [2048, 1024], BF16)
    pos = din("pos", [2048])
    w1 = din("w1", [2048, 256])
    b1 = din("b1", [256])
    w2 = din("w2", [256, 64])
    b2 = din("b2", [64])
    outT = nc.dram_tensor("outT", [64, 1024], BF16, kind="ExternalOutput").ap()
    s = Sched(nc)
    x_in = s.sb("x_in", [128, 16, 1024], BF16)
    x_b = s.sb("x_b", [128, 16, 1024], BF16)
    pos_sb = s.sb("pos_sb", [128, 16], F32)
    w1_f = s.sb("w1_f", [128, 16, 256], F32)
    w1_b = s.sb("w1_b", [128, 16, 256], BF16)
    b1_sb = s.sb("b1_sb", [128, 2], F32)
    w2_f = s.sb("w2_f", [128, 2, 64], F32)
    w2_b = s.sb("w2_b", [128, 2, 64], BF16)
    b2_sb = s.sb("b2_sb", [64, 1], F32)
    xh = s.sb("xh", [128, 512], F32)
    x2 = s.sb("x2", [128, 512], F32)
    sg = s.sb("sg", [128, 512], F32)
    hid = s.sb("hid", [128, 2, 1024], BF16)
    o_sb = s.sb("o_sb", [64, 1024], BF16)
    ph = [s.ps("ph%d" % i, [128, 512], key=("ph", i)) for i in range(2)]
    po = s.ps("po", [128, 512], key="po")
    s.dma("sp", x_in[:, :, :], xT.rearrange("(kc p) n -> p kc n", p=128), writes=["x_in"])
    s.dma("sp", pos_sb[:, :], pos.rearrange("(kc p) -> p kc", p=128), writes=["pos_sb"], allow_slow_non_contiguous=True)
    s.dma("sp", w1_f[:, :, :], w1.rearrange("(kc p) n -> p kc n", p=128), writes=["w1_f"])
    s.dma("sp", b1_sb[:, :], b1.rearrange("(fc p) -> p fc", p=128), writes=["b1_sb"], allow_slow_non_contiguous=True)
    s.dma("sp", w2_f[:, :, :], w2.rearrange("(fc p) n -> p fc n", p=128), writes=["w2_f"])
    s.dma("sp", b2_sb[:, :], b2.rearrange("(p o) -> p o", o=1), writes=["b2_sb"])
    s.op("pool", lambda e: e.tensor_copy(out=w1_b[:, :, :], in_=w1_f[:, :, :]), reads=["w1_f"], writes=["w1_b"])
    s.op("pool", lambda e: e.tensor_copy(out=w2_b[:, :, :], in_=w2_f[:, :, :]), reads=["w2_f"], writes=["w2_b"])
    for kc in range(16):
        s.op("dve", lambda e: e.tensor_scalar(out=x_b[:, kc, :], in0=x_in[:, kc, :], scalar1=pos_sb[:, kc:kc + 1], scalar2=None,
                                              op0=ALU.add), reads=["x_in", "pos_sb"], writes=[("x_b", kc)])
    xkeys = [("x_b", kc) for kc in range(16)]
    k = 0
    for nch in range(2):
        ns = slice(nch * 512, (nch + 1) * 512)
        for fc in range(2):
            p = ph[k % 2]
            pk = ("ph", k % 2)
            k += 1
            for kc in range(16):
                s.op("pe", lambda e: e.matmul(p[:, :], lhsT=w1_b[:, kc, fc * 128:(fc + 1) * 128], rhs=x_b[:, kc, ns],
                                              start=(kc == 0), stop=(kc == 15)), reads=["w1_b"] + xkeys, writes=[pk])
            s.op("dve", lambda e: e.tensor_scalar(out=xh[:, :], in0=p[:, :], scalar1=b1_sb[:, fc:fc + 1], scalar2=None, op0=ALU.add),
                 reads=[pk, "b1_sb"], writes=["xh"])
            s.op("pool", lambda e: e.tensor_tensor(out=x2[:, :], in0=xh[:, :], in1=xh[:, :], op=ALU.mult), reads=["xh"], writes=["x2"])
            s.op("dve", lambda e: e.tensor_scalar(out=x2[:, :], in0=x2[:, :], scalar1=0.044715, scalar2=1.0, op0=ALU.mult, op1=ALU.add),
                 reads=["x2"], writes=["x2"])
            s.op("pool", lambda e: e.tensor_tensor(out=x2[:, :], in0=x2[:, :], in1=xh[:, :], op=ALU.mult), reads=["x2", "xh"], writes=["x2"])
            s.op("act", lambda e: e.activation(out=sg[:, :], in_=x2[:, :], func=AF.Sigmoid, scale=1.5957691216057308),
                 reads=["x2"], writes=["sg"])
            s.op("dve", lambda e: e.tensor_tensor(out=hid[:, fc, ns], in0=xh[:, :], in1=sg[:, :], op=ALU.mult),
                 reads=["xh", "sg"], writes=[("hid", fc, nch)])
        for fc in range(2):
            s.op("pe", lambda e: e.matmul(po[0:64, :], lhsT=w2_b[:, fc, :], rhs=hid[:, fc, ns], start=(fc == 0), stop=(fc == 1)),
                 reads=["w2_b", ("hid", fc, nch)], writes=["po"])
        s.op("dve", lambda e: e.tensor_scalar(out=o_sb[:, ns], in0=po[0:64, :], scalar1=b2_sb[:, 0:1], scalar2=None, op0=ALU.add),
             reads=["po", "b2_sb"], writes=[("o_sb", nch)])
    s.dma("sp", outT, o_sb[:, :], reads=[("o_sb", 0), ("o_sb", 1)], writes=["outT"])
    s.finish(["outT"])
    s.close()
    return nc


NEG = -1.0e30
MNEG = -240000.0


def nsa_tables(S, heads4):
    slopes = (2.0 ** (-8.0 * np.arange(1, 17) / 16)).astype(np.float64)
    sl4 = slopes[list(heads4)]
    kl = np.arange(128)[:, None].astype(np.float64)
    ql = np.arange(128)[None, :].astype(np.float64)
    t = {}
    tabW = np.zeros((5, 128, 2, 128), np.float32)
    for dlt in range(5):
        dist = ql - kl + 128 * dlt
        valid = (dist >= 0) & (dist < 512)
        for h in range(2):
            tabW[dlt, :, h, :] = np.where(valid, -sl4[h] * dist, NEG)
    t["tabW"] = tabW
    tabS = np.zeros((2, 128, 2, 128), np.float32)
    for h in range(2):
        tabS[1, :, h, :] = -sl4[h] * (ql - kl)
        tabS[0, :, h, :] = np.where(ql - kl >= 0, -sl4[h] * (ql - kl), NEG)
    t["tabS"] = tabS
    cst = np.zeros((128, 4, 128), np.float32)
    for h in range(4):
        cst[:, h, :] = (-sl4[h] * 128.0 * np.arange(128))[None, :]
    t["cst"] = cst
    tabC = np.zeros((128, 4, 128), np.float32)
    for h in range(4):
        tabC[:, h, :] = -sl4[h] * (ql - 16 * kl - 31)
    t["tabC"] = tabC
    maskC = np.zeros((17, 128, 128), np.float32)
    for dlt in range(17):
        maskC[dlt] = np.where(128 * dlt + ql - 16 * kl - 31 >= 0, 0.0, MNEG)
    t["maskC"] = maskC
    E = np.zeros((64, 128, 128), np.float32)
    for v in range(64):
        for k in range(128):
            E[v, 2 * v + k // 64, k] = 1.0
    t["E"] = E
    NQB = S // 128
    nblk = S // 64
    keep = np.ones((NQB, 128, nblk), np.float32)
    force = np.zeros((NQB, 128, nblk), np.float32)
    n = np.arange(nblk)[None, :]
    for i in range(NQB):
        cur = (2 * i + (np.arange(128) >= 64).astype(np.int64))[:, None]
        forced = (n == 0) | (n == cur) | (n == cur - 1)
        fut = n > cur
        keep[i] = np.where(forced | fut, 0.0, 1.0)
        force[i] = np.where(fut, -1.0e4, np.where(forced, 1.0e4, 0.0))
    t["keep"] = keep
    t["force"] = force
    t["identb"] = np.eye(128, dtype=np.float32)
    return t


def build_nsa(S):
    nc = bass.Bass("TRN2", target_bir_lowering=False)
    NQB = S // 128
    NCH = S // 128
    NBLK = S // 64
    NHALF = (NBLK + 127) // 128
    NCMP = S // 16
    NCC = (NCMP + 127) // 128
    CW = min(128, NCMP)

    def din(name, shape, dt=F32):
        return nc.dram_tensor(name, shape, dt, kind="ExternalInput").ap()

    qT4 = din("qT4", [NQB, 64, 4, 128], BF16)
    kcT = din("kcT", [64, NCC * 128], BF16)
    vc = din("vc", [NCC * 128, 64], BF16)
    ksT = din("ksT", [64, S], BF16)
    vs = din("vs", [S, 64], BF16)
    kwT = din("kwT", [64, S], BF16)
    vw = din("vw", [S, 64], BF16)
    tabW_d = din("tabW", [5, 128, 2, 128])
    tabS_d = din("tabS", [2, 128, 2, 128])
    cst_d = din("cst", [128, 4, 128])
    tabC_d = din("tabC", [128, 4, 128])
    maskC_d = din("maskC", [17, 128, 128])
    E_d = din("E", [64, 128, 128])
    keep_d = din("keep", [NQB, 128, NBLK])
    force_d = din("force", [NQB, 128, NBLK])
    ident_d = din("identb", [128, 128])
    o3 = nc.dram_tensor("o3", [NQB, 128, 3, 2, 65], F32, kind="ExternalOutput").ap()

    s = Sched(nc)
    stg = s.sb("stg", [128, 17 * 128], F32)
    tabW = s.sb("tabW_s", [128, 5, 256], F32)
    tabS = s.sb("tabS_s", [128, 2, 256], F32)
    cst = s.sb("cst_s", [128, 4, 128], F32)
    tabC = s.sb("tabC_s", [128, 512], F32)
    maskC = s.sb("maskC_s", [128, 17, 128], BF16)
    E = s.sb("E_s", [128, 64, 128], BF16)
    identb = s.sb("identb_s", [128, 128], BF16)
    kc_sb = s.sb("kc_sb", [64, NCC * 128], BF16)
    vc_sb = s.sb("vc_sb", [128, NCC, 65], BF16)
    ks_sb = s.sb("ks_sb", [64, S], BF16)
    vs_sb = s.sb("vs_sb", [128, NCH, 65], BF16)
    kw_sb = s.sb("kw_sb", [64, S], BF16)
    vw_sb = s.sb("vw_sb", [128, NCH, 65], BF16)
    s.dma("sp", tabW[:, :, :], tabW_d.rearrange("v p h q -> p v (h q)"), writes=["tabW"])
    s.dma("sp", tabS[:, :, :], tabS_d.rearrange("v p h q -> p v (h q)"), writes=["tabS"])
    s.dma("sp", cst[:, :, :], cst_d, writes=["cst"])
    s.dma("sp", tabC[:, :], tabC_d.rearrange("p h q -> p (h q)"), writes=["tabC"])
    s.dma("sp", stg[:, 0:17 * 128].rearrange("p (v q) -> p v q", v=17), maskC_d.rearrange("v p q -> p v q"), writes=["stg"])
    s.op("dve", lambda e: e.tensor_copy(out=maskC[:, :, :], in_=stg[:, 0:17 * 128].rearrange("p (v q) -> p v q", v=17)),
         reads=["stg"], writes=["maskC"])
    for ei in range(4):
        s.dma("sp", stg[:, 0:2048].rearrange("p (v q) -> p v q", v=16), E_d[ei * 16:(ei + 1) * 16].rearrange("v p q -> p v q"), writes=["stg"])
        s.op("dve", lambda e: e.tensor_copy(out=E[:, ei * 16:(ei + 1) * 16, :], in_=stg[:, 0:2048].rearrange("p (v q) -> p v q", v=16)),
             reads=["stg"], writes=["E"])
    s.dma("sp", stg[:, 0:128], ident_d, writes=["stg"])
    s.op("dve", lambda e: e.tensor_copy(out=identb[:, :], in_=stg[:, 0:128]), reads=["stg"], writes=["identb"])
    s.dma("sp", kc_sb[:, :], kcT, writes=["kc_sb"])
    s.dma("sp", ks_sb[:, :], ksT, writes=["ks_sb"])
    s.dma("sp", kw_sb[:, :], kwT, writes=["kw_sb"])
    for (vsb, vd, nm, n_) in ((vc_sb, vc, "vc_sb", NCC), (vs_sb, vs, "vs_sb", NCH), (vw_sb, vw, "vw_sb", NCH)):
        s.op("pool", lambda e: e.memset(vsb[:, :, 64:65], 1.0), writes=[nm])
        s.dma("sp", vsb[:, :, 0:64], vd.rearrange("(c p) d -> p c d", p=128), writes=[nm])

    NB = 4
    LA = 2
    q_sb = [s.sb("q_sb%d" % i, [64, 4, 128], BF16) for i in range(2)]
    keep_sb = [s.sb("keep_sb%d" % i, [128, NBLK], F32) for i in range(2)]
    force_sb = [s.sb("force_sb%d" % i, [128, NBLK], F32) for i in range(2)]
    tmp = [s.sb("tmp%d" % i, [128, 512], F32) for i in range(NB)]
    pc = s.sb("pc", [128, NCC, 512], BF16)
    pw = s.sb("pw", [128, 5, 256], BF16)
    psl = [s.sb("psl%d" % i, [128, 256], BF16) for i in range(NB)]
    oc_sb = s.sb("oc_sb", [128, 4, 65], F32)
    rl = s.sb("rl", [128, 4], F32)
    imp = s.sb("imp", [128, NCC * 128], F32)
    sc = s.sb("sc", [128, NBLK], F32)
    sc2 = s.sb("sc2", [128, NBLK], F32)
    t8a = s.sb("t8a", [128, 8], F32)
    t8b = s.sb("t8b", [128, 8], F32)
    selb = s.sb("selb", [128, NHALF * 128], BF16)
    selT = [s.sb("selT%d" % i, [128, NHALF, 128], BF16) for i in range(2)]
    o_sb = [s.sb("o_sb%d" % i, [128, 3, 2, 65], F32) for i in range(2)]
    ps_s = [s.ps("ps_s%d" % i, [128, 512], key=("ps_s", i)) for i in range(NB)]
    ps_os = [s.ps("ps_os%d" % i, [128, 512], key=("ps_os", i)) for i in range(2)]
    ps_o = s.ps("ps_o", [128, 4, 128], key="ps_o")
    ps_t = s.ps("ps_t", [128, 1024], BF16, key="ps_t")
    if NHALF * 128 > NBLK:
        s.op("dve", lambda e: e.memset(selb[:, :], 0.0), writes=["selb"])
    sidx = [0]
    okeys = []

    def nextbuf():
        k = sidx[0] % NB
        sidx[0] += 1
        return k

    def sel_steps(i):
        b = i % 2
        steps = []

        def ld():
            s.dma("sp", q_sb[b][:, :, :], qT4[i], writes=[("q_sb", b)])
            s.dma("sp", keep_sb[b][:, :], keep_d[i], writes=[("keep", b)])
            s.dma("sp", force_sb[b][:, :], force_d[i], writes=[("force", b)])
        steps.append(ld)
        mlist = [m for m in range(NCC) if 16 * CW * m + 31 <= 128 * i + 127]

        def cmp_chunk(m):
            k = nextbuf()
            dl = i - 16 * m
            if dl <= 16:
                s.op("pe", lambda e: e.matmul(ps_s[k][:, :], lhsT=identb[:, :],
                                              rhs=maskC[:, dl, :].unsqueeze(1).to_broadcast([128, 4, 128]),
                                              start=True, stop=False),
                     reads=["identb", "maskC"], writes=[("ps_s", k)])
            for h in range(4):
                s.op("pe", lambda e: e.matmul(ps_s[k][:, h * 128:(h + 1) * 128], lhsT=kc_sb[:, m * 128:(m + 1) * 128],
                                              rhs=q_sb[b][:, h, :], start=(dl > 16), stop=(dl > 16 or h == 3)),
                     reads=["kc_sb", ("q_sb", b)], writes=[("ps_s", k)])
            s.op("dve", lambda e: e.scalar_tensor_tensor(out=tmp[k][:, :], in0=ps_s[k][:, :], scalar=0.125,
                                                         in1=tabC[:, :], op0=ALU.mult, op1=ALU.add),
                 reads=[("ps_s", k), "tabC"], writes=[("tmp", k)])
            for h in range(4):
                s.op("act", lambda e: e.activation(out=pc[:, m, h * 128:(h + 1) * 128], in_=tmp[k][:, h * 128:(h + 1) * 128],
                                                   func=AF.Exp, bias=cst[:, h, dl:dl + 1], scale=1.0),
                     reads=[("tmp", k), "cst"], writes=[("pc", m, h)])
        for m in mlist:
            steps.append(lambda m=m: cmp_chunk(m))

        def cmp_pv():
            for h in range(4):
                for mi, m in enumerate(mlist):
                    s.op("pe", lambda e: e.matmul(ps_o[:, h, 0:65], lhsT=pc[:, m, h * 128:(h + 1) * 128], rhs=vc_sb[:, m, :],
                                                  start=(mi == 0), stop=(mi == len(mlist) - 1)),
                         reads=[("pc", m, h), "vc_sb"], writes=["ps_o"])
            s.op("act", lambda e: e.copy(out=oc_sb[:, :, :], in_=ps_o[:, :, 0:65]), reads=["ps_o"], writes=["oc_sb"])
            s.op("pool", lambda e: e.tensor_copy(out=o_sb[b][:, 0, :, :], in_=oc_sb[:, 0:2, :]), reads=["oc_sb"], writes=[("o_sb", b, 0)])
            s.op("dve", lambda e: e.tensor_scalar(out=rl[:, :], in0=oc_sb[:, :, 64], scalar1=1.0e-30, scalar2=None, op0=ALU.max),
                 reads=["oc_sb"], writes=["rl"])
            s.op("dve", lambda e: e.reciprocal(out=rl[:, :], in_=rl[:, :]), reads=["rl"], writes=["rl"])
            s.op("pool", lambda e: e.memset(imp[:, :], 0.0), writes=["imp"])
        steps.append(cmp_pv)

        def imp_head(h):
            for m in mlist:
                s.op("pe", lambda e: e.transpose(out=ps_t[:, m * 128:(m + 1) * 128], in_=pc[:, m, h * 128:(h + 1) * 128],
                                                 identity=identb[:, :]),
                     reads=[("pc", m, h), "identb"], writes=["ps_t"])
            w = len(mlist) * 128
            s.op("dve", lambda e: e.scalar_tensor_tensor(out=imp[:, 0:w], in0=ps_t[:, 0:w], scalar=rl[:, h:h + 1],
                                                         in1=imp[:, 0:w], op0=ALU.mult, op1=ALU.add),
                 reads=["ps_t", "rl", "imp"], writes=["imp"])
        for h in range(4):
            steps.append(lambda h=h: imp_head(h))
        A = imp[:, :].rearrange("p (n f) -> p n f", f=4)
        nb = NBLK

        def score1():
            s.op("dve", lambda e: e.tensor_tensor(out=sc[:, :], in0=A[:, 0:nb, 0], in1=A[:, 0:nb, 1], op=ALU.add), reads=["imp"], writes=["sc"])
            s.op("dve", lambda e: e.tensor_tensor(out=sc[:, :], in0=sc[:, :], in1=A[:, 0:nb, 2], op=ALU.add), reads=["imp", "sc"], writes=["sc"])
            s.op("dve", lambda e: e.scalar_tensor_tensor(out=sc[:, :], in0=sc[:, :], scalar=2.0, in1=A[:, 0:nb, 3],
                                                         op0=ALU.mult, op1=ALU.add), reads=["imp", "sc"], writes=["sc"])
            s.op("dve", lambda e: e.tensor_tensor(out=sc[:, 1:nb], in0=sc[:, 1:nb], in1=A[:, 0:nb - 1, 3], op=ALU.add),
                 reads=["imp", "sc"], writes=["sc"])

        def score2():
            s.op("dve", lambda e: e.tensor_tensor(out=sc[:, :], in0=sc[:, :], in1=keep_sb[b][:, :], op=ALU.mult),
                 reads=["sc", ("keep", b)], writes=["sc"])
            s.op("dve", lambda e: e.tensor_tensor(out=sc[:, :], in0=sc[:, :], in1=force_sb[b][:, :], op=ALU.add),
                 reads=["sc", ("force", b)], writes=["sc"])
            s.op("dve", lambda e: e.max(out=t8a[:, :], in_=sc[:, :]), reads=["sc"], writes=["t8a"])

        def score3():
            s.op("dve", lambda e: e.match_replace(out=sc2[:, :], in_to_replace=t8a[:, :], in_values=sc[:, :], imm_value=-3.0e4),
                 reads=["sc", "t8a"], writes=["sc2"])
            s.op("dve", lambda e: e.max(out=t8b[:, :], in_=sc2[:, :]), reads=["sc2"], writes=["t8b"])
            s.op("dve", lambda e: e.tensor_scalar(out=sc2[:, :], in0=sc[:, :], scalar1=t8b[:, 7:8], scalar2=-MNEG,
                                                  op0=ALU.is_ge, op1=ALU.mult), reads=["sc", "t8b"], writes=["sc2"])
            s.op("dve", lambda e: e.tensor_scalar(out=selb[:, 0:nb], in0=sc2[:, :], scalar1=MNEG, scalar2=None, op0=ALU.add),
                 reads=["sc2"], writes=["selb"])

        def score4():
            for hf in range(NHALF):
                s.op("pe", lambda e: e.transpose(out=ps_t[:, hf * 128:(hf + 1) * 128], in_=selb[:, hf * 128:(hf + 1) * 128],
                                                 identity=identb[:, :]), reads=["selb", "identb"], writes=["ps_t"])
            s.op("act", lambda e: e.copy(out=selT[b][:, :, :], in_=ps_t[:, 0:NHALF * 128].rearrange("p (a b) -> p a b", a=NHALF)),
                 reads=["ps_t"], writes=[("selT", b)])
        steps += [score1, score2, score3, score4]
        return steps

    def s_stage(i, j, k):
        b = i % 2
        hf, v = (2 * j) // 128, ((2 * j) % 128) // 2
        s.op("pe", lambda e: e.matmul(ps_s[k][:, 0:256], lhsT=E[:, v, :],
                                      rhs=selT[b][:, hf, :].unsqueeze(1).to_broadcast([128, 2, 128]),
                                      start=True, stop=False),
             reads=["E", ("selT", b)], writes=[("ps_s", k)])
        for h in range(2):
            s.op("pe", lambda e: e.matmul(ps_s[k][:, h * 128:(h + 1) * 128], lhsT=ks_sb[:, j * 128:(j + 1) * 128],
                                          rhs=q_sb[b][:, h, :], start=False, stop=(h == 1)),
                 reads=["ks_sb", ("q_sb", b)], writes=[("ps_s", k)])
        s.op("dve", lambda e: e.scalar_tensor_tensor(out=tmp[k][:, 0:256], in0=ps_s[k][:, 0:256], scalar=0.125,
                                                     in1=tabS[:, 0 if j == i else 1, :], op0=ALU.mult, op1=ALU.add),
             reads=[("ps_s", k), "tabS"], writes=[("tmp", k)])
        for h in range(2):
            s.op("act", lambda e: e.activation(out=psl[k][:, h * 128:(h + 1) * 128], in_=tmp[k][:, h * 128:(h + 1) * 128],
                                               func=AF.Exp, bias=cst[:, h, i - j:i - j + 1], scale=1.0),
                 reads=[("tmp", k), "cst"], writes=[("psl", k, h)])

    def pv_stage(i, j, k):
        for h in range(2):
            s.op("pe", lambda e: e.matmul(ps_os[h][:, 0:65], lhsT=psl[k][:, h * 128:(h + 1) * 128], rhs=vs_sb[:, j, :],
                                          start=(j == 0), stop=(j == i)),
                 reads=[("psl", k, h), "vs_sb"], writes=[("ps_os", h)])

    def window(i):
        b = i % 2
        wl = [j for j in range(i - 4, i + 1) if j >= 0]
        for wi, j in enumerate(wl):
            k = nextbuf()
            for h in range(2):
                s.op("pe", lambda e: e.matmul(ps_s[k][:, h * 128:(h + 1) * 128], lhsT=kw_sb[:, j * 128:(j + 1) * 128],
                                              rhs=q_sb[b][:, h, :], start=True, stop=True),
                     reads=["kw_sb", ("q_sb", b)], writes=[("ps_s", k)])
            s.op("dve", lambda e: e.scalar_tensor_tensor(out=tmp[k][:, 0:256], in0=ps_s[k][:, 0:256], scalar=0.125,
                                                         in1=tabW[:, i - j, :], op0=ALU.mult, op1=ALU.add),
                 reads=[("ps_s", k), "tabW"], writes=[("tmp", k)])
            s.op("act", lambda e: e.activation(out=pw[:, wi, :], in_=tmp[k][:, 0:256], func=AF.Exp),
                 reads=[("tmp", k)], writes=[("pw", wi)])
        for h in range(2):
            for wi, j in enumerate(wl):
                s.op("pe", lambda e: e.matmul(ps_o[:, h, 0:65], lhsT=pw[:, wi, h * 128:(h + 1) * 128], rhs=vw_sb[:, j, :],
                                              start=(wi == 0), stop=(wi == len(wl) - 1)),
                     reads=[("pw", wi), "vw_sb"], writes=["ps_o"])
        s.op("act", lambda e: e.copy(out=o_sb[b][:, 2, :, :], in_=ps_o[:, 0:2, 0:65]), reads=["ps_o"], writes=[("o_sb", b, 2)])

    for st in sel_steps(0):
        st()
    for i in range(NQB):
        b = i % 2
        nxt = sel_steps(i + 1) if i + 1 < NQB else []
        n = i + 1
        bufs = {}
        for step in range(n + LA):
            if step < n:
                bufs[step] = nextbuf()
                s_stage(i, step, bufs[step])
            if step - LA >= 0:
                pv_stage(i, step - LA, bufs[step - LA])
            if nxt and step % 2 == 1:
                nxt.pop(0)()
        for h in range(2):
            s.op("act", lambda e: e.copy(out=o_sb[b][:, 1, h, :], in_=ps_os[h][:, 0:65]), reads=[("ps_os", h)], writes=[("o_sb", b, 1)])
        window(i)
        while nxt:
            nxt.pop(0)()
        s.dma("sp", o3[i], o_sb[b][:, :, :, :], reads=[("o_sb", b, 0), ("o_sb", b, 1), ("o_sb", b, 2)], writes=[("o3", i)])
        okeys.append(("o3", i))
    s.finish(okeys)
    print("nsa ninst", s.ninst)
    s.close()
    return nc


import numpy as np

NCORES = 8
S = 16384
TPC = S // NCORES
_cache = {}


def _get(key, fn):
    if key not in _cache:
        _cache[key] = fn()
    return _cache[key]


def run_proj(h, w, out_bf16=True):
    N = w.shape[1]
    nc = _get(("proj", N, out_bf16), lambda: build_proj(TPC // 128, N, out_bf16))
    in_maps = [{"xT": np.ascontiguousarray(h[c * TPC:(c + 1) * TPC].T), "w": w} for c in range(NCORES)]
    res = run_bass_kernel_spmd(nc, in_maps, core_ids=list(range(NCORES)))
    return np.concatenate([np.asarray(r["y"]) for r in res.results], axis=0)


def run_atta(qkv):
    qTs, kTs, vs, meta = atta_host_prep(qkv)
    upc = [len(m) // NCORES for m in meta]
    units = []
    for g in range(3):
        for p in range(upc[g]):
            units.append(3 * g + (0 if p == 0 else (2 if p == 8 else 1)))
    nc = _get(("atta",), lambda: build_atta(units, 9))
    tab = atta_bias_table()
    in_maps = []
    for c in range(NCORES):
        tabs = np.zeros((9, 128, 2, 16, 128), np.float32)
        for g in range(3):
            for slot, p in ((0, 0), (1, 1), (2, 8)):
                t = tab[g].copy()
                if meta[g][c * upc[g] + p][1]:
                    t[:, 0] = -1.0e30
                tabs[3 * g + slot] = t
        sl = [slice(c * upc[g], (c + 1) * upc[g]) for g in range(3)]
        in_maps.append({"qT": np.concatenate([qTs[g][sl[g]] for g in range(3)]),
                        "kT": np.concatenate([kTs[g][sl[g]] for g in range(3)]),
                        "v": np.concatenate([vs[g][sl[g]] for g in range(3)]),
                        "tab": tabs})
    res = run_bass_kernel_spmd(nc, in_maps, core_ids=list(range(NCORES)))
    og = []
    for g in range(3):
        off = sum(upc[:g])
        og.append(np.concatenate([np.asarray(r["o"])[off:off + upc[g]] for r in res.results], axis=0))
    return atta_host_post(og, S)


def run_post(h, o_or_og, modeA, wout, lnp, wr, br, wgu, wd):
    nc = _get(("post", modeA), lambda: build_post(TPC, modeA))
    consts = post_consts()
    in_maps = []
    for c in range(NCORES):
        rows = slice(c * TPC, (c + 1) * TPC)
        m = {"h": np.ascontiguousarray(h[rows]), "wout": wout, "lnp": lnp, "wr": wr, "br": br, "wgu": wgu, "wd": wd}
        m.update(consts)
        if modeA:
            m["og"] = np.ascontiguousarray(o_or_og[:, rows])
        else:
            m["o"] = np.ascontiguousarray(o_or_og[rows])
        in_maps.append(m)
    res = run_bass_kernel_spmd(nc, in_maps, core_ids=list(range(NCORES)))
    return np.concatenate([np.asarray(r["hout"]) for r in res.results], axis=0)


def layer_a(h, l, inp):
    qkv = run_proj(h, np.ascontiguousarray(inp["a_w_in"][l]))
    og = run_atta(qkv)
    lnp = np.stack([inp["ln_mix_g"][l], inp["ln_mix_b"][l], inp["ln_ffn_g"][l], inp["ln_ffn_b"][l]])
    return run_post(h, og, 1, np.ascontiguousarray(inp["a_w_out"][l]), lnp,
                    np.ascontiguousarray(inp["moe_w_router"][l]), np.ascontiguousarray(inp["moe_b_router"][l]),
                    np.ascontiguousarray(inp["moe_w_gate_up"][l]), np.ascontiguousarray(inp["moe_w_down"][l]))


BF = ml_dtypes.bfloat16


def run_proj_b(h, w, bias):
    N = w.shape[1]
    nc = _get(("projb", N), lambda: build_proj(TPC // 128, N, True, True))
    in_maps = [{"xT": np.ascontiguousarray(h[c * TPC:(c + 1) * TPC].T), "w": w, "bias": bias} for c in range(NCORES)]
    res = run_bass_kernel_spmd(nc, in_maps, core_ids=list(range(NCORES)))
    return np.concatenate([np.asarray(r["y"]) for r in res.results], axis=0)


def run_cmp(kv, inp):
    kv6 = kv.reshape(S, 6, 4, 64)
    nc = _get(("cmp",), build_cmp)
    in_maps = []
    nblk = S // 16
    for j in range(2):
        for g in range(4):
            u = kv6[:, j, g, :]
            c = u.reshape(nblk, 16, 64)
            blocks = np.concatenate([c[:-1], c[1:]], axis=1)
            X = np.zeros((nblk, 2048), kv.dtype)
            X[:nblk - 1] = blocks.reshape(nblk - 1, 2048)
            in_maps.append({"xT": np.ascontiguousarray(X.T), "pos": np.ascontiguousarray(inp["b_cmp_pos"][j].reshape(2048)),
                            "w1": np.ascontiguousarray(inp["b_cmp_w1"][j]), "b1": np.ascontiguousarray(inp["b_cmp_b1"][j]),
                            "w2": np.ascontiguousarray(inp["b_cmp_w2"][j]), "b2": np.ascontiguousarray(inp["b_cmp_b2"][j])})
    res = run_bass_kernel_spmd(nc, in_maps, core_ids=list(range(NCORES)))
    outs = [np.asarray(r["outT"]) for r in res.results]
    kcT = [outs[g] for g in range(4)]
    vc = [np.ascontiguousarray(outs[4 + g].T) for g in range(4)]
    return kcT, vc


def run_nsa(proj, kv, kcT, vc):
    nc = _get(("nsa",), lambda: build_nsa(S))
    kv6 = kv.reshape(S, 6, 4, 64)
    q = proj[:, :1024].reshape(S // 128, 128, 16, 64)
    in_maps = []
    for c in range(NCORES):
        g, half = c // 2, c % 2
        own = [4 * g + 2 * half, 4 * g + 2 * half + 1]
        oth = [4 * g + 2 * (1 - half), 4 * g + 2 * (1 - half) + 1]
        heads4 = own + oth
        m = nsa_tables(S, heads4)
        m["qT4"] = np.ascontiguousarray(q[:, :, heads4, :].transpose(0, 3, 2, 1))
        m["kcT"] = kcT[g]
        m["vc"] = vc[g]
        m["ksT"] = np.ascontiguousarray(kv6[:, 2, g, :].T)
        m["vs"] = np.ascontiguousarray(kv6[:, 3, g, :])
        m["kwT"] = np.ascontiguousarray(kv6[:, 4, g, :].T)
        m["vw"] = np.ascontiguousarray(kv6[:, 5, g, :])
        in_maps.append(m)
    res = run_bass_kernel_spmd(nc, in_maps, core_ids=list(range(NCORES)))
    og = np.zeros((3, S, 16, 65), np.float32)
    for c in range(NCORES):
        g, half = c // 2, c % 2
        o3 = np.asarray(res.results[c]["o3"]).reshape(S, 3, 2, 65)
        for hh in range(2):
            og[:, :, 4 * g + 2 * half + hh, :] = o3[:, :, hh, :].transpose(1, 0, 2)
    return og.reshape(3, S, 1040)


def layer_b(h, i, inp, shared):
    l = 2 + i
    proj = run_proj_b(h, np.ascontiguousarray(inp["b_w_q"][i]), np.ascontiguousarray(inp["b_b_q"][i]))
    kv, kcT, vc = shared
    og = run_nsa(proj, kv, kcT, vc)
    lnp = np.stack([inp["ln_mix_g"][l], inp["ln_mix_b"][l], inp["ln_ffn_g"][l], inp["ln_ffn_b"][l]])
    gl = np.ascontiguousarray(proj[:, 1024:1072])
    nc = _get(("post", 0), lambda: build_post(TPC, 0))
    consts = post_consts()
    in_maps = []
    for c in range(NCORES):
        rows = slice(c * TPC, (c + 1) * TPC)
        m = {"h": np.ascontiguousarray(h[rows]), "wout": np.ascontiguousarray(inp["b_w_out"][i]), "lnp": lnp,
             "wr": np.ascontiguousarray(inp["moe_w_router"][l]), "br": np.ascontiguousarray(inp["moe_b_router"][l]),
             "wgu": np.ascontiguousarray(inp["moe_w_gate_up"][l]), "wd": np.ascontiguousarray(inp["moe_w_down"][l]),
             "og": np.ascontiguousarray(og[:, rows]), "gl": np.ascontiguousarray(gl[rows])}
        m.update(consts)
        in_maps.append(m)
    res = run_bass_kernel_spmd(nc, in_maps, core_ids=list(range(NCORES)))
    return np.concatenate([np.asarray(r["hout"]) for r in res.results], axis=0)


def forward(inp):
    h = np.ascontiguousarray(inp["x"][0])
    for l in range(2):
        h = layer_a(h, l, inp)
    kv = run_proj(h, np.ascontiguousarray(inp["b_w_kv"]))
    kcT, vc = run_cmp(kv, inp)
    for i in range(2):
        h = layer_b(h, i, inp, (kv, kcT, vc))
    return h[None].astype(np.float32)


def kernel(**inputs):
    inp = {k: np.asarray(v) for k, v in inputs.items()}
    return forward(inp)
```

```python
import ml_dtypes
import contextlib
import numpy as np
import concourse.bass as bass
import concourse.mybir as mybir
from concourse.bass_utils import run_bass_kernel_spmd

F32 = mybir.dt.float32
BF16 = mybir.dt.bfloat16
I32 = mybir.dt.int32
U32 = mybir.dt.uint32
AF = mybir.ActivationFunctionType
ALU = mybir.AluOpType
AX = mybir.AxisListType

DMA_RING = 6


class Sched:
    def __init__(self, nc, same_engine_sync=None):
        if same_engine_sync is None:
            same_engine_sync = True
        self.nc = nc
        self.es = contextlib.ExitStack()
        self.engs = {"pe": nc.tensor, "dve": nc.vector, "act": nc.scalar,
                     "pool": nc.gpsimd, "sp": nc.sync}
        self.sem = {}
        self.cnt = {}
        for e in self.engs:
            self.sem[e] = self.es.enter_context(nc.semaphore("s_" + e))
            self.cnt[e] = 0
        self.dsem = {}
        self.dcnt = {}
        self.dissued = {}
        for q in ("sp", "act", "pool"):
            self.dsem[q] = [self.es.enter_context(nc.semaphore("d_%s%d" % (q, i)))
                            for i in range(DMA_RING)]
            self.dcnt[q] = [0] * DMA_RING
            self.dissued[q] = 0
        self.waited = {}
        self.lastw = {}
        self.readers = {}
        self.same = same_engine_sync
        self.excl = set()
        self.ninst = 0

    def sb(self, name, shape, dt):
        return self.es.enter_context(self.nc.sbuf_tensor(name, shape, dt))

    def ps(self, name, shape, dt=F32, key=None):
        self.excl.add(key if key is not None else name)
        return self.es.enter_context(self.nc.psum_tensor(name, shape, dt))

    def _semobj(self, key):
        if isinstance(key, tuple):
            return self.dsem[key[0]][key[1]]
        return self.sem[key]

    def _wait(self, eng, key, val):
        if key == eng and (not self.same or eng == "pe"):
            return
        w = self.waited.get((eng, key), 0)
        if val > w:
            self.engs[eng].wait_ge(self._semobj(key), val)
            self.waited[(eng, key)] = val

    def _deps(self, eng, reads, writes):
        for t in reads:
            lw = self.lastw.get(t)
            if lw is not None:
                self._wait(eng, lw[0], lw[1])
        for t in writes:
            lw = self.lastw.get(t)
            if lw is not None:
                self._wait(eng, lw[0], lw[1])
            for r in self.readers.get(t, ()):
                self._wait(eng, r[0], r[1])

    def _record(self, stamp, reads, writes):
        for t in writes:
            self.lastw[t] = stamp
            self.readers[t] = []
        for t in reads:
            self.readers.setdefault(t, []).append(stamp)

    def op(self, eng, fn, reads=(), writes=()):
        ex = [t for t in reads if t in self.excl]
        if ex:
            reads = [t for t in reads if t not in self.excl]
            writes = list(writes) + ex
        self._deps(eng, reads, writes)
        ins = fn(self.engs[eng])
        self.cnt[eng] += 1
        ins.then_inc(self.sem[eng], 1)
        self._record((eng, self.cnt[eng]), reads, writes)
        self.ninst += 1
        return ins

    def dma(self, q, out, in_, reads=(), writes=(), **kw):
        slot = self.dissued[q] % DMA_RING
        self.dissued[q] += 1
        key = (q, slot)
        if self.dcnt[q][slot] > 0:
            self._wait(q, key, self.dcnt[q][slot])
        self._deps(q, reads, writes)
        ins = self.engs[q].dma_start(out=out, in_=in_, **kw)
        self.dcnt[q][slot] += 16
        ins.then_inc(self.dsem[q][slot], 16)
        self._record((key, self.dcnt[q][slot]), reads, writes)
        self.ninst += 1
        return ins

    def finish(self, out_tiles):
        for t in out_tiles:
            lw = self.lastw.get(t)
            if lw is not None:
                self._wait("sp", lw[0], lw[1])

    def close(self):
        self.es.close()


def build_proj(NT, N, out_bf16=True, has_bias=False):
    nc = bass.Bass("TRN2", target_bir_lowering=False)
    T = NT * 128
    xT = nc.dram_tensor("xT", [1024, T], F32, kind="ExternalInput").ap()
    w = nc.dram_tensor("w", [1024, N], F32, kind="ExternalInput").ap()
    odt = BF16 if out_bf16 else F32
    y = nc.dram_tensor("y", [T, N], odt, kind="ExternalOutput").ap()
    if has_bias:
        bias = nc.dram_tensor("bias", [N], F32, kind="ExternalInput").ap()
    s = Sched(nc)
    if has_bias:
        bias_bc = s.sb("bias_bc", [128, N], F32)
        s.dma("sp", bias_bc[:, :], bias.partition_broadcast(128), writes=["bias_bc"])
    x_f = s.sb("x_f", [128, 8, 512], F32)
    x_b = s.sb("x_b", [128, 8, T], BF16)
    xTv = xT.rearrange("(kc p) t -> p kc t", p=128)
    for c in range(T // 512):
        s.dma("sp", x_f[:, :, :], xTv[:, :, c * 512:(c + 1) * 512], writes=["x_f"])
        s.op("dve", lambda e: e.tensor_copy(out=x_b[:, :, c * 512:(c + 1) * 512], in_=x_f[:, :, :]),
             reads=["x_f"], writes=[("x_b", c)])
    CW = 512
    nch = (N + CW - 1) // CW
    w_f = [s.sb("w_f%d" % i, [128, 8, CW], F32) for i in range(2)]
    w_b = [s.sb("w_b%d" % i, [128, 8, CW], BF16) for i in range(2)]
    pt = [s.ps("pt%d" % i, [128, CW], key=("pt", i)) for i in range(4)]
    ot = [s.sb("ot%d" % i, [128, CW], odt) for i in range(4)]
    wv = w.rearrange("(kc p) n -> p kc n", p=128)
    k = 0

    def load_chunk(ch):
        n0 = ch * CW
        cw = min(CW, N - n0)
        b = ch % 2
        s.dma("sp", w_f[b][:, :, :cw], wv[:, :, n0:n0 + cw], writes=[("w_f", b)])
        s.op("pool", lambda e: e.tensor_copy(out=w_b[b][:, :, :cw], in_=w_f[b][:, :, :cw]),
             reads=[("w_f", b)], writes=[("w_b", b)])

    load_chunk(0)
    for ch in range(nch):
        n0 = ch * CW
        cw = min(CW, N - n0)
        b = ch % 2
        if ch + 1 < nch:
            load_chunk(ch + 1)
        for t in range(NT):
            pb = k % 4
            k += 1
            for kc in range(8):
                s.op("pe", lambda e: e.matmul(pt[pb][:, :cw], lhsT=x_b[:, kc, t * 128:(t + 1) * 128],
                                              rhs=w_b[b][:, kc, :cw], start=(kc == 0), stop=(kc == 7)),
                     reads=[("x_b", t // 4), ("w_b", b)], writes=[("pt", pb)])
            if has_bias:
                s.op("dve", lambda e: e.tensor_tensor(out=ot[pb][:, :cw], in0=pt[pb][:, :cw], in1=bias_bc[:, n0:n0 + cw], op=ALU.add),
                     reads=[("pt", pb), "bias_bc"], writes=[("ot", pb)])
            elif pb % 2 == 0:
                s.op("act", lambda e: e.copy(out=ot[pb][:, :cw], in_=pt[pb][:, :cw]),
                     reads=[("pt", pb)], writes=[("ot", pb)])
            else:
                s.op("dve", lambda e: e.tensor_copy(out=ot[pb][:, :cw], in_=pt[pb][:, :cw]),
                     reads=[("pt", pb)], writes=[("ot", pb)])
            s.dma("sp" if pb % 2 else "pool", y[t * 128:(t + 1) * 128, n0:n0 + cw], ot[pb][:, :cw],
                  reads=[("ot", pb)], writes=[("y", t, ch)])
    s.finish([("y", t, ch) for t in range(NT) for ch in range(nch)])
    print("ninst", s.ninst)
    s.close()
    return nc


A_DIL = (1, 4, 16)


def atta_bias_table():
    slopes = (2.0 ** (-8.0 * np.arange(1, 17) / 16)).astype(np.float32)
    k = np.arange(128)[:, None]
    q = np.arange(128)[None, :]
    tab = np.zeros((3, 128, 2, 16, 128), np.float32)
    for g, d in enumerate(A_DIL):
        for ch in range(2):
            dist = (q - k + (128 if ch == 0 else 0))
            valid = (dist >= 0) & (dist <= 128)
            for hh in range(16):
                b = -(slopes[hh] * (dist * d).astype(np.float32))
                tab[g, :, ch, hh, :] = np.where(valid, b, -1.0e30)
    return tab


def build_atta(units, nslot):
    nc = bass.Bass("TRN2", target_bir_lowering=False)
    U = len(units)
    qT = nc.dram_tensor("qT", [U, 64, 16, 128], BF16, kind="ExternalInput").ap()
    kT = nc.dram_tensor("kT", [U, 64, 16, 256], BF16, kind="ExternalInput").ap()
    v = nc.dram_tensor("v", [U, 2, 128, 16, 64], BF16, kind="ExternalInput").ap()
    tab = nc.dram_tensor("tab", [nslot, 128, 2, 16, 128], F32, kind="ExternalInput").ap()
    o = nc.dram_tensor("o", [U, 128, 16 * 65], F32, kind="ExternalOutput").ap()
    s = Sched(nc)
    tb = s.sb("tb", [128, 2, 16, 128], F32)
    q_sb = [s.sb("q_sb%d" % i, [64, 16, 128], BF16) for i in range(2)]
    k_sb = [s.sb("k_sb%d" % i, [64, 16, 256], BF16) for i in range(2)]
    v_sb = [s.sb("v_sb%d" % i, [128, 2, 16, 65], BF16) for i in range(2)]
    tmp = [s.sb("tmp%d" % i, [128, 2, 512], F32) for i in range(2)]
    pT = [s.sb("pT%d" % i, [128, 2, 512], BF16) for i in range(2)]
    o_sb = [s.sb("o_sb%d" % i, [128, 16, 65], F32) for i in range(2)]
    ps_s = [[s.ps("ps_s%d_%d" % (i, c), [128, 512], key=("ps_s", i, c)) for c in range(2)] for i in range(2)]
    ps_o = [s.ps("ps_o%d" % i, [128, 4, 128], key=("ps_o", i)) for i in range(2)]
    for i in range(2):
        s.op("pool", lambda e: e.memset(v_sb[i][:, :, :, 64:65], 1.0), writes=[("v_sb", i)])
    okeys = []
    state = {"cur_g": -1}
    items = [(u, hg) for u in range(U) for hg in range(4)]

    def S(n):
        u, hg = items[n]
        g = units[u]
        b = u % 2
        i = n % 2
        if hg == 0:
            if g != state["cur_g"]:
                s.dma("sp", tb[:, :, :, :], tab[g], writes=["tb"])
                state["cur_g"] = g
            s.dma("sp", q_sb[b][:, :, :], qT[u], writes=[("q_sb", b)])
            s.dma("sp", k_sb[b][:, :, :], kT[u], writes=[("k_sb", b)])
            for c in range(2):
                s.dma("sp", v_sb[b][:, c, :, 0:64], v[u, c], writes=[("v_sb", b)], reads=[("v_sb", b)])
        for c in range(2):
            for hh in range(4):
                hd = hg * 4 + hh
                s.op("pe", lambda e: e.matmul(ps_s[i][c][:, hh * 128:(hh + 1) * 128],
                                              lhsT=k_sb[b][:, hd, c * 128:(c + 1) * 128], rhs=q_sb[b][:, hd, :],
                                              start=True, stop=True),
                     reads=[("k_sb", b), ("q_sb", b)], writes=[("ps_s", i, c)])
            s.op("dve", lambda e: e.scalar_tensor_tensor(
                out=tmp[i][:, c, :], in0=ps_s[i][c][:, :], scalar=0.125,
                in1=tb[:, c, hg * 4:hg * 4 + 4, :].rearrange("p a b -> p (a b)"),
                op0=ALU.mult, op1=ALU.add),
                reads=[("ps_s", i, c), "tb"], writes=[("tmp", i, c)])
            s.op("act", lambda e: e.activation(out=pT[i][:, c, :], in_=tmp[i][:, c, :], func=AF.Exp),
                 reads=[("tmp", i, c)], writes=[("pT", i, c)])

    def PV(n):
        u, hg = items[n]
        b = u % 2
        i = n % 2
        for hh in range(4):
            hd = hg * 4 + hh
            for c in range(2):
                s.op("pe", lambda e: e.matmul(ps_o[i][:, hh, 0:65], lhsT=pT[i][:, c, hh * 128:(hh + 1) * 128],
                                              rhs=v_sb[b][:, c, hd, :], start=(c == 0), stop=(c == 1)),
                     reads=[("pT", i, c), ("v_sb", b)], writes=[("ps_o", i)])
        s.op("dve" if hg % 2 else "act", (lambda e: e.tensor_copy(out=o_sb[b][:, hg * 4:hg * 4 + 4, :], in_=ps_o[i][:, :, 0:65])) if hg % 2
             else (lambda e: e.copy(out=o_sb[b][:, hg * 4:hg * 4 + 4, :], in_=ps_o[i][:, :, 0:65])),
             reads=[("ps_o", i)], writes=[("o_sb", b)])
        if hg == 3:
            s.dma("sp", o[u], o_sb[b][:, :, :].rearrange("p a b -> p (a b)"), reads=[("o_sb", b)], writes=[("o", u)])
            okeys.append(("o", u))

    S(0)
    for n in range(len(items)):
        if n + 1 < len(items):
            S(n + 1)
        PV(n)
    s.finish(okeys)
    print("atta ninst", s.ninst)
    s.close()
    return nc


def atta_host_prep(qkv):
    S = qkv.shape[0]
    x = qkv.reshape(S, 3, 3, 16, 64)
    qTs, kTs, vs, meta = [], [], [], []
    for g, d in enumerate(A_DIL):
        L = S // d
        nb = L // 128
        def perm(a):
            return a.reshape(L, d, 16, 64).transpose(1, 0, 2, 3)
        q = perm(x[:, g, 0]).reshape(d, nb, 128, 16, 64)
        k = perm(x[:, g, 1])
        vv = perm(x[:, g, 2])
        z = np.zeros((d, 128, 16, 64), qkv.dtype)
        kp = np.concatenate([z, k], axis=1).reshape(d, nb + 1, 128, 16, 64)
        vp = np.concatenate([z, vv], axis=1).reshape(d, nb + 1, 128, 16, 64)
        qT = q.transpose(0, 1, 4, 3, 2).reshape(d * nb, 64, 16, 128)
        k2 = np.stack([kp[:, :-1], kp[:, 1:]], axis=2)
        kT = k2.transpose(0, 1, 5, 4, 2, 3).reshape(d * nb, 64, 16, 256)
        v2 = np.stack([vp[:, :-1], vp[:, 1:]], axis=2).reshape(d * nb, 2, 128, 16, 64)
        qTs.append(np.ascontiguousarray(qT))
        kTs.append(np.ascontiguousarray(kT))
        vs.append(np.ascontiguousarray(v2))
        meta.append([(g, (i % nb) == 0) for i in range(d * nb)])
    return qTs, kTs, vs, meta


def atta_host_post(o_groups, S):
    out = []
    for g, d in enumerate(A_DIL):
        L = S // d
        a = o_groups[g].reshape(d, L, 1040).transpose(1, 0, 2).reshape(S, 1040)
        out.append(a)
    return np.stack(out)


ALPHA = 8.0 ** 0.25
LN_EPS = 1e-5
BIG = 1.0e30


def build_post(T, modeA):
    nc = bass.Bass("TRN2", target_bir_lowering=False)
    D = 1024
    NTL = T // 128
    SG = min(1024, T)
    NSG = T // SG
    TPS = SG // 128

    def din(name, shape, dt=F32):
        return nc.dram_tensor(name, shape, dt, kind="ExternalInput").ap()

    h = din("h", [T, D])
    if modeA:
        og = din("og", [3, T, 16 * 65])
    else:
        og = din("og", [3, T, 16 * 65])
        gl = din("gl", [T, 48], BF16)
    wout = din("wout", [D, D])
    lnp = din("lnp", [4, D])
    wr = din("wr", [D, 20])
    br = din("br", [20])
    wgu = din("wgu", [16, D, 512])
    wd = din("wd", [16, 256, D])
    ident = din("ident", [128, 128])
    sel = din("sel", [128, 16 * 128])
    hout = nc.dram_tensor("hout", [T, D], F32, kind="ExternalOutput").ap()

    s = Sched(nc)
    ident_f = s.sb("ident_f", [128, 128], F32)
    ident_b = s.sb("ident_b", [128, 128], BF16)
    sel_f = s.sb("sel_f", [128, 16 * 128], F32)
    lnbc = s.sb("lnbc", [128, 4, D], F32)
    br_bc = s.sb("br_bc", [128, 20], F32)
    wr_f = s.sb("wr_f", [128, 8, 20], F32)
    wout_b = s.sb("wout_b", [128, 8, D], BF16)
    eps_t = s.sb("eps_t", [128, 1], F32)
    stage = [s.sb("stage%d" % i, [128, 4096], F32) for i in range(2)]
    wgu_b = [s.sb("wgu_b%d" % i, [128, 8, 512], BF16) for i in range(2)]
    wd_b = [s.sb("wd_b%d" % i, [128, 2, D], BF16) for i in range(2)]
    nstage = [0]

    def stage_load(src_ap, n_inner, dst_ap, dkey):
        b = nstage[0] % 2
        nstage[0] += 1
        sv = stage[b][:, :src_ap.shape[1] * src_ap.shape[2]].rearrange("p (a b) -> p a b", a=src_ap.shape[1])
        s.dma("sp", sv, src_ap, writes=[("stage", b)])
        s.op("pool", lambda e: e.tensor_copy(out=dst_ap, in_=sv), reads=[("stage", b)], writes=[dkey])

    s.dma("sp", ident_f[:, :], ident, writes=["ident_f"])
    s.op("dve", lambda e: e.tensor_copy(out=ident_b[:, :], in_=ident_f[:, :]), reads=["ident_f"], writes=["ident_b"])
    s.dma("sp", sel_f[:, :], sel, writes=["sel_f"])
    for i in range(4):
        s.dma("sp", lnbc[:, i, :], lnp[i].partition_broadcast(128), writes=[("lnbc", i)])
    s.dma("sp", br_bc[:, :], br.partition_broadcast(128), writes=["br_bc"])
    s.dma("sp", wr_f[:, :, :], wr.rearrange("(kc p) n -> p kc n", p=128), writes=["wr_f"])
    s.op("dve", lambda e: e.memset(eps_t[:, :], LN_EPS), writes=["eps_t"])
    woutv = wout.rearrange("(kc p) n -> p kc n", p=128)
    for i in range(2):
        stage_load(woutv[:, 4 * i:4 * i + 4, :], None, wout_b[:, 4 * i:4 * i + 4, :], ("wout_b", i))

    h_t = s.sb("h_t", [128, D], F32)
    if modeA:
        og_t = s.sb("og_t", [128, 3, 16 * 65], F32)
        acc = s.sb("acc", [128, 16, 65], F32)
        rl = s.sb("rl", [128, 16], F32)
    else:
        og_t = s.sb("og_t", [128, 3, 16 * 65], F32)
        gl_t = s.sb("gl_t", [128, 48], BF16)
        gate = s.sb("gate", [128, 3, 16], F32)
        rl3 = s.sb("rl3", [128, 3, 16], F32)

    o_b = s.sb("o_b", [128, D], BF16)
    oT = s.sb("oT", [128, 8, 128], BF16)
    r = s.sb("r", [128, D], F32)
    xn = s.sb("xn", [128, D], F32)
    tA = r[:, :].rearrange("p (a b) -> p a b", a=16)
    tB = xn[:, :].rearrange("p (a b) -> p a b", a=16)
    h1 = s.sb("h1", [128, D], F32)
    st = s.sb("st", [128, 2, 6], F32)
    mv = s.sb("mv", [128, 2], F32)
    rstd = s.sb("rstd", [128, 1], F32)
    yacc = s.sb("yacc", [128, TPS, D], F32)
    h1T_b = s.sb("h1T_b", [128, 8, SG], BF16)
    h1T_f = s.sb("h1T_f", [128, 8, 128], F32)
    combT = s.sb("combT", [128, SG], F32)
    L = s.sb("L", [128, 20], F32)
    sm = s.sb("sm", [128, 16], F32)
    goh = s.sb("goh", [128, 4], F32)
    em = s.sb("em", [128, 4, 4], F32)
    top8 = s.sb("top8", [128, 8], F32)
    c0 = s.sb("c0", [128, 16], F32)
    c1 = s.sb("c1", [128, 16], F32)
    comb = s.sb("comb", [128, 128], F32)
    s.op("dve", lambda e: e.memset(comb[:, :], 0.0), writes=["comb"])
    sg = [s.sb("sg%d" % i, [128, 512], F32) for i in range(2)]
    tt_ = [s.sb("tt%d" % i, [128, 512], F32) for i in range(2)]
    hid2 = [[s.sb("hid%d_%d" % (p, i), [128, 512], BF16) for i in range(2)] for p in range(2)]
    cb2 = [s.sb("cb2_%d" % p, [128, 512], F32) for p in range(2)]
    gu = [s.ps("gu%d" % i, [128, 512], key=("gu", i)) for i in range(4)]
    pcb = s.ps("pcb", [128, 512])
    py = [s.ps("py%d" % i, [128, 512], key=("py", i)) for i in range(2)]
    pT_b = s.ps("pT_b", [128, 1024], BF16)

    def layer_norm(src, src_key, gi, dst, dst_key):
        for i in range(2):
            s.op("dve", lambda e: e.bn_stats(out=st[:, i, :], in_=src[:, i * 512:(i + 1) * 512]),
                 reads=[src_key], writes=[("st", i)])
        s.op("dve", lambda e: e.bn_aggr(out=mv[:, :], in_=st[:, :, :]), reads=[("st", 0), ("st", 1)], writes=["mv"])
        s.op("act", lambda e: e.activation(out=rstd[:, :], in_=mv[:, 1:2], func=AF.Sqrt, bias=eps_t[:, :], scale=1.0),
             reads=["mv", "eps_t"], writes=["rstd"])
        s.op("dve", lambda e: e.reciprocal(out=rstd[:, :], in_=rstd[:, :]), reads=["rstd"], writes=["rstd"])
        s.op("dve", lambda e: e.tensor_scalar(out=xn[:, :], in0=src[:, :], scalar1=mv[:, 0:1], scalar2=rstd[:, 0:1],
                                              op0=ALU.subtract, op1=ALU.mult),
             reads=[src_key, "mv", "rstd"], writes=["xn"])
        s.op("pool", lambda e: e.tensor_tensor(out=xn[:, :], in0=xn[:, :], in1=lnbc[:, gi, :], op=ALU.mult),
             reads=["xn", ("lnbc", gi)], writes=["xn"])
        s.op("pool", lambda e: e.tensor_tensor(out=dst, in0=xn[:, :], in1=lnbc[:, gi + 1, :], op=ALU.add),
             reads=["xn", ("lnbc", gi + 1)], writes=[dst_key])

    STOP = 99

    class StopBuild(Exception):
        pass

    def ck(n):
        if STOP == n:
            raise StopBuild()

    out_keys = []
    try:
      ck(1)
      for sgi in range(NSG):
          for ti in range(TPS):
              gt = sgi * TPS + ti
              rows = slice(gt * 128, (gt + 1) * 128)
              s.dma("sp", h_t[:, :], h[rows, :], writes=["h_t"])
              if modeA:
                  s.dma("sp", og_t[:, :, :], og[:, rows, :].rearrange("g t c -> t g c"), writes=["og_t"])
                  accf = acc[:, :, :].rearrange("p a b -> p (a b)")
                  s.op("dve", lambda e: e.tensor_tensor(out=accf, in0=og_t[:, 0, :], in1=og_t[:, 1, :], op=ALU.add),
                       reads=["og_t"], writes=["acc"])
                  s.op("dve", lambda e: e.tensor_tensor(out=accf, in0=accf, in1=og_t[:, 2, :], op=ALU.add),
                       reads=["og_t", "acc"], writes=["acc"])
                  s.op("dve", lambda e: e.reciprocal(out=rl[:, :], in_=acc[:, :, 64]), reads=["acc"], writes=["rl"])
                  s.op("dve", lambda e: e.tensor_tensor(out=o_b[:, :].rearrange("p (a b) -> p a b", a=16),
                                                        in0=acc[:, :, 0:64],
                                                        in1=rl[:, :].unsqueeze(2).to_broadcast([128, 16, 64]),
                                                        op=ALU.mult),
                       reads=["acc", "rl"], writes=["o_b"])
              else:
                  s.dma("sp", og_t[:, :, :], og[:, rows, :].rearrange("g t c -> t g c"), writes=["og_t"])
                  s.dma("sp", gl_t[:, :], gl[rows, :], writes=["gl_t"])
                  s.op("act", lambda e: e.activation(out=gate[:, :, :].rearrange("p a b -> p (a b)"), in_=gl_t[:, :], func=AF.Sigmoid),
                       reads=["gl_t"], writes=["gate"])
                  ogv = og_t[:, :, :].rearrange("p g (h c) -> p g h c", c=65)
                  s.op("dve", lambda e: e.tensor_scalar(out=rl3[:, :, :], in0=ogv[:, :, :, 64], scalar1=1.0e-30, scalar2=None, op0=ALU.max),
                       reads=["og_t"], writes=["rl3"])
                  s.op("dve", lambda e: e.reciprocal(out=rl3[:, :, :], in_=rl3[:, :, :]), reads=["rl3"], writes=["rl3"])
                  s.op("dve", lambda e: e.tensor_tensor(out=rl3[:, :, :], in0=rl3[:, :, :], in1=gate[:, :, :], op=ALU.mult),
                       reads=["rl3", "gate"], writes=["rl3"])
                  s.op("dve", lambda e: e.tensor_tensor(out=tA[:, :, :], in0=ogv[:, 0, :, 0:64],
                                                        in1=rl3[:, 0, :].unsqueeze(2).to_broadcast([128, 16, 64]), op=ALU.mult),
                       reads=["og_t", "rl3"], writes=["r"])
                  s.op("dve", lambda e: e.tensor_tensor(out=tB[:, :, :], in0=ogv[:, 1, :, 0:64],
                                                        in1=rl3[:, 1, :].unsqueeze(2).to_broadcast([128, 16, 64]), op=ALU.mult),
                       reads=["og_t", "rl3"], writes=["xn"])
                  s.op("dve", lambda e: e.tensor_tensor(out=tA[:, :, :], in0=tA[:, :, :], in1=tB[:, :, :], op=ALU.add),
                       reads=["r", "xn"], writes=["r"])
                  s.op("dve", lambda e: e.tensor_tensor(out=tB[:, :, :], in0=ogv[:, 2, :, 0:64],
                                                        in1=rl3[:, 2, :].unsqueeze(2).to_broadcast([128, 16, 64]), op=ALU.mult),
                       reads=["og_t", "rl3"], writes=["xn"])
                  s.op("dve", lambda e: e.tensor_tensor(out=o_b[:, :].rearrange("p (a b) -> p a b", a=16), in0=tA[:, :, :], in1=tB[:, :, :], op=ALU.add),
                       reads=["r", "xn"], writes=["o_b"])
              for kc in range(8):
                  s.op("pe", lambda e: e.transpose(out=pT_b[:, kc * 128:(kc + 1) * 128],
                                                   in_=o_b[:, kc * 128:(kc + 1) * 128], identity=ident_b[:, :]),
                       reads=["o_b", "ident_b"], writes=["pT_b"])
              s.op("act", lambda e: e.copy(out=oT[:, :, :].rearrange("p a b -> p (a b)"), in_=pT_b[:, :]),
                   reads=["pT_b"], writes=["oT"])
              for hh in range(2):
                  for kc in range(8):
                      s.op("pe", lambda e: e.matmul(gu[hh][:, :], lhsT=oT[:, kc, :],
                                                    rhs=wout_b[:, kc, hh * 512:(hh + 1) * 512],
                                                    start=(kc == 0), stop=(kc == 7)),
                           reads=["oT", ("wout_b", kc // 4)], writes=[("gu", hh)])
                  s.op("dve", lambda e: e.scalar_tensor_tensor(out=r[:, hh * 512:(hh + 1) * 512],
                                                               in0=h_t[:, hh * 512:(hh + 1) * 512], scalar=ALPHA,
                                                               in1=gu[hh][:, :], op0=ALU.mult, op1=ALU.add),
                       reads=["h_t", ("gu", hh)], writes=["r"])
              ck(2)
              layer_norm(r, "r", 0, h1[:, :], "h1")
              ck(3)
              s.op("act", lambda e: e.mul(out=yacc[:, ti, :], in_=h1[:, :], mul=ALPHA),
                   reads=["h1"], writes=[("yacc", ti)])
              ck(31)
              for q in range(2):
                  for j in range(4):
                      kc = q * 4 + j
                      s.op("pe", lambda e: e.transpose(out=gu[2 + q][:, j * 128:(j + 1) * 128],
                                                       in_=h1[:, kc * 128:(kc + 1) * 128], identity=ident_f[:, :]),
                           reads=["h1", "ident_f"], writes=[("gu", 2 + q)])
                  ck(32)
                  s.op("act", lambda e: e.copy(out=h1T_f[:, q * 4:q * 4 + 4, :],
                                               in_=gu[2 + q][:, :].rearrange("p (a b) -> p a b", a=4)),
                       reads=[("gu", 2 + q)], writes=[("h1T_f", q)])
                  ck(33)
                  s.op("dve", lambda e: e.tensor_copy(out=h1T_b[:, q * 4:q * 4 + 4, ti * 128:(ti + 1) * 128],
                                                      in_=gu[2 + q][:, :].rearrange("p (a b) -> p a b", a=4)),
                       reads=[("gu", 2 + q)], writes=[("h1T_b", ti)])
              ck(4)
              for kc in range(8):
                  s.op("pe", lambda e: e.matmul(pcb[:, 0:20], lhsT=h1T_f[:, kc, :], rhs=wr_f[:, kc, :],
                                                start=(kc == 0), stop=(kc == 7)),
                       reads=[("h1T_f", kc // 4), "wr_f"], writes=["pcb"])
              s.op("dve", lambda e: e.tensor_tensor(out=L[:, :], in0=pcb[:, 0:20], in1=br_bc[:, :], op=ALU.add),
                   reads=["pcb", "br_bc"], writes=["L"])
              ck(5)
              V = lambda eng, fn, rd, wr_: s.op(eng, fn, reads=rd, writes=wr_)
              V("dve", lambda e: e.reduce_max(out=sm[:, 0:1], in_=L[:, 0:4], axis=AX.X), ["L"], ["sm"])
              V("dve", lambda e: e.tensor_scalar(out=sm[:, 1:2], in0=sm[:, 0:1], scalar1=-1.0, scalar2=None, op0=ALU.mult),
                ["sm"], ["sm"])
              V("act", lambda e: e.activation(out=goh[:, :], in_=L[:, 0:4], func=AF.Exp, bias=sm[:, 1:2], scale=1.0),
                ["L", "sm"], ["goh"])
              V("dve", lambda e: e.reduce_sum(out=sm[:, 2:3], in_=goh[:, :], axis=AX.X), ["goh"], ["sm"])
              V("dve", lambda e: e.reciprocal(out=sm[:, 3:4], in_=sm[:, 2:3]), ["sm"], ["sm"])
              V("dve", lambda e: e.tensor_scalar(out=goh[:, :], in0=L[:, 0:4], scalar1=sm[:, 0:1], scalar2=None,
                                                 op0=ALU.is_equal), ["L", "sm", "goh"], ["goh"])
              V("dve", lambda e: e.tensor_scalar(out=goh[:, :], in0=goh[:, :], scalar1=BIG, scalar2=BIG,
                                                 op0=ALU.mult, op1=ALU.subtract), ["goh"], ["goh"])
              V("dve", lambda e: e.tensor_tensor(out=em[:, :, :], in0=L[:, 4:20].rearrange("p (a b) -> p a b", a=4),
                                                 in1=goh[:, :].unsqueeze(2).to_broadcast([128, 4, 4]), op=ALU.add),
                ["L", "goh"], ["em"])
              emf = em[:, :, :].rearrange("p a b -> p (a b)")
              V("dve", lambda e: e.max(out=top8[:, :], in_=emf), ["em"], ["top8"])
              V("dve", lambda e: e.tensor_tensor(out=sm[:, 4:5], in0=top8[:, 1:2], in1=top8[:, 0:1], op=ALU.subtract),
                ["top8", "sm"], ["sm"])
              V("act", lambda e: e.activation(out=sm[:, 5:6], in_=sm[:, 4:5], func=AF.Exp), ["sm"], ["sm"])
              V("dve", lambda e: e.tensor_scalar(out=sm[:, 6:7], in0=sm[:, 5:6], scalar1=1.0, scalar2=None, op0=ALU.add),
                ["sm"], ["sm"])
              V("dve", lambda e: e.reciprocal(out=sm[:, 6:7], in_=sm[:, 6:7]), ["sm"], ["sm"])
              V("dve", lambda e: e.tensor_tensor(out=sm[:, 7:8], in0=sm[:, 6:7], in1=sm[:, 3:4], op=ALU.mult), ["sm"], ["sm"])
              V("dve", lambda e: e.tensor_tensor(out=sm[:, 8:9], in0=sm[:, 7:8], in1=sm[:, 5:6], op=ALU.mult), ["sm"], ["sm"])
              V("dve", lambda e: e.tensor_scalar(out=c0[:, :], in0=emf, scalar1=top8[:, 0:1], scalar2=sm[:, 7:8],
                                                 op0=ALU.is_equal, op1=ALU.mult), ["em", "top8", "sm"], ["c0"])
              V("dve", lambda e: e.tensor_scalar(out=c1[:, :], in0=emf, scalar1=top8[:, 1:2], scalar2=sm[:, 8:9],
                                                 op0=ALU.is_equal, op1=ALU.mult), ["em", "top8", "sm"], ["c1"])
              V("dve", lambda e: e.tensor_tensor(out=comb[:, 0:16], in0=c0[:, :], in1=c1[:, :], op=ALU.add),
                ["c0", "c1"], ["comb"])
              ck(6)
              V("pe", lambda e: e.transpose(out=pcb[:, 128:256], in_=comb[:, :], identity=ident_f[:, :]),
                ["comb", "ident_f"], ["pcb"])
              V("act", lambda e: e.copy(out=combT[:, ti * 128:(ti + 1) * 128], in_=pcb[:, 128:256]),
                ["pcb"], [("combT", ti)])

          ck(7)
          NSUB = SG // 512
          units = [(ex, sub) for ex in range(16) for sub in range(NSUB)]

          def load_w(ex):
              wb = ex % 2
              stage_load(wgu[ex].rearrange("(kc p) n -> p kc n", p=128), None, wgu_b[wb][:, :, :], ("wgu_b", wb))
              stage_load(wd[ex].rearrange("(fc p) n -> p fc n", p=128), None, wd_b[wb][:, :, :], ("wd_b", wb))

          def GU(n, fc):
              ex, sub = units[n]
              wb = ex % 2
              tok = slice(sub * 512, (sub + 1) * 512)
              tkeys = [("h1T_b", sub * 4 + i) for i in range(4)]
              for j in (fc, 2 + fc):
                  for kc in range(8):
                      s.op("pe", lambda e: e.matmul(gu[j][:, :], lhsT=wgu_b[wb][:, kc, j * 128:(j + 1) * 128],
                                                    rhs=h1T_b[:, kc, tok], start=(kc == 0), stop=(kc == 7)),
                           reads=[("wgu_b", wb)] + tkeys, writes=[("gu", j)])

          def CB(n):
              ex, sub = units[n]
              tok = slice(sub * 512, (sub + 1) * 512)
              s.op("pe", lambda e: e.matmul(pcb[:, :], lhsT=sel_f[:, ex * 128:(ex + 1) * 128], rhs=combT[:, tok],
                                            start=True, stop=True),
                   reads=["sel_f"] + [("combT", sub * 4 + i) for i in range(4)], writes=["pcb"])
              s.op("act", lambda e: e.copy(out=cb2[n % 2][:, :], in_=pcb[:, :]), reads=["pcb"], writes=[("cb", n % 2)])

          def EW(n, fc):
              p = n % 2
              s.op("act", lambda e: e.activation(out=sg[fc][:, :], in_=gu[fc][:, :], func=AF.Silu),
                   reads=[("gu", fc)], writes=[("sg", fc)])
              s.op("dve", lambda e: e.tensor_tensor(out=tt_[fc][:, :], in0=gu[2 + fc][:, :], in1=cb2[p][:, :], op=ALU.mult),
                   reads=[("gu", 2 + fc), ("cb", p)], writes=[("tt", fc)])
              s.op("pool", lambda e: e.tensor_tensor(out=hid2[p][fc][:, :], in0=sg[fc][:, :], in1=tt_[fc][:, :], op=ALU.mult),
                   reads=[("sg", fc), ("tt", fc)], writes=[("hid", p, fc)])

          def DOWN(n):
              ex, sub = units[n]
              wb = ex % 2
              p = n % 2
              for t4 in range(4):
                  ti = sub * 4 + t4
                  for hh in range(2):
                      for fc in range(2):
                          s.op("pe", lambda e: e.matmul(py[hh][:, :], lhsT=hid2[p][fc][:, t4 * 128:(t4 + 1) * 128],
                                                        rhs=wd_b[wb][:, fc, hh * 512:(hh + 1) * 512],
                                                        start=(fc == 0), stop=(fc == 1)),
                               reads=[("hid", p, fc), ("wd_b", wb)], writes=[("py", hh)])
                      s.op("dve", lambda e: e.tensor_tensor(out=yacc[:, ti, hh * 512:(hh + 1) * 512],
                                                            in0=yacc[:, ti, hh * 512:(hh + 1) * 512],
                                                            in1=py[hh][:, :], op=ALU.add),
                           reads=[("yacc", ti), ("py", hh)], writes=[("yacc", ti)])

          load_w(0)
          GU(0, 0); CB(0); EW(0, 0); GU(0, 1); EW(0, 1)
          for n in range(len(units)):
              ex, sub = units[n]
              if sub == 0 and ex + 1 < 16:
                  load_w(ex + 1)
              if n + 1 < len(units):
                  GU(n + 1, 0); CB(n + 1); EW(n + 1, 0)
              DOWN(n)
              if n + 1 < len(units):
                  GU(n + 1, 1); EW(n + 1, 1)
          ck(8)
          for ti in range(TPS):
              gt = sgi * TPS + ti
              layer_norm(yacc[:, ti, :], ("yacc", ti), 2, h1[:, :], "h1")
              s.dma("sp", hout[gt * 128:(gt + 1) * 128, :], h1[:, :], reads=["h1"], writes=[("hout", gt)])
              out_keys.append(("hout", gt))
    except StopBuild:
        pass
    s.finish(out_keys)
    print("post ninst", s.ninst)
    s.close()
    return nc


def post_consts():
    ident = np.eye(128, dtype=np.float32)
    sel = np.zeros((128, 16 * 128), np.float32)
    for e in range(16):
        sel[e, e * 128:(e + 1) * 128] = 1.0
    return {"ident": ident, "sel": sel}


def build_cmp():
    nc = bass.Bass("TRN2", target_bir_lowering=False)

    def din(name, shape, dt=F32):
        return nc.dram_tensor(name, shape, dt, kind="ExternalInput").ap()
    xT = din("xT", [2048, 1024], BF16)
    pos = din("pos", [2048])
    w1 = din("w1", [2048, 256])
    b1 = din("b1", [256])
    w2 = din("w2", [256, 64])
    b2 = din("b2", [64])
    outT = nc.dram_tensor("outT", [64, 1024], BF16, kind="ExternalOutput").ap()
    s = Sched(nc)
    x_in = s.sb("x_in", [128, 16, 1024], BF16)
    x_b = s.sb("x_b", [128, 16, 1024], BF16)
    pos_sb = s.sb("pos_sb", [128, 16], F32)
    w1_f = s.sb("w1_f", [128, 16, 256], F32)
    w1_b = s.sb("w1_b", [128, 16, 256], BF16)
    b1_sb = s.sb("b1_sb", [128, 2], F32)
    w2_f = s.sb("w2_f", [128, 2, 64], F32)
    w2_b = s.sb("w2_b", [128, 2, 64], BF16)
    b2_sb = s.sb("b2_sb", [64, 1], F32)
    xh = s.sb("xh", [128, 512], F32)
    x2 = s.sb("x2", [128, 512], F32)
    sg = s.sb("sg", [128, 512], F32)
    hid = s.sb("hid", [128, 2, 1024], BF16)
    o_sb = s.sb("o_sb", [64, 1024], BF16)
    ph = [s.ps("ph%d" % i, [128, 512], key=("ph", i)) for i in range(2)]
    po = s.ps("po", [128, 512], key="po")
    s.dma("sp", x_in[:, :, :], xT.rearrange("(kc p) n -> p kc n", p=128), writes=["x_in"])
    s.dma("sp", pos_sb[:, :], pos.rearrange("(kc p) -> p kc", p=128), writes=["pos_sb"], allow_slow_non_contiguous=True)
    s.dma("sp", w1_f[:, :, :], w1.rearrange("(kc p) n -> p kc n", p=128), writes=["w1_f"])
    s.dma("sp", b1_sb[:, :], b1.rearrange("(fc p) -> p fc", p=128), writes=["b1_sb"], allow_slow_non_contiguous=True)
    s.dma("sp", w2_f[:, :, :], w2.rearrange("(fc p) n -> p fc n", p=128), writes=["w2_f"])
    s.dma("sp", b2_sb[:, :], b2.rearrange("(p o) -> p o", o=1), writes=["b2_sb"])
    s.op("pool", lambda e: e.tensor_copy(out=w1_b[:, :, :], in_=w1_f[:, :, :]), reads=["w1_f"], writes=["w1_b"])
    s.op("pool", lambda e: e.tensor_copy(out=w2_b[:, :, :], in_=w2_f[:, :, :]), reads=["w2_f"], writes=["w2_b"])
    for kc in range(16):
        s.op("dve", lambda e: e.tensor_scalar(out=x_b[:, kc, :], in0=x_in[:, kc, :], scalar1=pos_sb[:, kc:kc + 1], scalar2=None,
                                              op0=ALU.add), reads=["x_in", "pos_sb"], writes=[("x_b", kc)])
    xkeys = [("x_b", kc) for kc in range(16)]
    k = 0
    for nch in range(2):
        ns = slice(nch * 512, (nch + 1) * 512)
        for fc in range(2):
            p = ph[k % 2]
            pk = ("ph", k % 2)
            k += 1
            for kc in range(16):
                s.op("pe", lambda e: e.matmul(p[:, :], lhsT=w1_b[:, kc, fc * 128:(fc + 1) * 128], rhs=x_b[:, kc, ns],
                                              start=(kc == 0), stop=(kc == 15)), reads=["w1_b"] + xkeys, writes=[pk])
            s.op("dve", lambda e: e.tensor_scalar(out=xh[:, :], in0=p[:, :], scalar1=b1_sb[:, fc:fc + 1], scalar2=None, op0=ALU.add),
                 reads=[pk, "b1_sb"], writes=["xh"])
            s.op("pool", lambda e: e.tensor_tensor(out=x2[:, :], in0=xh[:, :], in1=xh[:, :], op=ALU.mult), reads=["xh"], writes=["x2"])
            s.op("dve", lambda e: e.tensor_scalar(out=x2[:, :], in0=x2[:, :], scalar1=0.044715, scalar2=1.0, op0=ALU.mult, op1=ALU.add),
                 reads=["x2"], writes=["x2"])
            s.op("pool", lambda e: e.tensor_tensor(out=x2[:, :], in0=x2[:, :], in1=xh[:, :], op=ALU.mult), reads=["x2", "xh"], writes=["x2"])
            s.op("act", lambda e: e.activation(out=sg[:, :], in_=x2[:, :], func=AF.Sigmoid, scale=1.5957691216057308),
                 reads=["x2"], writes=["sg"])
            s.op("dve", lambda e: e.tensor_tensor(out=hid[:, fc, ns], in0=xh[:, :], in1=sg[:, :], op=ALU.mult),
                 reads=["xh", "sg"], writes=[("hid", fc, nch)])
        for fc in range(2):
            s.op("pe", lambda e: e.matmul(po[0:64, :], lhsT=w2_b[:, fc, :], rhs=hid[:, fc, ns], start=(fc == 0), stop=(fc == 1)),
                 reads=["w2_b", ("hid", fc, nch)], writes=["po"])
        s.op("dve", lambda e: e.tensor_scalar(out=o_sb[:, ns], in0=po[0:64, :], scalar1=b2_sb[:, 0:1], scalar2=None, op0=ALU.add),
             reads=["po", "b2_sb"], writes=[("o_sb", nch)])
    s.dma("sp", outT, o_sb[:, :], reads=[("o_sb", 0), ("o_sb", 1)], writes=["outT"])
    s.finish(["outT"])
    s.close()
    return nc


NEG = -1.0e30
MNEG = -240000.0


def nsa_tables(S, heads4):
    slopes = (2.0 ** (-8.0 * np.arange(1, 17) / 16)).astype(np.float64)
    sl4 = slopes[list(heads4)]
    kl = np.arange(128)[:, None].astype(np.float64)
    ql = np.arange(128)[None, :].astype(np.float64)
    t = {}
    tabW = np.zeros((5, 128, 2, 128), np.float32)
    for dlt in range(5):
        dist = ql - kl + 128 * dlt
        valid = (dist >= 0) & (dist < 512)
        for h in range(2):
            tabW[dlt, :, h, :] = np.where(valid, -sl4[h] * dist, NEG)
    t["tabW"] = tabW
    tabS = np.zeros((2, 128, 2, 128), np.float32)
    for h in range(2):
        tabS[1, :, h, :] = -sl4[h] * (ql - kl)
        tabS[0, :, h, :] = np.where(ql - kl >= 0, -sl4[h] * (ql - kl), NEG)
    t["tabS"] = tabS
    cst = np.zeros((128, 4, 128), np.float32)
    for h in range(4):
        cst[:, h, :] = (-sl4[h] * 128.0 * np.arange(128))[None, :]
    t["cst"] = cst
    tabC = np.zeros((128, 4, 128), np.float32)
    for h in range(4):
        tabC[:, h, :] = -sl4[h] * (ql - 16 * kl - 31)
    t["tabC"] = tabC
    maskC = np.zeros((17, 128, 128), np.float32)
    for dlt in range(17):
        maskC[dlt] = np.where(128 * dlt + ql - 16 * kl - 31 >= 0, 0.0, MNEG)
    t["maskC"] = maskC
    NQB = S // 128
    nblk = S // 64
    keep = np.ones((NQB, 128, nblk), np.float32)
    force = np.zeros((NQB, 128, nblk), np.float32)
    n = np.arange(nblk)[None, :]
    for i in range(NQB):
        cur = (2 * i + (np.arange(128) >= 64).astype(np.int64))[:, None]
        forced = (n == 0) | (n == cur) | (n == cur - 1)
        fut = n > cur
        keep[i] = np.where(forced | fut, 0.0, 1.0)
        force[i] = np.where(fut, -1.0e4, np.where(forced, 1.0e4, 0.0))
    t["keep"] = keep
    t["force"] = force
    t["identb"] = np.eye(128, dtype=np.float32)
    oh = np.zeros((64, S), np.float32)
    for j in range(S // 128):
        for k in range(128):
            oh[2 * (j % 32) + k // 64, j * 128 + k] = 1.0
    t["oh"] = oh.astype(ml_dtypes.bfloat16)
    return t


def build_nsa(S):
    nc = bass.Bass("TRN2", target_bir_lowering=False)
    NQB = S // 128
    NCH = S // 128
    NBLK = S // 64
    NHALF = (NBLK + 127) // 128
    NCMP = S // 16
    NCC = (NCMP + 127) // 128
    CW = min(128, NCMP)

    def din(name, shape, dt=F32):
        return nc.dram_tensor(name, shape, dt, kind="ExternalInput").ap()

    qT4 = din("qT4", [NQB, 64, 4, 128], BF16)
    kcT = din("kcT", [64, NCC * 128], BF16)
    vc = din("vc", [NCC * 128, 64], BF16)
    ksT = din("ksT", [64, S], BF16)
    vs = din("vs", [S, 64], BF16)
    kwT = din("kwT", [64, S], BF16)
    vw = din("vw", [S, 64], BF16)
    tabW_d = din("tabW", [5, 128, 2, 128])
    tabS_d = din("tabS", [2, 128, 2, 128])
    cst_d = din("cst", [128, 4, 128])
    tabC_d = din("tabC", [128, 4, 128])
    maskC_d = din("maskC", [17, 128, 128])
    keep_d = din("keep", [NQB, 128, NBLK])
    force_d = din("force", [NQB, 128, NBLK])
    ident_d = din("identb", [128, 128])
    oh_d = din("oh", [64, S], BF16)
    o3s = nc.dram_tensor("o3s", [NQB, 65, 2, 128], F32, kind="ExternalOutput").ap()
    o3 = nc.dram_tensor("o3", [NQB, 128, 3, 2, 65], F32, kind="ExternalOutput").ap()

    s = Sched(nc)
    stg = s.sb("stg", [128, 17 * 128], F32)
    tabW = s.sb("tabW_s", [128, 5, 256], F32)
    tabS = s.sb("tabS_s", [128, 2, 256], F32)
    cst = s.sb("cst_s", [128, 4, 128], F32)
    tabC = s.sb("tabC_s", [128, 512], F32)
    maskC = s.sb("maskC_s", [128, 17, 128], BF16)
    identb = s.sb("identb_s", [128, 128], BF16)
    kc_sb = s.sb("kc_sb", [64, NCC * 128], BF16)
    vc_sb = s.sb("vc_sb", [128, NCC, 65], BF16)
    ks_sb = s.sb("ks_sb", [128, S], BF16)
    vs_sb = s.sb("vs_sb", [128, NCH, 65], BF16)
    kw_sb = s.sb("kw_sb", [64, S], BF16)
    vw_sb = s.sb("vw_sb", [128, NCH, 65], BF16)
    s.dma("sp", tabW[:, :, :], tabW_d.rearrange("v p h q -> p v (h q)"), writes=["tabW"])
    s.dma("sp", tabS[:, :, :], tabS_d.rearrange("v p h q -> p v (h q)"), writes=["tabS"])
    s.dma("sp", cst[:, :, :], cst_d, writes=["cst"])
    s.dma("sp", tabC[:, :], tabC_d.rearrange("p h q -> p (h q)"), writes=["tabC"])
    s.dma("sp", stg[:, 0:17 * 128].rearrange("p (v q) -> p v q", v=17), maskC_d.rearrange("v p q -> p v q"), writes=["stg"])
    s.op("dve", lambda e: e.tensor_copy(out=maskC[:, :, :], in_=stg[:, 0:17 * 128].rearrange("p (v q) -> p v q", v=17)),
         reads=["stg"], writes=["maskC"])
    s.dma("sp", stg[:, 0:128], ident_d, writes=["stg"])
    s.op("dve", lambda e: e.tensor_copy(out=identb[:, :], in_=stg[:, 0:128]), reads=["stg"], writes=["identb"])
    s.dma("sp", kc_sb[:, :], kcT, writes=["kc_sb"])
    s.dma("sp", ks_sb[0:64, :], ksT, writes=["ks_sb"])
    s.dma("sp", ks_sb[64:128, :], oh_d, writes=["ks_sb"])
    s.dma("sp", kw_sb[:, :], kwT, writes=["kw_sb"])
    for (vsb, vd, nm, n_) in ((vc_sb, vc, "vc_sb", NCC), (vs_sb, vs, "vs_sb", NCH), (vw_sb, vw, "vw_sb", NCH)):
        s.op("pool", lambda e: e.memset(vsb[:, :, 64:65], 1.0), writes=[nm])
        s.dma("sp", vsb[:, :, 0:64], vd.rearrange("(c p) d -> p c d", p=128), writes=[nm])

    NB = 4
    LA = 2
    q_sb = [s.sb("q_sb%d" % i, [64, 4, 128], BF16) for i in range(2)]
    keep_sb = [s.sb("keep_sb%d" % i, [128, NBLK], F32) for i in range(2)]
    force_sb = [s.sb("force_sb%d" % i, [128, NBLK], F32) for i in range(2)]
    tmp = [s.sb("tmp%d" % i, [128, 512], F32) for i in range(NB)]
    pc = s.sb("pc", [128, NCC, 512], BF16)
    pw = s.sb("pw", [128, 5, 256], BF16)
    psl = [s.sb("psl%d" % i, [128, 256], BF16) for i in range(NB)]
    oc_sb = s.sb("oc_sb", [128, 4, 65], F32)
    rl = s.sb("rl", [128, 4], F32)
    imp = s.sb("imp", [128, NCC * 128], F32)
    sc = s.sb("sc", [128, NBLK], F32)
    sc2 = s.sb("sc2", [128, NBLK], F32)
    t8a = s.sb("t8a", [128, 8], F32)
    t8b = s.sb("t8b", [128, 8], F32)
    NV = (NBLK + 63) // 64
    selb = s.sb("selb", [128, 64 + 64 * NV], BF16)
    qaug = [[s.sb("qaug%d_%d" % (i, v), [128, 2, 128], BF16) for v in range(NV)] for i in range(2)]
    osT = [s.sb("osT%d" % i, [65, 2, 128], F32) for i in range(2)]
    o_sb = [s.sb("o_sb%d" % i, [128, 3, 2, 65], F32) for i in range(2)]
    ps_s = [s.ps("ps_s%d" % i, [128, 512], key=("ps_s", i)) for i in range(NB)]
    ps_os = [s.ps("ps_os%d" % i, [128, 512], key=("ps_os", i)) for i in range(2)]
    ps_o = s.ps("ps_o", [128, 4, 128], key="ps_o")
    ps_t = s.ps("ps_t", [128, 1024], BF16, key="ps_t")
    s.op("dve", lambda e: e.memset(selb[:, :], 0.0), writes=["selb"])
    for i_ in range(2):
        s.op("pool", lambda e: e.memset(o_sb[i_][:, :, :, :], 0.0), writes=[("o_sb", i_, 0), ("o_sb", i_, 2)])
    for i_ in range(2):
        for v_ in range(NV):
            s.op("pool", lambda e: e.memset(qaug[i_][v_][:, :, :], 0.0), writes=[("qaug", i_, v_)])
    sidx = [0]
    okeys = []

    def nextbuf():
        k = sidx[0] % NB
        sidx[0] += 1
        return k

    def sel_steps(i):
        b = i % 2
        steps = []

        def ld():
            s.dma("sp", q_sb[b][:, :, :], qT4[i], writes=[("q_sb", b)])
            for v_ in range(NV):
                s.dma("sp", qaug[b][v_][0:64, :, :], qT4[i][:, 0:2, :], writes=[("qaug", b, v_)])
            s.dma("sp", keep_sb[b][:, :], keep_d[i], writes=[("keep", b)])
            s.dma("sp", force_sb[b][:, :], force_d[i], writes=[("force", b)])
        steps.append(ld)
        mlist = [m for m in range(NCC) if 16 * CW * m + 31 <= 128 * i + 127]

        def cmp_chunk(m):
            k = nextbuf()
            dl = i - 16 * m
            if dl <= 16:
                s.op("pe", lambda e: e.matmul(ps_s[k][:, :], lhsT=identb[:, :],
                                              rhs=maskC[:, dl, :].unsqueeze(1).to_broadcast([128, 4, 128]),
                                              start=True, stop=False),
                     reads=["identb", "maskC"], writes=[("ps_s", k)])
            for h in range(4):
                s.op("pe", lambda e: e.matmul(ps_s[k][:, h * 128:(h + 1) * 128], lhsT=kc_sb[:, m * 128:(m + 1) * 128],
                                              rhs=q_sb[b][:, h, :], start=(dl > 16), stop=(dl > 16 or h == 3)),
                     reads=["kc_sb", ("q_sb", b)], writes=[("ps_s", k)])
            s.op("dve", lambda e: e.scalar_tensor_tensor(out=tmp[k][:, :], in0=ps_s[k][:, :], scalar=0.125,
                                                         in1=tabC[:, :], op0=ALU.mult, op1=ALU.add),
                 reads=[("ps_s", k), "tabC"], writes=[("tmp", k)])
            for h in range(4):
                s.op("act", lambda e: e.activation(out=pc[:, m, h * 128:(h + 1) * 128], in_=tmp[k][:, h * 128:(h + 1) * 128],
                                                   func=AF.Exp, bias=cst[:, h, dl:dl + 1], scale=1.0),
                     reads=[("tmp", k), "cst"], writes=[("pc", m, h)])
        for m in mlist:
            steps.append(lambda m=m: cmp_chunk(m))

        def cmp_pv():
            for h in range(4):
                for mi, m in enumerate(mlist):
                    s.op("pe", lambda e: e.matmul(ps_o[:, h, 0:65], lhsT=pc[:, m, h * 128:(h + 1) * 128], rhs=vc_sb[:, m, :],
                                                  start=(mi == 0), stop=(mi == len(mlist) - 1)),
                         reads=[("pc", m, h), "vc_sb"], writes=["ps_o"])
            s.op("act", lambda e: e.copy(out=oc_sb[:, :, :], in_=ps_o[:, :, 0:65]), reads=["ps_o"], writes=["oc_sb"])
            s.op("pool", lambda e: e.tensor_copy(out=o_sb[b][:, 0, :, :], in_=oc_sb[:, 0:2, :]), reads=["oc_sb"], writes=[("o_sb", b, 0)])
            s.op("dve", lambda e: e.tensor_scalar(out=rl[:, :], in0=oc_sb[:, :, 64], scalar1=1.0e-30, scalar2=None, op0=ALU.max),
                 reads=["oc_sb"], writes=["rl"])
            s.op("dve", lambda e: e.reciprocal(out=rl[:, :], in_=rl[:, :]), reads=["rl"], writes=["rl"])
            s.op("pool", lambda e: e.memset(imp[:, :], 0.0), writes=["imp"])
        steps.append(cmp_pv)

        def imp_head(h):
            for m in mlist:
                s.op("pe", lambda e: e.transpose(out=ps_t[:, m * 128:(m + 1) * 128], in_=pc[:, m, h * 128:(h + 1) * 128],
                                                 identity=identb[:, :]),
                     reads=[("pc", m, h), "identb"], writes=["ps_t"])
            w = len(mlist) * 128
            s.op("dve", lambda e: e.scalar_tensor_tensor(out=imp[:, 0:w], in0=ps_t[:, 0:w], scalar=rl[:, h:h + 1],
                                                         in1=imp[:, 0:w], op0=ALU.mult, op1=ALU.add),
                 reads=["ps_t", "rl", "imp"], writes=["imp"])
        for h in range(4):
            steps.append(lambda h=h: imp_head(h))
        A = imp[:, :].rearrange("p (n f) -> p n f", f=4)
        nb = NBLK

        def score1():
            s.op("dve", lambda e: e.tensor_tensor(out=sc[:, :], in0=A[:, 0:nb, 0], in1=A[:, 0:nb, 1], op=ALU.add), reads=["imp"], writes=["sc"])
            s.op("dve", lambda e: e.tensor_tensor(out=sc[:, :], in0=sc[:, :], in1=A[:, 0:nb, 2], op=ALU.add), reads=["imp", "sc"], writes=["sc"])
            s.op("dve", lambda e: e.scalar_tensor_tensor(out=sc[:, :], in0=sc[:, :], scalar=2.0, in1=A[:, 0:nb, 3],
                                                         op0=ALU.mult, op1=ALU.add), reads=["imp", "sc"], writes=["sc"])
            s.op("dve", lambda e: e.tensor_tensor(out=sc[:, 1:nb], in0=sc[:, 1:nb], in1=A[:, 0:nb - 1, 3], op=ALU.add),
                 reads=["imp", "sc"], writes=["sc"])

        def score2():
            s.op("dve", lambda e: e.tensor_tensor(out=sc[:, :], in0=sc[:, :], in1=keep_sb[b][:, :], op=ALU.mult),
                 reads=["sc", ("keep", b)], writes=["sc"])
            s.op("dve", lambda e: e.tensor_tensor(out=sc[:, :], in0=sc[:, :], in1=force_sb[b][:, :], op=ALU.add),
                 reads=["sc", ("force", b)], writes=["sc"])
            s.op("dve", lambda e: e.max(out=t8a[:, :], in_=sc[:, :]), reads=["sc"], writes=["t8a"])

        def score3():
            s.op("dve", lambda e: e.match_replace(out=sc2[:, :], in_to_replace=t8a[:, :], in_values=sc[:, :], imm_value=-3.0e4),
                 reads=["sc", "t8a"], writes=["sc2"])
            s.op("dve", lambda e: e.max(out=t8b[:, :], in_=sc2[:, :]), reads=["sc2"], writes=["t8b"])
            s.op("dve", lambda e: e.tensor_scalar(out=sc2[:, :], in0=sc[:, :], scalar1=t8b[:, 7:8], scalar2=-MNEG,
                                                  op0=ALU.is_ge, op1=ALU.mult), reads=["sc", "t8b"], writes=["sc2"])
            s.op("dve", lambda e: e.tensor_scalar(out=selb[:, 64:64 + nb], in0=sc2[:, :], scalar1=MNEG, scalar2=None, op0=ALU.add),
                 reads=["sc2"], writes=["selb"])

        def score4():
            for v_ in range(NV):
                s.op("pe", lambda e: e.transpose(out=ps_t[:, v_ * 128:(v_ + 1) * 128], in_=selb[:, 64 * v_:64 * v_ + 128],
                                                 identity=identb[:, :]), reads=["selb", "identb"], writes=["ps_t"])
            for v_ in range(NV):
                s.op("act", lambda e: e.copy(out=qaug[b][v_][64:128, :, :],
                                             in_=ps_t[64:128, v_ * 128:(v_ + 1) * 128].unsqueeze(1).to_broadcast([64, 2, 128])),
                     reads=["ps_t"], writes=[("qaug", b, v_)])
        steps += [score1, score2, score3, score4]
        return steps

    def s_stage(i, j, k):
        b = i % 2
        v_ = j // 32
        s.op("pe", lambda e: e.matmul(ps_s[k][:, 0:256], lhsT=ks_sb[:, j * 128:(j + 1) * 128],
                                      rhs=qaug[b][v_][:, :, :].rearrange("p a b -> p (a b)"), start=True, stop=True),
             reads=["ks_sb", ("qaug", b, v_)], writes=[("ps_s", k)])
        s.op("dve", lambda e: e.scalar_tensor_tensor(out=tmp[k][:, 0:256], in0=ps_s[k][:, 0:256], scalar=0.125,
                                                     in1=tabS[:, 0 if j == i else 1, :], op0=ALU.mult, op1=ALU.add),
             reads=[("ps_s", k), "tabS"], writes=[("tmp", k)])
        for h in range(2):
            s.op("act", lambda e: e.activation(out=psl[k][:, h * 128:(h + 1) * 128], in_=tmp[k][:, h * 128:(h + 1) * 128],
                                               func=AF.Exp, bias=cst[:, h, i - j:i - j + 1], scale=1.0),
                 reads=[("tmp", k), "cst"], writes=[("psl", k, h)])

    def pv_stage(i, j, k):
        s.op("pe", lambda e: e.matmul(ps_os[0][0:65, 0:256], lhsT=vs_sb[:, j, :], rhs=psl[k][:, 0:256],
                                      start=(j == 0), stop=(j == i)),
             reads=[("psl", k, 0), ("psl", k, 1), "vs_sb"], writes=[("ps_os", 0)])

    def window(i):
        b = i % 2
        wl = [j for j in range(i - 4, i + 1) if j >= 0]
        for wi, j in enumerate(wl):
            k = nextbuf()
            for h in range(2):
                s.op("pe", lambda e: e.matmul(ps_s[k][:, h * 128:(h + 1) * 128], lhsT=kw_sb[:, j * 128:(j + 1) * 128],
                                              rhs=q_sb[b][:, h, :], start=True, stop=True),
                     reads=["kw_sb", ("q_sb", b)], writes=[("ps_s", k)])
            s.op("dve", lambda e: e.scalar_tensor_tensor(out=tmp[k][:, 0:256], in0=ps_s[k][:, 0:256], scalar=0.125,
                                                         in1=tabW[:, i - j, :], op0=ALU.mult, op1=ALU.add),
                 reads=[("ps_s", k), "tabW"], writes=[("tmp", k)])
            s.op("act", lambda e: e.activation(out=pw[:, wi, :], in_=tmp[k][:, 0:256], func=AF.Exp),
                 reads=[("tmp", k)], writes=[("pw", wi)])
        for h in range(2):
            for wi, j in enumerate(wl):
                s.op("pe", lambda e: e.matmul(ps_o[:, h, 0:65], lhsT=pw[:, wi, h * 128:(h + 1) * 128], rhs=vw_sb[:, j, :],
                                              start=(wi == 0), stop=(wi == len(wl) - 1)),
                     reads=[("pw", wi), "vw_sb"], writes=["ps_o"])
        s.op("act", lambda e: e.copy(out=o_sb[b][:, 2, :, :], in_=ps_o[:, 0:2, 0:65]), reads=["ps_o"], writes=[("o_sb", b, 2)])

    for st in sel_steps(0):
        st()
    for i in range(NQB):
        b = i % 2
        nxt = sel_steps(i + 1) if i + 1 < NQB else []
        n = i + 1
        bufs = {}
        for step in range(n + LA):
            if step < n:
                bufs[step] = nextbuf()
                s_stage(i, step, bufs[step])
            if step - LA >= 0:
                pv_stage(i, step - LA, bufs[step - LA])
            if nxt and step % 2 == 1:
                nxt.pop(0)()
        s.op("act", lambda e: e.copy(out=osT[b][:, :, :].rearrange("p a b -> p (a b)"), in_=ps_os[0][0:65, 0:256]),
             reads=[("ps_os", 0)], writes=[("osT", b)])
        s.dma("sp", o3s[i], osT[b][:, :, :], reads=[("osT", b)], writes=[("o3s", i)])
        okeys.append(("o3s", i))
        window(i)
        while nxt:
            nxt.pop(0)()
        s.dma("sp", o3[i], o_sb[b][:, :, :, :], reads=[("o_sb", b, 0), ("o_sb", b, 2)], writes=[("o3", i)])
        okeys.append(("o3", i))
    s.finish(okeys)
    print("nsa ninst", s.ninst)
    s.close()
    return nc


import numpy as np

NCORES = 8
S = 16384
TPC = S // NCORES
_cache = {}


def _get(key, fn):
    if key not in _cache:
        _cache[key] = fn()
    return _cache[key]


def run_proj(h, w, out_bf16=True):
    N = w.shape[1]
    nc = _get(("proj", N, out_bf16), lambda: build_proj(TPC // 128, N, out_bf16))
    in_maps = [{"xT": np.ascontiguousarray(h[c * TPC:(c + 1) * TPC].T), "w": w} for c in range(NCORES)]
    res = run_bass_kernel_spmd(nc, in_maps, core_ids=list(range(NCORES)))
    return np.concatenate([np.asarray(r["y"]) for r in res.results], axis=0)


def run_atta(qkv):
    qTs, kTs, vs, meta = atta_host_prep(qkv)
    upc = [len(m) // NCORES for m in meta]
    units = []
    for g in range(3):
        for p in range(upc[g]):
            units.append(3 * g + (0 if p == 0 else (2 if p == 8 else 1)))
    nc = _get(("atta",), lambda: build_atta(units, 9))
    tab = atta_bias_table()
    in_maps = []
    for c in range(NCORES):
        tabs = np.zeros((9, 128, 2, 16, 128), np.float32)
        for g in range(3):
            for slot, p in ((0, 0), (1, 1), (2, 8)):
                t = tab[g].copy()
                if meta[g][c * upc[g] + p][1]:
                    t[:, 0] = -1.0e30
                tabs[3 * g + slot] = t
        sl = [slice(c * upc[g], (c + 1) * upc[g]) for g in range(3)]
        in_maps.append({"qT": np.concatenate([qTs[g][sl[g]] for g in range(3)]),
                        "kT": np.concatenate([kTs[g][sl[g]] for g in range(3)]),
                        "v": np.concatenate([vs[g][sl[g]] for g in range(3)]),
                        "tab": tabs})
    res = run_bass_kernel_spmd(nc, in_maps, core_ids=list(range(NCORES)))
    og = []
    for g in range(3):
        off = sum(upc[:g])
        og.append(np.concatenate([np.asarray(r["o"])[off:off + upc[g]] for r in res.results], axis=0))
    return atta_host_post(og, S)


def run_post(h, o_or_og, modeA, wout, lnp, wr, br, wgu, wd):
    nc = _get(("post", modeA), lambda: build_post(TPC, modeA))
    consts = post_consts()
    in_maps = []
    for c in range(NCORES):
        rows = slice(c * TPC, (c + 1) * TPC)
        m = {"h": np.ascontiguousarray(h[rows]), "wout": wout, "lnp": lnp, "wr": wr, "br": br, "wgu": wgu, "wd": wd}
        m.update(consts)
        if modeA:
            m["og"] = np.ascontiguousarray(o_or_og[:, rows])
        else:
            m["o"] = np.ascontiguousarray(o_or_og[rows])
        in_maps.append(m)
    res = run_bass_kernel_spmd(nc, in_maps, core_ids=list(range(NCORES)))
    return np.concatenate([np.asarray(r["hout"]) for r in res.results], axis=0)


def layer_a(h, l, inp):
    qkv = run_proj(h, np.ascontiguousarray(inp["a_w_in"][l]))
    og = run_atta(qkv)
    lnp = np.stack([inp["ln_mix_g"][l], inp["ln_mix_b"][l], inp["ln_ffn_g"][l], inp["ln_ffn_b"][l]])
    return run_post(h, og, 1, np.ascontiguousarray(inp["a_w_out"][l]), lnp,
                    np.ascontiguousarray(inp["moe_w_router"][l]), np.ascontiguousarray(inp["moe_b_router"][l]),
                    np.ascontiguousarray(inp["moe_w_gate_up"][l]), np.ascontiguousarray(inp["moe_w_down"][l]))


BF = ml_dtypes.bfloat16


def run_proj_b(h, w, bias):
    N = w.shape[1]
    nc = _get(("projb", N), lambda: build_proj(TPC // 128, N, True, True))
    in_maps = [{"xT": np.ascontiguousarray(h[c * TPC:(c + 1) * TPC].T), "w": w, "bias": bias} for c in range(NCORES)]
    res = run_bass_kernel_spmd(nc, in_maps, core_ids=list(range(NCORES)))
    return np.concatenate([np.asarray(r["y"]) for r in res.results], axis=0)


def run_cmp(kv, inp):
    kv6 = kv.reshape(S, 6, 4, 64)
    nc = _get(("cmp",), build_cmp)
    in_maps = []
    nblk = S // 16
    for j in range(2):
        for g in range(4):
            u = kv6[:, j, g, :]
            c = u.reshape(nblk, 16, 64)
            blocks = np.concatenate([c[:-1], c[1:]], axis=1)
            X = np.zeros((nblk, 2048), kv.dtype)
            X[:nblk - 1] = blocks.reshape(nblk - 1, 2048)
            in_maps.append({"xT": np.ascontiguousarray(X.T), "pos": np.ascontiguousarray(inp["b_cmp_pos"][j].reshape(2048)),
                            "w1": np.ascontiguousarray(inp["b_cmp_w1"][j]), "b1": np.ascontiguousarray(inp["b_cmp_b1"][j]),
                            "w2": np.ascontiguousarray(inp["b_cmp_w2"][j]), "b2": np.ascontiguousarray(inp["b_cmp_b2"][j])})
    res = run_bass_kernel_spmd(nc, in_maps, core_ids=list(range(NCORES)))
    outs = [np.asarray(r["outT"]) for r in res.results]
    kcT = [outs[g] for g in range(4)]
    vc = [np.ascontiguousarray(outs[4 + g].T) for g in range(4)]
    return kcT, vc


def run_nsa(proj, kv, kcT, vc):
    nc = _get(("nsa",), lambda: build_nsa(S))
    kv6 = kv.reshape(S, 6, 4, 64)
    q = proj[:, :1024].reshape(S // 128, 128, 16, 64)
    in_maps = []
    for c in range(NCORES):
        g, half = c // 2, c % 2
        own = [4 * g + 2 * half, 4 * g + 2 * half + 1]
        oth = [4 * g + 2 * (1 - half), 4 * g + 2 * (1 - half) + 1]
        heads4 = own + oth
        m = nsa_tables(S, heads4)
        m["qT4"] = np.ascontiguousarray(q[:, :, heads4, :].transpose(0, 3, 2, 1))
        m["kcT"] = kcT[g]
        m["vc"] = vc[g]
        m["ksT"] = np.ascontiguousarray(kv6[:, 2, g, :].T)
        m["vs"] = np.ascontiguousarray(kv6[:, 3, g, :])
        m["kwT"] = np.ascontiguousarray(kv6[:, 4, g, :].T)
        m["vw"] = np.ascontiguousarray(kv6[:, 5, g, :])
        in_maps.append(m)
    res = run_bass_kernel_spmd(nc, in_maps, core_ids=list(range(NCORES)))
    og = np.zeros((3, S, 16, 65), np.float32)
    for c in range(NCORES):
        g, half = c // 2, c % 2
        o3 = np.asarray(res.results[c]["o3"]).reshape(S, 3, 2, 65).copy()
        o3[:, 1] = np.asarray(res.results[c]["o3s"]).transpose(0, 3, 2, 1).reshape(S, 2, 65)
        for hh in range(2):
            og[:, :, 4 * g + 2 * half + hh, :] = o3[:, :, hh, :].transpose(1, 0, 2)
    return og.reshape(3, S, 1040)


def layer_b(h, i, inp, shared):
    l = 2 + i
    proj = run_proj_b(h, np.ascontiguousarray(inp["b_w_q"][i]), np.ascontiguousarray(inp["b_b_q"][i]))
    kv, kcT, vc = shared
    og = run_nsa(proj, kv, kcT, vc)
    lnp = np.stack([inp["ln_mix_g"][l], inp["ln_mix_b"][l], inp["ln_ffn_g"][l], inp["ln_ffn_b"][l]])
    gl = np.ascontiguousarray(proj[:, 1024:1072])
    nc = _get(("post", 0), lambda: build_post(TPC, 0))
    consts = post_consts()
    in_maps = []
    for c in range(NCORES):
        rows = slice(c * TPC, (c + 1) * TPC)
        m = {"h": np.ascontiguousarray(h[rows]), "wout": np.ascontiguousarray(inp["b_w_out"][i]), "lnp": lnp,
             "wr": np.ascontiguousarray(inp["moe_w_router"][l]), "br": np.ascontiguousarray(inp["moe_b_router"][l]),
             "wgu": np.ascontiguousarray(inp["moe_w_gate_up"][l]), "wd": np.ascontiguousarray(inp["moe_w_down"][l]),
             "og": np.ascontiguousarray(og[:, rows]), "gl": np.ascontiguousarray(gl[rows])}
        m.update(consts)
        in_maps.append(m)
    res = run_bass_kernel_spmd(nc, in_maps, core_ids=list(range(NCORES)))
    return np.concatenate([np.asarray(r["hout"]) for r in res.results], axis=0)


def forward(inp):
    h = np.ascontiguousarray(inp["x"][0])
    for l in range(2):
        h = layer_a(h, l, inp)
    kv = run_proj(h, np.ascontiguousarray(inp["b_w_kv"]))
    kcT, vc = run_cmp(kv, inp)
    for i in range(2):
        h = layer_b(h, i, inp, (kv, kcT, vc))
    return h[None].astype(np.float32)


def kernel(**inputs):
    inp = {k: np.asarray(v) for k, v in inputs.items()}
    return forward(inp)
```

```python
import ml_dtypes
import contextlib
import numpy as np
import concourse.bass as bass
import concourse.mybir as mybir
from concourse.bass_utils import run_bass_kernel_spmd

F32 = mybir.dt.float32
BF16 = mybir.dt.bfloat16
I32 = mybir.dt.int32
U32 = mybir.dt.uint32
AF = mybir.ActivationFunctionType
ALU = mybir.AluOpType
AX = mybir.AxisListType

DMA_RING = 6


class Sched:
    def __init__(self, nc, same_engine_sync=None):
        if same_engine_sync is None:
            same_engine_sync = True
        self.nc = nc
        self.es = contextlib.ExitStack()
        self.engs = {"pe": nc.tensor, "dve": nc.vector, "act": nc.scalar,
                     "pool": nc.gpsimd, "sp": nc.sync}
        self.sem = {}
        self.cnt = {}
        for e in self.engs:
            self.sem[e] = self.es.enter_context(nc.semaphore("s_" + e))
            self.cnt[e] = 0
        self.dsem = {}
        self.dcnt = {}
        self.dissued = {}
        for q in ("sp", "act", "pool"):
            self.dsem[q] = [self.es.enter_context(nc.semaphore("d_%s%d" % (q, i)))
                            for i in range(DMA_RING)]
            self.dcnt[q] = [0] * DMA_RING
            self.dissued[q] = 0
        self.waited = {}
        self.lastw = {}
        self.readers = {}
        self.same = same_engine_sync
        self.excl = set()
        self.ninst = 0

    def sb(self, name, shape, dt):
        return self.es.enter_context(self.nc.sbuf_tensor(name, shape, dt))

    def ps(self, name, shape, dt=F32, key=None):
        self.excl.add(key if key is not None else name)
        return self.es.enter_context(self.nc.psum_tensor(name, shape, dt))

    def _semobj(self, key):
        if isinstance(key, tuple):
            return self.dsem[key[0]][key[1]]
        return self.sem[key]

    def _wait(self, eng, key, val):
        if key == eng and (not self.same or eng == "pe"):
            return
        w = self.waited.get((eng, key), 0)
        if val > w:
            self.engs[eng].wait_ge(self._semobj(key), val)
            self.waited[(eng, key)] = val

    def _deps(self, eng, reads, writes):
        for t in reads:
            lw = self.lastw.get(t)
            if lw is not None:
                self._wait(eng, lw[0], lw[1])
        for t in writes:
            lw = self.lastw.get(t)
            if lw is not None:
                self._wait(eng, lw[0], lw[1])
            for r in self.readers.get(t, ()):
                self._wait(eng, r[0], r[1])

    def _record(self, stamp, reads, writes):
        for t in writes:
            self.lastw[t] = stamp
            self.readers[t] = []
        for t in reads:
            self.readers.setdefault(t, []).append(stamp)

    def op(self, eng, fn, reads=(), writes=()):
        ex = [t for t in reads if t in self.excl]
        if ex:
            reads = [t for t in reads if t not in self.excl]
            writes = list(writes) + ex
        self._deps(eng, reads, writes)
        ins = fn(self.engs[eng])
        self.cnt[eng] += 1
        ins.then_inc(self.sem[eng], 1)
        self._record((eng, self.cnt[eng]), reads, writes)
        self.ninst += 1
        return ins

    def dma(self, q, out, in_, reads=(), writes=(), **kw):
        slot = self.dissued[q] % DMA_RING
        self.dissued[q] += 1
        key = (q, slot)
        if self.dcnt[q][slot] > 0:
            self._wait(q, key, self.dcnt[q][slot])
        self._deps(q, reads, writes)
        ins = self.engs[q].dma_start(out=out, in_=in_, **kw)
        self.dcnt[q][slot] += 16
        ins.then_inc(self.dsem[q][slot], 16)
        self._record((key, self.dcnt[q][slot]), reads, writes)
        self.ninst += 1
        return ins

    def finish(self, out_tiles):
        for t in out_tiles:
            lw = self.lastw.get(t)
            if lw is not None:
                self._wait("sp", lw[0], lw[1])

    def close(self):
        self.es.close()


def build_proj(NT, N, out_bf16=True, has_bias=False):
    nc = bass.Bass("TRN2", target_bir_lowering=False)
    T = NT * 128
    xT = nc.dram_tensor("xT", [1024, T], F32, kind="ExternalInput").ap()
    w = nc.dram_tensor("w", [1024, N], F32, kind="ExternalInput").ap()
    odt = BF16 if out_bf16 else F32
    y = nc.dram_tensor("y", [T, N], odt, kind="ExternalOutput").ap()
    if has_bias:
        bias = nc.dram_tensor("bias", [N], F32, kind="ExternalInput").ap()
    s = Sched(nc)
    if has_bias:
        bias_bc = s.sb("bias_bc", [128, N], F32)
        s.dma("sp", bias_bc[:, :], bias.partition_broadcast(128), writes=["bias_bc"])
    x_f = s.sb("x_f", [128, 8, 512], F32)
    x_b = s.sb("x_b", [128, 8, T], BF16)
    xTv = xT.rearrange("(kc p) t -> p kc t", p=128)
    for c in range(T // 512):
        s.dma("sp", x_f[:, :, :], xTv[:, :, c * 512:(c + 1) * 512], writes=["x_f"])
        s.op("dve", lambda e: e.tensor_copy(out=x_b[:, :, c * 512:(c + 1) * 512], in_=x_f[:, :, :]),
             reads=["x_f"], writes=[("x_b", c)])
    CW = 512
    nch = (N + CW - 1) // CW
    w_f = [s.sb("w_f%d" % i, [128, 8, CW], F32) for i in range(2)]
    w_b = [s.sb("w_b%d" % i, [128, 8, CW], BF16) for i in range(2)]
    pt = [s.ps("pt%d" % i, [128, CW], key=("pt", i)) for i in range(4)]
    ot = [s.sb("ot%d" % i, [128, CW], odt) for i in range(4)]
    wv = w.rearrange("(kc p) n -> p kc n", p=128)
    k = 0

    def load_chunk(ch):
        n0 = ch * CW
        cw = min(CW, N - n0)
        b = ch % 2
        s.dma("sp", w_f[b][:, :, :cw], wv[:, :, n0:n0 + cw], writes=[("w_f", b)])
        s.op("pool", lambda e: e.tensor_copy(out=w_b[b][:, :, :cw], in_=w_f[b][:, :, :cw]),
             reads=[("w_f", b)], writes=[("w_b", b)])

    load_chunk(0)
    for ch in range(nch):
        n0 = ch * CW
        cw = min(CW, N - n0)
        b = ch % 2
        if ch + 1 < nch:
            load_chunk(ch + 1)
        for t in range(NT):
            pb = k % 4
            k += 1
            for kc in range(8):
                s.op("pe", lambda e: e.matmul(pt[pb][:, :cw], lhsT=x_b[:, kc, t * 128:(t + 1) * 128],
                                              rhs=w_b[b][:, kc, :cw], start=(kc == 0), stop=(kc == 7)),
                     reads=[("x_b", t // 4), ("w_b", b)], writes=[("pt", pb)])
            if has_bias:
                s.op("dve", lambda e: e.tensor_tensor(out=ot[pb][:, :cw], in0=pt[pb][:, :cw], in1=bias_bc[:, n0:n0 + cw], op=ALU.add),
                     reads=[("pt", pb), "bias_bc"], writes=[("ot", pb)])
            elif pb % 2 == 0:
                s.op("act", lambda e: e.copy(out=ot[pb][:, :cw], in_=pt[pb][:, :cw]),
                     reads=[("pt", pb)], writes=[("ot", pb)])
            else:
                s.op("dve", lambda e: e.tensor_copy(out=ot[pb][:, :cw], in_=pt[pb][:, :cw]),
                     reads=[("pt", pb)], writes=[("ot", pb)])
            s.dma("sp" if pb % 2 else "pool", y[t * 128:(t + 1) * 128, n0:n0 + cw], ot[pb][:, :cw],
                  reads=[("ot", pb)], writes=[("y", t, ch)])
    s.finish([("y", t, ch) for t in range(NT) for ch in range(nch)])
    print("ninst", s.ninst)
    s.close()
    return nc


A_DIL = (1, 4, 16)


def atta_bias_table():
    slopes = (2.0 ** (-8.0 * np.arange(1, 17) / 16)).astype(np.float32)
    k = np.arange(128)[:, None]
    q = np.arange(128)[None, :]
    tab = np.zeros((3, 128, 2, 16, 128), np.float32)
    for g, d in enumerate(A_DIL):
        for ch in range(2):
            dist = (q - k + (128 if ch == 0 else 0))
            valid = (dist >= 0) & (dist <= 128)
            for hh in range(16):
                b = -(slopes[hh] * (dist * d).astype(np.float32))
                tab[g, :, ch, hh, :] = np.where(valid, b, -1.0e30)
    return tab


def build_atta(units, nslot):
    nc = bass.Bass("TRN2", target_bir_lowering=False)
    U = len(units)
    qT = nc.dram_tensor("qT", [U, 64, 16, 128], BF16, kind="ExternalInput").ap()
    kT = nc.dram_tensor("kT", [U, 64, 16, 256], BF16, kind="ExternalInput").ap()
    v = nc.dram_tensor("v", [U, 2, 128, 16, 64], BF16, kind="ExternalInput").ap()
    tab = nc.dram_tensor("tab", [nslot, 128, 2, 16, 128], F32, kind="ExternalInput").ap()
    o = nc.dram_tensor("o", [U, 128, 16 * 65], F32, kind="ExternalOutput").ap()
    s = Sched(nc)
    tb = s.sb("tb", [128, 2, 16, 128], F32)
    q_sb = [s.sb("q_sb%d" % i, [64, 16, 128], BF16) for i in range(2)]
    k_sb = [s.sb("k_sb%d" % i, [64, 16, 256], BF16) for i in range(2)]
    v_sb = [s.sb("v_sb%d" % i, [128, 2, 16, 65], BF16) for i in range(2)]
    tmp = [s.sb("tmp%d" % i, [128, 2, 512], F32) for i in range(2)]
    pT = [s.sb("pT%d" % i, [128, 2, 512], BF16) for i in range(2)]
    o_sb = [s.sb("o_sb%d" % i, [128, 16, 65], F32) for i in range(2)]
    ps_s = [[s.ps("ps_s%d_%d" % (i, c), [128, 512], key=("ps_s", i, c)) for c in range(2)] for i in range(2)]
    ps_o = [s.ps("ps_o%d" % i, [128, 4, 128], key=("ps_o", i)) for i in range(2)]
    for i in range(2):
        s.op("pool", lambda e: e.memset(v_sb[i][:, :, :, 64:65], 1.0), writes=[("v_sb", i)])
    okeys = []
    state = {"cur_g": -1}
    items = [(u, hg) for u in range(U) for hg in range(4)]

    def S(n):
        u, hg = items[n]
        g = units[u]
        b = u % 2
        i = n % 2
        if hg == 0:
            if g != state["cur_g"]:
                s.dma("sp", tb[:, :, :, :], tab[g], writes=["tb"])
                state["cur_g"] = g
            s.dma("sp", q_sb[b][:, :, :], qT[u], writes=[("q_sb", b)])
            s.dma("sp", k_sb[b][:, :, :], kT[u], writes=[("k_sb", b)])
            for c in range(2):
                s.dma("sp", v_sb[b][:, c, :, 0:64], v[u, c], writes=[("v_sb", b)], reads=[("v_sb", b)])
        for c in range(2):
            for hh in range(4):
                hd = hg * 4 + hh
                s.op("pe", lambda e: e.matmul(ps_s[i][c][:, hh * 128:(hh + 1) * 128],
                                              lhsT=k_sb[b][:, hd, c * 128:(c + 1) * 128], rhs=q_sb[b][:, hd, :],
                                              start=True, stop=True),
                     reads=[("k_sb", b), ("q_sb", b)], writes=[("ps_s", i, c)])
            s.op("dve", lambda e: e.scalar_tensor_tensor(
                out=tmp[i][:, c, :], in0=ps_s[i][c][:, :], scalar=0.125,
                in1=tb[:, c, hg * 4:hg * 4 + 4, :].rearrange("p a b -> p (a b)"),
                op0=ALU.mult, op1=ALU.add),
                reads=[("ps_s", i, c), "tb"], writes=[("tmp", i, c)])
            s.op("act", lambda e: e.activation(out=pT[i][:, c, :], in_=tmp[i][:, c, :], func=AF.Exp),
                 reads=[("tmp", i, c)], writes=[("pT", i, c)])

    def PV(n):
        u, hg = items[n]
        b = u % 2
        i = n % 2
        for hh in range(4):
            hd = hg * 4 + hh
            for c in range(2):
                s.op("pe", lambda e: e.matmul(ps_o[i][:, hh, 0:65], lhsT=pT[i][:, c, hh * 128:(hh + 1) * 128],
                                              rhs=v_sb[b][:, c, hd, :], start=(c == 0), stop=(c == 1)),
                     reads=[("pT", i, c), ("v_sb", b)], writes=[("ps_o", i)])
        s.op("dve" if hg % 2 else "act", (lambda e: e.tensor_copy(out=o_sb[b][:, hg * 4:hg * 4 + 4, :], in_=ps_o[i][:, :, 0:65])) if hg % 2
             else (lambda e: e.copy(out=o_sb[b][:, hg * 4:hg * 4 + 4, :], in_=ps_o[i][:, :, 0:65])),
             reads=[("ps_o", i)], writes=[("o_sb", b)])
        if hg == 3:
            s.dma("sp", o[u], o_sb[b][:, :, :].rearrange("p a b -> p (a b)"), reads=[("o_sb", b)], writes=[("o", u)])
            okeys.append(("o", u))

    S(0)
    for n in range(len(items)):
        if n + 1 < len(items):
            S(n + 1)
        PV(n)
    s.finish(okeys)
    print("atta ninst", s.ninst)
    s.close()
    return nc


def atta_host_prep(qkv):
    S = qkv.shape[0]
    x = qkv.reshape(S, 3, 3, 16, 64)
    qTs, kTs, vs, meta = [], [], [], []
    for g, d in enumerate(A_DIL):
        L = S // d
        nb = L // 128
        def perm(a):
            return a.reshape(L, d, 16, 64).transpose(1, 0, 2, 3)
        q = perm(x[:, g, 0]).reshape(d, nb, 128, 16, 64)
        k = perm(x[:, g, 1])
        vv = perm(x[:, g, 2])
        z = np.zeros((d, 128, 16, 64), qkv.dtype)
        kp = np.concatenate([z, k], axis=1).reshape(d, nb + 1, 128, 16, 64)
        vp = np.concatenate([z, vv], axis=1).reshape(d, nb + 1, 128, 16, 64)
        qT = q.transpose(0, 1, 4, 3, 2).reshape(d * nb, 64, 16, 128)
        k2 = np.stack([kp[:, :-1], kp[:, 1:]], axis=2)
        kT = k2.transpose(0, 1, 5, 4, 2, 3).reshape(d * nb, 64, 16, 256)
        v2 = np.stack([vp[:, :-1], vp[:, 1:]], axis=2).reshape(d * nb, 2, 128, 16, 64)
        qTs.append(np.ascontiguousarray(qT))
        kTs.append(np.ascontiguousarray(kT))
        vs.append(np.ascontiguousarray(v2))
        meta.append([(g, (i % nb) == 0) for i in range(d * nb)])
    return qTs, kTs, vs, meta


def atta_host_post(o_groups, S):
    out = []
    for g, d in enumerate(A_DIL):
        L = S // d
        a = o_groups[g].reshape(d, L, 1040).transpose(1, 0, 2).reshape(S, 1040)
        out.append(a)
    return np.stack(out)


ALPHA = 8.0 ** 0.25
LN_EPS = 1e-5
BIG = 1.0e30


def build_post(T, modeA):
    nc = bass.Bass("TRN2", target_bir_lowering=False)
    D = 1024
    NTL = T // 128
    SG = min(1024, T)
    NSG = T // SG
    TPS = SG // 128

    def din(name, shape, dt=F32):
        return nc.dram_tensor(name, shape, dt, kind="ExternalInput").ap()

    h = din("h", [T, D])
    if modeA:
        og = din("og", [3, T, 16 * 65])
    else:
        og = din("og", [3, T, 16 * 65])
        gl = din("gl", [T, 48], BF16)
    wout = din("wout", [D, D])
    lnp = din("lnp", [4, D])
    wr = din("wr", [D, 20])
    br = din("br", [20])
    wgu = din("wgu", [16, D, 512])
    wd = din("wd", [16, 256, D])
    ident = din("ident", [128, 128])
    sel = din("sel", [128, 16 * 128])
    hout = nc.dram_tensor("hout", [T, D], F32, kind="ExternalOutput").ap()

    s = Sched(nc)
    ident_f = s.sb("ident_f", [128, 128], F32)
    ident_b = s.sb("ident_b", [128, 128], BF16)
    sel_f = s.sb("sel_f", [128, 16 * 128], F32)
    lnbc = s.sb("lnbc", [128, 4, D], F32)
    br_bc = s.sb("br_bc", [128, 20], F32)
    wr_f = s.sb("wr_f", [128, 8, 20], F32)
    wout_b = s.sb("wout_b", [128, 8, D], BF16)
    eps_t = s.sb("eps_t", [128, 1], F32)
    stage = [s.sb("stage%d" % i, [128, 4096], F32) for i in range(2)]
    wgu_b = [s.sb("wgu_b%d" % i, [128, 8, 512], BF16) for i in range(2)]
    wd_b = [s.sb("wd_b%d" % i, [128, 2, D], BF16) for i in range(2)]
    nstage = [0]

    def stage_load(src_ap, n_inner, dst_ap, dkey):
        b = nstage[0] % 2
        nstage[0] += 1
        sv = stage[b][:, :src_ap.shape[1] * src_ap.shape[2]].rearrange("p (a b) -> p a b", a=src_ap.shape[1])
        s.dma("sp", sv, src_ap, writes=[("stage", b)])
        s.op("pool", lambda e: e.tensor_copy(out=dst_ap, in_=sv), reads=[("stage", b)], writes=[dkey])

    s.dma("sp", ident_f[:, :], ident, writes=["ident_f"])
    s.op("dve", lambda e: e.tensor_copy(out=ident_b[:, :], in_=ident_f[:, :]), reads=["ident_f"], writes=["ident_b"])
    s.dma("sp", sel_f[:, :], sel, writes=["sel_f"])
    for i in range(4):
        s.dma("sp", lnbc[:, i, :], lnp[i].partition_broadcast(128), writes=[("lnbc", i)])
    s.dma("sp", br_bc[:, :], br.partition_broadcast(128), writes=["br_bc"])
    s.dma("sp", wr_f[:, :, :], wr.rearrange("(kc p) n -> p kc n", p=128), writes=["wr_f"])
    s.op("dve", lambda e: e.memset(eps_t[:, :], LN_EPS), writes=["eps_t"])
    woutv = wout.rearrange("(kc p) n -> p kc n", p=128)
    for i in range(2):
        stage_load(woutv[:, 4 * i:4 * i + 4, :], None, wout_b[:, 4 * i:4 * i + 4, :], ("wout_b", i))

    h_t = s.sb("h_t", [128, D], F32)
    if modeA:
        og_t = s.sb("og_t", [128, 3, 16 * 65], F32)
        acc = s.sb("acc", [128, 16, 65], F32)
        rl = s.sb("rl", [128, 16], F32)
    else:
        og_t = s.sb("og_t", [128, 3, 16 * 65], F32)
        gl_t = s.sb("gl_t", [128, 48], BF16)
        gate = s.sb("gate", [128, 3, 16], F32)
        rl3 = s.sb("rl3", [128, 3, 16], F32)

    o_b = s.sb("o_b", [128, D], BF16)
    oT = s.sb("oT", [128, 8, 128], BF16)
    r = s.sb("r", [128, D], F32)
    xn = s.sb("xn", [128, D], F32)
    tA = r[:, :].rearrange("p (a b) -> p a b", a=16)
    tB = xn[:, :].rearrange("p (a b) -> p a b", a=16)
    h1 = s.sb("h1", [128, D], F32)
    st = s.sb("st", [128, 2, 6], F32)
    mv = s.sb("mv", [128, 2], F32)
    rstd = s.sb("rstd", [128, 1], F32)
    yacc = s.sb("yacc", [128, TPS, D], F32)
    h1T_b = s.sb("h1T_b", [128, 8, SG], BF16)
    h1T_f = s.sb("h1T_f", [128, 8, 128], F32)
    combT = s.sb("combT", [128, SG], F32)
    L = s.sb("L", [128, 20], F32)
    sm = s.sb("sm", [128, 16], F32)
    goh = s.sb("goh", [128, 4], F32)
    em = s.sb("em", [128, 4, 4], F32)
    top8 = s.sb("top8", [128, 8], F32)
    c0 = s.sb("c0", [128, 16], F32)
    c1 = s.sb("c1", [128, 16], F32)
    comb = s.sb("comb", [128, 128], F32)
    s.op("dve", lambda e: e.memset(comb[:, :], 0.0), writes=["comb"])
    sg = [s.sb("sg%d" % i, [128, 512], F32) for i in range(2)]
    tt_ = [s.sb("tt%d" % i, [128, 512], F32) for i in range(2)]
    hid2 = [[s.sb("hid%d_%d" % (p, i), [128, 512], BF16) for i in range(2)] for p in range(2)]
    cb2 = [s.sb("cb2_%d" % p, [128, 512], F32) for p in range(2)]
    gu = [s.ps("gu%d" % i, [128, 512], key=("gu", i)) for i in range(4)]
    pcb = s.ps("pcb", [128, 512])
    py = [s.ps("py%d" % i, [128, 512], key=("py", i)) for i in range(2)]
    pT_b = s.ps("pT_b", [128, 1024], BF16)

    def layer_norm(src, src_key, gi, dst, dst_key):
        for i in range(2):
            s.op("dve", lambda e: e.bn_stats(out=st[:, i, :], in_=src[:, i * 512:(i + 1) * 512]),
                 reads=[src_key], writes=[("st", i)])
        s.op("dve", lambda e: e.bn_aggr(out=mv[:, :], in_=st[:, :, :]), reads=[("st", 0), ("st", 1)], writes=["mv"])
        s.op("act", lambda e: e.activation(out=rstd[:, :], in_=mv[:, 1:2], func=AF.Sqrt, bias=eps_t[:, :], scale=1.0),
             reads=["mv", "eps_t"], writes=["rstd"])
        s.op("dve", lambda e: e.reciprocal(out=rstd[:, :], in_=rstd[:, :]), reads=["rstd"], writes=["rstd"])
        s.op("dve", lambda e: e.tensor_scalar(out=xn[:, :], in0=src[:, :], scalar1=mv[:, 0:1], scalar2=rstd[:, 0:1],
                                              op0=ALU.subtract, op1=ALU.mult),
             reads=[src_key, "mv", "rstd"], writes=["xn"])
        s.op("pool", lambda e: e.tensor_tensor(out=xn[:, :], in0=xn[:, :], in1=lnbc[:, gi, :], op=ALU.mult),
             reads=["xn", ("lnbc", gi)], writes=["xn"])
        s.op("pool", lambda e: e.tensor_tensor(out=dst, in0=xn[:, :], in1=lnbc[:, gi + 1, :], op=ALU.add),
             reads=["xn", ("lnbc", gi + 1)], writes=[dst_key])

    STOP = 99

    class StopBuild(Exception):
        pass

    def ck(n):
        if STOP == n:
            raise StopBuild()

    out_keys = []
    try:
      ck(1)
      for sgi in range(NSG):
          for ti in range(TPS):
              gt = sgi * TPS + ti
              rows = slice(gt * 128, (gt + 1) * 128)
              s.dma("sp", h_t[:, :], h[rows, :], writes=["h_t"])
              if modeA:
                  s.dma("sp", og_t[:, :, :], og[:, rows, :].rearrange("g t c -> t g c"), writes=["og_t"])
                  accf = acc[:, :, :].rearrange("p a b -> p (a b)")
                  s.op("dve", lambda e: e.tensor_tensor(out=accf, in0=og_t[:, 0, :], in1=og_t[:, 1, :], op=ALU.add),
                       reads=["og_t"], writes=["acc"])
                  s.op("dve", lambda e: e.tensor_tensor(out=accf, in0=accf, in1=og_t[:, 2, :], op=ALU.add),
                       reads=["og_t", "acc"], writes=["acc"])
                  s.op("dve", lambda e: e.reciprocal(out=rl[:, :], in_=acc[:, :, 64]), reads=["acc"], writes=["rl"])
                  s.op("dve", lambda e: e.tensor_tensor(out=o_b[:, :].rearrange("p (a b) -> p a b", a=16),
                                                        in0=acc[:, :, 0:64],
                                                        in1=rl[:, :].unsqueeze(2).to_broadcast([128, 16, 64]),
                                                        op=ALU.mult),
                       reads=["acc", "rl"], writes=["o_b"])
              else:
                  s.dma("sp", og_t[:, :, :], og[:, rows, :].rearrange("g t c -> t g c"), writes=["og_t"])
                  s.dma("sp", gl_t[:, :], gl[rows, :], writes=["gl_t"])
                  s.op("act", lambda e: e.activation(out=gate[:, :, :].rearrange("p a b -> p (a b)"), in_=gl_t[:, :], func=AF.Sigmoid),
                       reads=["gl_t"], writes=["gate"])
                  ogv = og_t[:, :, :].rearrange("p g (h c) -> p g h c", c=65)
                  s.op("dve", lambda e: e.tensor_scalar(out=rl3[:, :, :], in0=ogv[:, :, :, 64], scalar1=1.0e-30, scalar2=None, op0=ALU.max),
                       reads=["og_t"], writes=["rl3"])
                  s.op("dve", lambda e: e.reciprocal(out=rl3[:, :, :], in_=rl3[:, :, :]), reads=["rl3"], writes=["rl3"])
                  s.op("dve", lambda e: e.tensor_tensor(out=rl3[:, :, :], in0=rl3[:, :, :], in1=gate[:, :, :], op=ALU.mult),
                       reads=["rl3", "gate"], writes=["rl3"])
                  s.op("dve", lambda e: e.tensor_tensor(out=tA[:, :, :], in0=ogv[:, 0, :, 0:64],
                                                        in1=rl3[:, 0, :].unsqueeze(2).to_broadcast([128, 16, 64]), op=ALU.mult),
                       reads=["og_t", "rl3"], writes=["r"])
                  s.op("dve", lambda e: e.tensor_tensor(out=tB[:, :, :], in0=ogv[:, 1, :, 0:64],
                                                        in1=rl3[:, 1, :].unsqueeze(2).to_broadcast([128, 16, 64]), op=ALU.mult),
                       reads=["og_t", "rl3"], writes=["xn"])
                  s.op("dve", lambda e: e.tensor_tensor(out=tA[:, :, :], in0=tA[:, :, :], in1=tB[:, :, :], op=ALU.add),
                       reads=["r", "xn"], writes=["r"])
                  s.op("dve", lambda e: e.tensor_tensor(out=tB[:, :, :], in0=ogv[:, 2, :, 0:64],
                                                        in1=rl3[:, 2, :].unsqueeze(2).to_broadcast([128, 16, 64]), op=ALU.mult),
                       reads=["og_t", "rl3"], writes=["xn"])
                  s.op("dve", lambda e: e.tensor_tensor(out=o_b[:, :].rearrange("p (a b) -> p a b", a=16), in0=tA[:, :, :], in1=tB[:, :, :], op=ALU.add),
                       reads=["r", "xn"], writes=["o_b"])
              for kc in range(8):
                  s.op("pe", lambda e: e.transpose(out=pT_b[:, kc * 128:(kc + 1) * 128],
                                                   in_=o_b[:, kc * 128:(kc + 1) * 128], identity=ident_b[:, :]),
                       reads=["o_b", "ident_b"], writes=["pT_b"])
              s.op("act", lambda e: e.copy(out=oT[:, :, :].rearrange("p a b -> p (a b)"), in_=pT_b[:, :]),
                   reads=["pT_b"], writes=["oT"])
              for hh in range(2):
                  for kc in range(8):
                      s.op("pe", lambda e: e.matmul(gu[hh][:, :], lhsT=oT[:, kc, :],
                                                    rhs=wout_b[:, kc, hh * 512:(hh + 1) * 512],
                                                    start=(kc == 0), stop=(kc == 7)),
                           reads=["oT", ("wout_b", kc // 4)], writes=[("gu", hh)])
                  s.op("dve", lambda e: e.scalar_tensor_tensor(out=r[:, hh * 512:(hh + 1) * 512],
                                                               in0=h_t[:, hh * 512:(hh + 1) * 512], scalar=ALPHA,
                                                               in1=gu[hh][:, :], op0=ALU.mult, op1=ALU.add),
                       reads=["h_t", ("gu", hh)], writes=["r"])
              ck(2)
              layer_norm(r, "r", 0, h1[:, :], "h1")
              ck(3)
              s.op("act", lambda e: e.mul(out=yacc[:, ti, :], in_=h1[:, :], mul=ALPHA),
                   reads=["h1"], writes=[("yacc", ti)])
              ck(31)
              for q in range(2):
                  for j in range(4):
                      kc = q * 4 + j
                      s.op("pe", lambda e: e.transpose(out=gu[2 + q][:, j * 128:(j + 1) * 128],
                                                       in_=h1[:, kc * 128:(kc + 1) * 128], identity=ident_f[:, :]),
                           reads=["h1", "ident_f"], writes=[("gu", 2 + q)])
                  ck(32)
                  s.op("act", lambda e: e.copy(out=h1T_f[:, q * 4:q * 4 + 4, :],
                                               in_=gu[2 + q][:, :].rearrange("p (a b) -> p a b", a=4)),
                       reads=[("gu", 2 + q)], writes=[("h1T_f", q)])
                  ck(33)
                  s.op("dve", lambda e: e.tensor_copy(out=h1T_b[:, q * 4:q * 4 + 4, ti * 128:(ti + 1) * 128],
                                                      in_=gu[2 + q][:, :].rearrange("p (a b) -> p a b", a=4)),
                       reads=[("gu", 2 + q)], writes=[("h1T_b", ti)])
              ck(4)
              for kc in range(8):
                  s.op("pe", lambda e: e.matmul(pcb[:, 0:20], lhsT=h1T_f[:, kc, :], rhs=wr_f[:, kc, :],
                                                start=(kc == 0), stop=(kc == 7)),
                       reads=[("h1T_f", kc // 4), "wr_f"], writes=["pcb"])
              s.op("dve", lambda e: e.tensor_tensor(out=L[:, :], in0=pcb[:, 0:20], in1=br_bc[:, :], op=ALU.add),
                   reads=["pcb", "br_bc"], writes=["L"])
              ck(5)
              V = lambda eng, fn, rd, wr_: s.op(eng, fn, reads=rd, writes=wr_)
              V("dve", lambda e: e.reduce_max(out=sm[:, 0:1], in_=L[:, 0:4], axis=AX.X), ["L"], ["sm"])
              V("dve", lambda e: e.tensor_scalar(out=sm[:, 1:2], in0=sm[:, 0:1], scalar1=-1.0, scalar2=None, op0=ALU.mult),
                ["sm"], ["sm"])
              V("act", lambda e: e.activation(out=goh[:, :], in_=L[:, 0:4], func=AF.Exp, bias=sm[:, 1:2], scale=1.0),
                ["L", "sm"], ["goh"])
              V("dve", lambda e: e.reduce_sum(out=sm[:, 2:3], in_=goh[:, :], axis=AX.X), ["goh"], ["sm"])
              V("dve", lambda e: e.reciprocal(out=sm[:, 3:4], in_=sm[:, 2:3]), ["sm"], ["sm"])
              V("dve", lambda e: e.tensor_scalar(out=goh[:, :], in0=L[:, 0:4], scalar1=sm[:, 0:1], scalar2=None,
                                                 op0=ALU.is_equal), ["L", "sm", "goh"], ["goh"])
              V("dve", lambda e: e.tensor_scalar(out=goh[:, :], in0=goh[:, :], scalar1=BIG, scalar2=BIG,
                                                 op0=ALU.mult, op1=ALU.subtract), ["goh"], ["goh"])
              V("dve", lambda e: e.tensor_tensor(out=em[:, :, :], in0=L[:, 4:20].rearrange("p (a b) -> p a b", a=4),
                                                 in1=goh[:, :].unsqueeze(2).to_broadcast([128, 4, 4]), op=ALU.add),
                ["L", "goh"], ["em"])
              emf = em[:, :, :].rearrange("p a b -> p (a b)")
              V("dve", lambda e: e.max(out=top8[:, :], in_=emf), ["em"], ["top8"])
              V("dve", lambda e: e.tensor_tensor(out=sm[:, 4:5], in0=top8[:, 1:2], in1=top8[:, 0:1], op=ALU.subtract),
                ["top8", "sm"], ["sm"])
              V("act", lambda e: e.activation(out=sm[:, 5:6], in_=sm[:, 4:5], func=AF.Exp), ["sm"], ["sm"])
              V("dve", lambda e: e.tensor_scalar(out=sm[:, 6:7], in0=sm[:, 5:6], scalar1=1.0, scalar2=None, op0=ALU.add),
                ["sm"], ["sm"])
              V("dve", lambda e: e.reciprocal(out=sm[:, 6:7], in_=sm[:, 6:7]), ["sm"], ["sm"])
              V("dve", lambda e: e.tensor_tensor(out=sm[:, 7:8], in0=sm[:, 6:7], in1=sm[:, 3:4], op=ALU.mult), ["sm"], ["sm"])
              V("dve", lambda e: e.tensor_tensor(out=sm[:, 8:9], in0=sm[:, 7:8], in1=sm[:, 5:6], op=ALU.mult), ["sm"], ["sm"])
              V("dve", lambda e: e.tensor_scalar(out=c0[:, :], in0=emf, scalar1=top8[:, 0:1], scalar2=sm[:, 7:8],
                                                 op0=ALU.is_equal, op1=ALU.mult), ["em", "top8", "sm"], ["c0"])
              V("dve", lambda e: e.tensor_scalar(out=c1[:, :], in0=emf, scalar1=top8[:, 1:2], scalar2=sm[:, 8:9],
                                                 op0=ALU.is_equal, op1=ALU.mult), ["em", "top8", "sm"], ["c1"])
              V("dve", lambda e: e.tensor_tensor(out=comb[:, 0:16], in0=c0[:, :], in1=c1[:, :], op=ALU.add),
                ["c0", "c1"], ["comb"])
              ck(6)
              V("pe", lambda e: e.transpose(out=pcb[:, 128:256], in_=comb[:, :], identity=ident_f[:, :]),
                ["comb", "ident_f"], ["pcb"])
              V("act", lambda e: e.copy(out=combT[:, ti * 128:(ti + 1) * 128], in_=pcb[:, 128:256]),
                ["pcb"], [("combT", ti)])

          ck(7)
          NSUB = SG // 512
          units = [(ex, sub) for ex in range(16) for sub in range(NSUB)]

          def load_w(ex):
              wb = ex % 2
              stage_load(wgu[ex].rearrange("(kc p) n -> p kc n", p=128), None, wgu_b[wb][:, :, :], ("wgu_b", wb))
              stage_load(wd[ex].rearrange("(fc p) n -> p fc n", p=128), None, wd_b[wb][:, :, :], ("wd_b", wb))

          def GU(n, fc):
              ex, sub = units[n]
              wb = ex % 2
              tok = slice(sub * 512, (sub + 1) * 512)
              tkeys = [("h1T_b", sub * 4 + i) for i in range(4)]
              for j in (fc, 2 + fc):
                  for kc in range(8):
                      s.op("pe", lambda e: e.matmul(gu[j][:, :], lhsT=wgu_b[wb][:, kc, j * 128:(j + 1) * 128],
                                                    rhs=h1T_b[:, kc, tok], start=(kc == 0), stop=(kc == 7)),
                           reads=[("wgu_b", wb)] + tkeys, writes=[("gu", j)])

          def CB(n):
              ex, sub = units[n]
              tok = slice(sub * 512, (sub + 1) * 512)
              s.op("pe", lambda e: e.matmul(pcb[:, :], lhsT=sel_f[:, ex * 128:(ex + 1) * 128], rhs=combT[:, tok],
                                            start=True, stop=True),
                   reads=["sel_f"] + [("combT", sub * 4 + i) for i in range(4)], writes=["pcb"])
              s.op("act", lambda e: e.copy(out=cb2[n % 2][:, :], in_=pcb[:, :]), reads=["pcb"], writes=[("cb", n % 2)])

          def EW(n, fc):
              p = n % 2
              s.op("act", lambda e: e.activation(out=sg[fc][:, :], in_=gu[fc][:, :], func=AF.Silu),
                   reads=[("gu", fc)], writes=[("sg", fc)])
              s.op("dve", lambda e: e.tensor_tensor(out=tt_[fc][:, :], in0=gu[2 + fc][:, :], in1=cb2[p][:, :], op=ALU.mult),
                   reads=[("gu", 2 + fc), ("cb", p)], writes=[("tt", fc)])
              s.op("pool", lambda e: e.tensor_tensor(out=hid2[p][fc][:, :], in0=sg[fc][:, :], in1=tt_[fc][:, :], op=ALU.mult),
                   reads=[("sg", fc), ("tt", fc)], writes=[("hid", p, fc)])

          def DOWN(n):
              ex, sub = units[n]
              wb = ex % 2
              p = n % 2
              for t4 in range(4):
                  ti = sub * 4 + t4
                  for hh in range(2):
                      for fc in range(2):
                          s.op("pe", lambda e: e.matmul(py[hh][:, :], lhsT=hid2[p][fc][:, t4 * 128:(t4 + 1) * 128],
                                                        rhs=wd_b[wb][:, fc, hh * 512:(hh + 1) * 512],
                                                        start=(fc == 0), stop=(fc == 1)),
                               reads=[("hid", p, fc), ("wd_b", wb)], writes=[("py", hh)])
                      s.op("dve", lambda e: e.tensor_tensor(out=yacc[:, ti, hh * 512:(hh + 1) * 512],
                                                            in0=yacc[:, ti, hh * 512:(hh + 1) * 512],
                                                            in1=py[hh][:, :], op=ALU.add),
                           reads=[("yacc", ti), ("py", hh)], writes=[("yacc", ti)])

          load_w(0)
          GU(0, 0); CB(0); EW(0, 0); GU(0, 1); EW(0, 1)
          for n in range(len(units)):
              ex, sub = units[n]
              if sub == 0 and ex + 1 < 16:
                  load_w(ex + 1)
              if n + 1 < len(units):
                  GU(n + 1, 0); CB(n + 1); EW(n + 1, 0)
              DOWN(n)
              if n + 1 < len(units):
                  GU(n + 1, 1); EW(n + 1, 1)
          ck(8)
          for ti in range(TPS):
              gt = sgi * TPS + ti
              layer_norm(yacc[:, ti, :], ("yacc", ti), 2, h1[:, :], "h1")
              s.dma("sp", hout[gt * 128:(gt + 1) * 128, :], h1[:, :], reads=["h1"], writes=[("hout", gt)])
              out_keys.append(("hout", gt))
    except StopBuild:
        pass
    s.finish(out_keys)
    print("post ninst", s.ninst)
    s.close()
    return nc


def post_consts():
    ident = np.eye(128, dtype=np.float32)
    sel = np.zeros((128, 16 * 128), np.float32)
    for e in range(16):
        sel[e, e * 128:(e + 1) * 128] = 1.0
    return {"ident": ident, "sel": sel}


def build_cmp():
    nc = bass.Bass("TRN2", target_bir_lowering=False)

    def din(name, shape, dt=F32):
        return nc.dram_tensor(name, shape, dt, kind="ExternalInput").ap()
    xT = din("xT", [2048, 1024], BF16)
    pos = din("pos", [2048])
    w1 = din("w1", [2048, 256])
    b1 = din("b1", [256])
    w2 = din("w2", [256, 64])
    b2 = din("b2", [64])
    outT = nc.dram_tensor("outT", [64, 1024], BF16, kind="ExternalOutput").ap()
    s = Sched(nc)
    x_in = s.sb("x_in", [128, 16, 1024], BF16)
    x_b = s.sb("x_b", [128, 16, 1024], BF16)
    pos_sb = s.sb("pos_sb", [128, 16], F32)
    w1_f = s.sb("w1_f", [128, 16, 256], F32)
    w1_b = s.sb("w1_b", [128, 16, 256], BF16)
    b1_sb = s.sb("b1_sb", [128, 2], F32)
    w2_f = s.sb("w2_f", [128, 2, 64], F32)
    w2_b = s.sb("w2_b", [128, 2, 64], BF16)
    b2_sb = s.sb("b2_sb", [64, 1], F32)
    xh = s.sb("xh", [128, 512], F32)
    x2 = s.sb("x2", [128, 512], F32)
    sg = s.sb("sg", [128, 512], F32)
    hid = s.sb("hid", [128, 2, 1024], BF16)
    o_sb = s.sb("o_sb", [64, 1024], BF16)
    ph = [s.ps("ph%d" % i, [128, 512], key=("ph", i)) for i in range(2)]
    po = s.ps("po", [128, 512], key="po")
    s.dma("sp", x_in[:, :, :], xT.rearrange("(kc p) n -> p kc n", p=128), writes=["x_in"])
    s.dma("sp", pos_sb[:, :], pos.rearrange("(kc p) -> p kc", p=128), writes=["pos_sb"], allow_slow_non_contiguous=True)
    s.dma("sp", w1_f[:, :, :], w1.rearrange("(kc p) n -> p kc n", p=128), writes=["w1_f"])
    s.dma("sp", b1_sb[:, :], b1.rearrange("(fc p) -> p fc", p=128), writes=["b1_sb"], allow_slow_non_contiguous=True)
    s.dma("sp", w2_f[:, :, :], w2.rearrange("(fc p) n -> p fc n", p=128), writes=["w2_f"])
    s.dma("sp", b2_sb[:, :], b2.rearrange("(p o) -> p o", o=1), writes=["b2_sb"])
    s.op("pool", lambda e: e.tensor_copy(out=w1_b[:, :, :], in_=w1_f[:, :, :]), reads=["w1_f"], writes=["w1_b"])
    s.op("pool", lambda e: e.tensor_copy(out=w2_b[:, :, :], in_=w2_f[:, :, :]), reads=["w2_f"], writes=["w2_b"])
    for kc in range(16):
        s.op("dve", lambda e: e.tensor_scalar(out=x_b[:, kc, :], in0=x_in[:, kc, :], scalar1=pos_sb[:, kc:kc + 1], scalar2=None,
                                              op0=ALU.add), reads=["x_in", "pos_sb"], writes=[("x_b", kc)])
    xkeys = [("x_b", kc) for kc in range(16)]
    k = 0
    for nch in range(2):
        ns = slice(nch * 512, (nch + 1) * 512)
        for fc in range(2):
            p = ph[k % 2]
            pk = ("ph", k % 2)
            k += 1
            for kc in range(16):
                s.op("pe", lambda e: e.matmul(p[:, :], lhsT=w1_b[:, kc, fc * 128:(fc + 1) * 128], rhs=x_b[:, kc, ns],
                                              start=(kc == 0), stop=(kc == 15)), reads=["w1_b"] + xkeys, writes=[pk])
            s.op("dve", lambda e: e.tensor_scalar(out=xh[:, :], in0=p[:, :], scalar1=b1_sb[:, fc:fc + 1], scalar2=None, op0=ALU.add),
                 reads=[pk, "b1_sb"], writes=["xh"])
            s.op("pool", lambda e: e.tensor_tensor(out=x2[:, :], in0=xh[:, :], in1=xh[:, :], op=ALU.mult), reads=["xh"], writes=["x2"])
            s.op("dve", lambda e: e.tensor_scalar(out=x2[:, :], in0=x2[:, :], scalar1=0.044715, scalar2=1.0, op0=ALU.mult, op1=ALU.add),
                 reads=["x2"], writes=["x2"])
            s.op("pool", lambda e: e.tensor_tensor(out=x2[:, :], in0=x2[:, :], in1=xh[:, :], op=ALU.mult), reads=["x2", "xh"], writes=["x2"])
            s.op("act", lambda e: e.activation(out=sg[:, :], in_=x2[:, :], func=AF.Sigmoid, scale=1.5957691216057308),
                 reads=["x2"], writes=["sg"])
            s.op("dve", lambda e: e.tensor_tensor(out=hid[:, fc, ns], in0=xh[:, :], in1=sg[:, :], op=ALU.mult),
                 reads=["xh", "sg"], writes=[("hid", fc, nch)])
        for fc in range(2):
            s.op("pe", lambda e: e.matmul(po[0:64, :], lhsT=w2_b[:, fc, :], rhs=hid[:, fc, ns], start=(fc == 0), stop=(fc == 1)),
                 reads=["w2_b", ("hid", fc, nch)], writes=["po"])
        s.op("dve", lambda e: e.tensor_scalar(out=o_sb[:, ns], in0=po[0:64, :], scalar1=b2_sb[:, 0:1], scalar2=None, op0=ALU.add),
             reads=["po", "b2_sb"], writes=[("o_sb", nch)])
    s.dma("sp", outT, o_sb[:, :], reads=[("o_sb", 0), ("o_sb", 1)], writes=["outT"])
    s.finish(["outT"])
    s.close()
    return nc


NEG = -1.0e30
MNEG = -240000.0


def nsa_tables(S, heads4):
    slopes = (2.0 ** (-8.0 * np.arange(1, 17) / 16)).astype(np.float64)
    sl4 = slopes[list(heads4)]
    kl = np.arange(128)[:, None].astype(np.float64)
    ql = np.arange(128)[None, :].astype(np.float64)
    t = {}
    tabW = np.zeros((5, 128, 2, 128), np.float32)
    for dlt in range(5):
        dist = ql - kl + 128 * dlt
        valid = (dist >= 0) & (dist < 512)
        for h in range(2):
            tabW[dlt, :, h, :] = np.where(valid, -sl4[h] * dist, NEG)
    t["tabW"] = tabW
    tabS = np.zeros((2, 128, 2, 128), np.float32)
    for h in range(2):
        tabS[1, :, h, :] = -sl4[h] * (ql - kl)
        tabS[0, :, h, :] = np.where(ql - kl >= 0, -sl4[h] * (ql - kl), NEG)
    t["tabS"] = tabS
    cst = np.zeros((128, 4, 128), np.float32)
    for h in range(4):
        cst[:, h, :] = (-sl4[h] * 128.0 * np.arange(128))[None, :]
    t["cst"] = cst
    tabC = np.zeros((128, 4, 128), np.float32)
    for h in range(4):
        tabC[:, h, :] = -sl4[h] * (ql - 16 * kl - 31)
    t["tabC"] = tabC
    maskC = np.zeros((17, 128, 128), np.float32)
    for dlt in range(17):
        maskC[dlt] = np.where(128 * dlt + ql - 16 * kl - 31 >= 0, 0.0, MNEG)
    t["maskC"] = maskC
    NQB = S // 128
    nblk = S // 64
    keep = np.ones((NQB, 128, nblk), np.float32)
    force = np.zeros((NQB, 128, nblk), np.float32)
    n = np.arange(nblk)[None, :]
    for i in range(NQB):
        cur = (2 * i + (np.arange(128) >= 64).astype(np.int64))[:, None]
        forced = (n == 0) | (n == cur) | (n == cur - 1)
        fut = n > cur
        keep[i] = np.where(forced | fut, 0.0, 1.0)
        force[i] = np.where(fut, -1.0e4, np.where(forced, 1.0e4, 0.0))
    t["keep"] = keep
    t["force"] = force
    t["identb"] = np.eye(128, dtype=np.float32)
    oh = np.zeros((64, S), np.float32)
    for j in range(S // 128):
        for k in range(128):
            oh[2 * (j % 16) + k // 64, j * 128 + k] = 1.0
        oh[32:35, j * 128:(j + 1) * 128] = 1.0
        oh[35:38, j * 128:(j + 1) * 128] = float(j)
    t["oh"] = oh.astype(ml_dtypes.bfloat16)
    bf = ml_dtypes.bfloat16
    def split3(x):
        x = np.asarray(x, np.float64)
        h1 = x.astype(np.float32).astype(bf).astype(np.float64)
        h2 = (x - h1).astype(np.float32).astype(bf).astype(np.float64)
        h3 = (x - h1 - h2).astype(np.float32).astype(bf).astype(np.float64)
        return [h1, h2, h3]
    augq = np.zeros((NQB, 6, 2), np.float64)
    for h in range(2):
        A = -sl4[h] * 1024.0 * np.arange(NQB)
        Bv = np.full(NQB, sl4[h] * 1024.0)
        for r, comp in enumerate(split3(A) + split3(Bv)):
            augq[:, r, h] = comp
    t["augq"] = augq.astype(np.float32).astype(bf)
    return t


def build_nsa(S):
    nc = bass.Bass("TRN2", target_bir_lowering=False)
    NQB = S // 128
    NCH = S // 128
    NBLK = S // 64
    NHALF = (NBLK + 127) // 128
    NCMP = S // 16
    NCC = (NCMP + 127) // 128
    CW = min(128, NCMP)

    def din(name, shape, dt=F32):
        return nc.dram_tensor(name, shape, dt, kind="ExternalInput").ap()

    qT4 = din("qT4", [NQB, 64, 4, 128], BF16)
    kcT = din("kcT", [64, NCC * 128], BF16)
    vc = din("vc", [NCC * 128, 64], BF16)
    ksT = din("ksT", [64, S], BF16)
    vs = din("vs", [S, 64], BF16)
    kwT = din("kwT", [64, S], BF16)
    vw = din("vw", [S, 64], BF16)
    tabW_d = din("tabW", [5, 128, 2, 128])
    tabS_d = din("tabS", [2, 128, 2, 128])
    cst_d = din("cst", [128, 4, 128])
    tabC_d = din("tabC", [128, 4, 128])
    maskC_d = din("maskC", [17, 128, 128])
    keep_d = din("keep", [NQB, 128, NBLK])
    force_d = din("force", [NQB, 128, NBLK])
    ident_d = din("identb", [128, 128])
    oh_d = din("oh", [64, S], BF16)
    qaug_d = din("qaugsrc", [NQB, 128, 2, 128], BF16)
    o3s = nc.dram_tensor("o3s", [NQB, 65, 2, 128], F32, kind="ExternalOutput").ap()
    o3 = nc.dram_tensor("o3", [NQB, 128, 3, 2, 65], F32, kind="ExternalOutput").ap()

    s = Sched(nc)
    stg = s.sb("stg", [128, 17 * 128], F32)
    tabW = s.sb("tabW_s", [128, 5, 256], F32)
    tabS = s.sb("tabS_s", [128, 2, 256], F32)
    cst = s.sb("cst_s", [128, 4, 128], F32)
    tabC = s.sb("tabC_s", [128, 512], F32)
    maskC = s.sb("maskC_s", [128, 17, 128], BF16)
    identb = s.sb("identb_s", [128, 128], BF16)
    kc_sb = s.sb("kc_sb", [64, NCC * 128], BF16)
    vc_sb = s.sb("vc_sb", [128, NCC, 65], BF16)
    ks_sb = s.sb("ks_sb", [128, S], BF16)
    vs_sb = s.sb("vs_sb", [128, NCH, 65], BF16)
    kw_sb = s.sb("kw_sb", [64, S], BF16)
    vw_sb = s.sb("vw_sb", [128, NCH, 65], BF16)
    s.dma("sp", tabW[:, :, :], tabW_d.rearrange("v p h q -> p v (h q)"), writes=["tabW"])
    s.dma("sp", tabS[:, :, :], tabS_d.rearrange("v p h q -> p v (h q)"), writes=["tabS"])
    s.dma("sp", cst[:, :, :], cst_d, writes=["cst"])
    s.dma("sp", tabC[:, :], tabC_d.rearrange("p h q -> p (h q)"), writes=["tabC"])
    s.dma("sp", stg[:, 0:17 * 128].rearrange("p (v q) -> p v q", v=17), maskC_d.rearrange("v p q -> p v q"), writes=["stg"])
    s.op("dve", lambda e: e.tensor_copy(out=maskC[:, :, :], in_=stg[:, 0:17 * 128].rearrange("p (v q) -> p v q", v=17)),
         reads=["stg"], writes=["maskC"])
    s.dma("sp", stg[:, 0:128], ident_d, writes=["stg"])
    s.op("dve", lambda e: e.tensor_copy(out=identb[:, :], in_=stg[:, 0:128]), reads=["stg"], writes=["identb"])
    s.dma("sp", kc_sb[:, :], kcT, writes=["kc_sb"])
    s.dma("sp", ks_sb[0:64, :], ksT, writes=["ks_sb"])
    s.dma("sp", ks_sb[64:128, :], oh_d, writes=["ks_sb"])
    s.dma("sp", kw_sb[:, :], kwT, writes=["kw_sb"])
    for (vsb, vd, nm, n_) in ((vc_sb, vc, "vc_sb", NCC), (vs_sb, vs, "vs_sb", NCH), (vw_sb, vw, "vw_sb", NCH)):
        s.op("pool", lambda e: e.memset(vsb[:, :, 64:65], 1.0), writes=[nm])
        s.dma("sp", vsb[:, :, 0:64], vd.rearrange("(c p) d -> p c d", p=128), writes=[nm])

    NB = 5
    LA = 3
    q_sb = [s.sb("q_sb%d" % i, [64, 4, 128], BF16) for i in range(2)]
    keep_sb = [s.sb("keep_sb%d" % i, [128, NBLK], F32) for i in range(2)]
    force_sb = [s.sb("force_sb%d" % i, [128, NBLK], F32) for i in range(2)]
    tmp = [s.sb("tmp%d" % i, [128, 512], F32) for i in range(NB)]
    pc = s.sb("pc", [128, NCC, 512], BF16)
    pw = s.sb("pw", [128, 5, 256], BF16)
    psl = [s.sb("psl%d" % i, [128, 256], BF16) for i in range(NB)]
    oc_sb = s.sb("oc_sb", [128, 4, 65], F32)
    rl = s.sb("rl", [128, 4], F32)
    imp = s.sb("imp", [128, NCC * 128], F32)
    sc = s.sb("sc", [128, NBLK], F32)
    sc2 = s.sb("sc2", [128, NBLK], F32)
    t8a = s.sb("t8a", [128, 8], F32)
    t8b = s.sb("t8b", [128, 8], F32)
    NV = (NCH + 15) // 16
    selb = s.sb("selb", [128, max(64 + NBLK, 32 * (NV - 1) + 128)], BF16)
    qaug = [s.sb("qaug%d" % i, [128, NV, 2, 128], BF16) for i in range(2)]
    osT = [s.sb("osT%d" % i, [65, 2, 128], F32) for i in range(2)]
    o_sb = [s.sb("o_sb%d" % i, [128, 3, 2, 65], F32) for i in range(2)]
    ps_s = [s.ps("ps_s%d" % i, [128, 512], key=("ps_s", i)) for i in range(NB)]
    ps_os = [s.ps("ps_os%d" % i, [128, 512], key=("ps_os", i)) for i in range(1)]
    ps_o = s.ps("ps_o", [128, 4, 128], key="ps_o")
    ps_t = s.ps("ps_t", [128, 1024], BF16, key="ps_t")
    s.op("dve", lambda e: e.memset(selb[:, :], 0.0), writes=["selb"])
    for i_ in range(2):
        s.op("pool", lambda e: e.memset(o_sb[i_][:, :, :, :], 0.0), writes=[("o_sb", i_, 0), ("o_sb", i_, 2)])

    sidx = [0]
    okeys = []

    def nextbuf():
        k = sidx[0] % NB
        sidx[0] += 1
        return k

    def sel_steps(i):
        b = i % 2
        steps = []

        def ld():
            s.dma("sp", q_sb[b][:, :, :], qT4[i], writes=[("q_sb", b)])
            s.dma("sp", qaug[b][:, :, :, :], qaug_d[i].unsqueeze(1).to_broadcast([128, NV, 2, 128]), writes=[("qaug", b)])
            s.dma("sp", keep_sb[b][:, :], keep_d[i], writes=[("keep", b)])
            s.dma("sp", force_sb[b][:, :], force_d[i], writes=[("force", b)])
        steps.append(ld)
        mlist = [m for m in range(NCC) if 16 * CW * m + 31 <= 128 * i + 127]

        def cmp_chunk(m):
            k = nextbuf()
            dl = i - 16 * m
            if dl <= 16:
                s.op("pe", lambda e: e.matmul(ps_s[k][:, :], lhsT=identb[:, :],
                                              rhs=maskC[:, dl, :].unsqueeze(1).to_broadcast([128, 4, 128]),
                                              start=True, stop=False),
                     reads=["identb", "maskC"], writes=[("ps_s", k)])
            for h in range(4):
                s.op("pe", lambda e: e.matmul(ps_s[k][:, h * 128:(h + 1) * 128], lhsT=kc_sb[:, m * 128:(m + 1) * 128],
                                              rhs=q_sb[b][:, h, :], start=(dl > 16), stop=(dl > 16 or h == 3)),
                     reads=["kc_sb", ("q_sb", b)], writes=[("ps_s", k)])
            s.op("dve", lambda e: e.scalar_tensor_tensor(out=tmp[k][:, :], in0=ps_s[k][:, :], scalar=0.125,
                                                         in1=tabC[:, :], op0=ALU.mult, op1=ALU.add),
                 reads=[("ps_s", k), "tabC"], writes=[("tmp", k)])
            for h in range(4):
                s.op("act", lambda e: e.activation(out=pc[:, m, h * 128:(h + 1) * 128], in_=tmp[k][:, h * 128:(h + 1) * 128],
                                                   func=AF.Exp, bias=cst[:, h, dl:dl + 1], scale=1.0),
                     reads=[("tmp", k), "cst"], writes=[("pc", m, h)])
        for m in mlist:
            steps.append(lambda m=m: cmp_chunk(m))

        def cmp_pv():
            for h in range(4):
                for mi, m in enumerate(mlist):
                    s.op("pe", lambda e: e.matmul(ps_o[:, h, 0:65], lhsT=pc[:, m, h * 128:(h + 1) * 128], rhs=vc_sb[:, m, :],
                                                  start=(mi == 0), stop=(mi == len(mlist) - 1)),
                         reads=[("pc", m, h), "vc_sb"], writes=["ps_o"])
            s.op("act", lambda e: e.copy(out=oc_sb[:, :, :], in_=ps_o[:, :, 0:65]), reads=["ps_o"], writes=["oc_sb"])
            s.op("pool", lambda e: e.tensor_copy(out=o_sb[b][:, 0, :, :], in_=oc_sb[:, 0:2, :]), reads=["oc_sb"], writes=[("o_sb", b, 0)])
            s.op("dve", lambda e: e.tensor_scalar(out=rl[:, :], in0=oc_sb[:, :, 64], scalar1=1.0e-30, scalar2=None, op0=ALU.max),
                 reads=["oc_sb"], writes=["rl"])
            s.op("dve", lambda e: e.reciprocal(out=rl[:, :], in_=rl[:, :]), reads=["rl"], writes=["rl"])
            s.op("pool", lambda e: e.memset(imp[:, :], 0.0), writes=["imp"])
        steps.append(cmp_pv)

        def imp_head(h):
            for m in mlist:
                s.op("pe", lambda e: e.transpose(out=ps_t[:, m * 128:(m + 1) * 128], in_=pc[:, m, h * 128:(h + 1) * 128],
                                                 identity=identb[:, :]),
                     reads=[("pc", m, h), "identb"], writes=["ps_t"])
            w = len(mlist) * 128
            s.op("dve", lambda e: e.scalar_tensor_tensor(out=imp[:, 0:w], in0=ps_t[:, 0:w], scalar=rl[:, h:h + 1],
                                                         in1=imp[:, 0:w], op0=ALU.mult, op1=ALU.add),
                 reads=["ps_t", "rl", "imp"], writes=["imp"])
        for h in range(4):
            steps.append(lambda h=h: imp_head(h))
        A = imp[:, :].rearrange("p (n f) -> p n f", f=4)
        nb = NBLK

        def score1():
            s.op("dve", lambda e: e.tensor_tensor(out=sc[:, :], in0=A[:, 0:nb, 0], in1=A[:, 0:nb, 1], op=ALU.add), reads=["imp"], writes=["sc"])
            s.op("dve", lambda e: e.tensor_tensor(out=sc[:, :], in0=sc[:, :], in1=A[:, 0:nb, 2], op=ALU.add), reads=["imp", "sc"], writes=["sc"])
            s.op("dve", lambda e: e.scalar_tensor_tensor(out=sc[:, :], in0=sc[:, :], scalar=2.0, in1=A[:, 0:nb, 3],
                                                         op0=ALU.mult, op1=ALU.add), reads=["imp", "sc"], writes=["sc"])
            s.op("dve", lambda e: e.tensor_tensor(out=sc[:, 1:nb], in0=sc[:, 1:nb], in1=A[:, 0:nb - 1, 3], op=ALU.add),
                 reads=["imp", "sc"], writes=["sc"])

        def score2():
            s.op("dve", lambda e: e.tensor_tensor(out=sc[:, :], in0=sc[:, :], in1=keep_sb[b][:, :], op=ALU.mult),
                 reads=["sc", ("keep", b)], writes=["sc"])
            s.op("dve", lambda e: e.tensor_tensor(out=sc[:, :], in0=sc[:, :], in1=force_sb[b][:, :], op=ALU.add),
                 reads=["sc", ("force", b)], writes=["sc"])
            s.op("dve", lambda e: e.max(out=t8a[:, :], in_=sc[:, :]), reads=["sc"], writes=["t8a"])

        def score3():
            s.op("dve", lambda e: e.match_replace(out=sc2[:, :], in_to_replace=t8a[:, :], in_values=sc[:, :], imm_value=-3.0e4),
                 reads=["sc", "t8a"], writes=["sc2"])
            s.op("dve", lambda e: e.max(out=t8b[:, :], in_=sc2[:, :]), reads=["sc2"], writes=["t8b"])
            s.op("dve", lambda e: e.tensor_scalar(out=sc2[:, :], in0=sc[:, :], scalar1=t8b[:, 7:8], scalar2=-MNEG,
                                                  op0=ALU.is_ge, op1=ALU.mult), reads=["sc", "t8b"], writes=["sc2"])
            s.op("dve", lambda e: e.tensor_scalar(out=selb[:, 64:64 + nb], in0=sc2[:, :], scalar1=MNEG, scalar2=None, op0=ALU.add),
                 reads=["sc2"], writes=["selb"])

        def score4():
            for v_ in range(NV):
                s.op("pe", lambda e: e.transpose(out=ps_t[:, v_ * 128:(v_ + 1) * 128], in_=selb[:, 32 * v_:32 * v_ + 128],
                                                 identity=identb[:, :]), reads=["selb", "identb"], writes=["ps_t"])
            s.op("act", lambda e: e.copy(out=qaug[b][64:96, :, :, :],
                                         in_=ps_t[64:96, 0:NV * 128].rearrange("p (v q) -> p v q", v=NV).unsqueeze(2).to_broadcast([32, NV, 2, 128])),
                 reads=["ps_t"], writes=[("qaug", b)])
        steps += [score1, score2, score3, score4]
        return steps

    def s_stage(i, j, k):
        b = i % 2
        v_ = j // 16
        s.op("pe", lambda e: e.matmul(ps_s[k][:, 0:256], lhsT=ks_sb[:, j * 128:(j + 1) * 128],
                                      rhs=qaug[b][:, v_, :, :].rearrange("p a b -> p (a b)"), start=True, stop=True),
             reads=["ks_sb", ("qaug", b)], writes=[("ps_s", k)])
        s.op("dve", lambda e: e.scalar_tensor_tensor(out=tmp[k][:, 0:256], in0=ps_s[k][:, 0:256], scalar=0.125,
                                                     in1=tabS[:, 0 if j == i else 1, :], op0=ALU.mult, op1=ALU.add),
             reads=[("ps_s", k), "tabS"], writes=[("tmp", k)])
        s.op("act", lambda e: e.activation(out=psl[k][:, 0:256], in_=tmp[k][:, 0:256], func=AF.Exp),
             reads=[("tmp", k)], writes=[("psl", k, 0)])

    def pv_stage(i, j, k):
        s.op("pe", lambda e: e.matmul(ps_os[0][0:65, 0:256], lhsT=vs_sb[:, j, :], rhs=psl[k][:, 0:256],
                                      start=(j == 0), stop=(j == i)),
             reads=[("psl", k, 0), "vs_sb"], writes=[("ps_os", 0)])

    def window(i):
        b = i % 2
        wl = [j for j in range(i - 4, i + 1) if j >= 0]
        for wi, j in enumerate(wl):
            k = nextbuf()
            for h in range(2):
                s.op("pe", lambda e: e.matmul(ps_s[k][:, h * 128:(h + 1) * 128], lhsT=kw_sb[:, j * 128:(j + 1) * 128],
                                              rhs=q_sb[b][:, h, :], start=True, stop=True),
                     reads=["kw_sb", ("q_sb", b)], writes=[("ps_s", k)])
            s.op("dve", lambda e: e.scalar_tensor_tensor(out=tmp[k][:, 0:256], in0=ps_s[k][:, 0:256], scalar=0.125,
                                                         in1=tabW[:, i - j, :], op0=ALU.mult, op1=ALU.add),
                 reads=[("ps_s", k), "tabW"], writes=[("tmp", k)])
            s.op("act", lambda e: e.activation(out=pw[:, wi, :], in_=tmp[k][:, 0:256], func=AF.Exp),
                 reads=[("tmp", k)], writes=[("pw", wi)])
        for h in range(2):
            for wi, j in enumerate(wl):
                s.op("pe", lambda e: e.matmul(ps_o[:, h, 0:65], lhsT=pw[:, wi, h * 128:(h + 1) * 128], rhs=vw_sb[:, j, :],
                                              start=(wi == 0), stop=(wi == len(wl) - 1)),
                     reads=[("pw", wi), "vw_sb"], writes=["ps_o"])
        s.op("act", lambda e: e.copy(out=o_sb[b][:, 2, :, :], in_=ps_o[:, 0:2, 0:65]), reads=["ps_o"], writes=[("o_sb", b, 2)])

    for st in sel_steps(0):
        st()
    for i in range(NQB):
        b = i % 2
        nxt = sel_steps(i + 1) if i + 1 < NQB else []
        n = i + 1
        bufs = {}
        for step in range(n + LA):
            if step < n:
                bufs[step] = nextbuf()
                s_stage(i, step, bufs[step])
            if step - LA >= 0:
                pv_stage(i, step - LA, bufs[step - LA])
            if nxt and step % 2 == 1:
                nxt.pop(0)()
        s.op("act", lambda e: e.copy(out=osT[b][:, :, :].rearrange("p a b -> p (a b)"), in_=ps_os[0][0:65, 0:256]),
             reads=[("ps_os", 0)], writes=[("osT", b)])
        s.dma("sp", o3s[i], osT[b][:, :, :], reads=[("osT", b)], writes=[("o3s", i)])
        okeys.append(("o3s", i))
        window(i)
        while nxt:
            nxt.pop(0)()
        s.dma("sp", o3[i], o_sb[b][:, :, :, :], reads=[("o_sb", b, 0), ("o_sb", b, 2)], writes=[("o3", i)])
        okeys.append(("o3", i))
    s.finish(okeys)
    print("nsa ninst", s.ninst)
    s.close()
    return nc


import numpy as np

NCORES = 8
S = 16384
TPC = S // NCORES
_cache = {}


def _get(key, fn):
    if key not in _cache:
        _cache[key] = fn()
    return _cache[key]


def run_proj(h, w, out_bf16=True):
    N = w.shape[1]
    nc = _get(("proj", N, out_bf16), lambda: build_proj(TPC // 128, N, out_bf16))
    in_maps = [{"xT": np.ascontiguousarray(h[c * TPC:(c + 1) * TPC].T), "w": w} for c in range(NCORES)]
    res = run_bass_kernel_spmd(nc, in_maps, core_ids=list(range(NCORES)))
    return np.concatenate([np.asarray(r["y"]) for r in res.results], axis=0)


def run_atta(qkv):
    qTs, kTs, vs, meta = atta_host_prep(qkv)
    upc = [len(m) // NCORES for m in meta]
    units = []
    for g in range(3):
        for p in range(upc[g]):
            units.append(3 * g + (0 if p == 0 else (2 if p == 8 else 1)))
    nc = _get(("atta",), lambda: build_atta(units, 9))
    tab = atta_bias_table()
    in_maps = []
    for c in range(NCORES):
        tabs = np.zeros((9, 128, 2, 16, 128), np.float32)
        for g in range(3):
            for slot, p in ((0, 0), (1, 1), (2, 8)):
                t = tab[g].copy()
                if meta[g][c * upc[g] + p][1]:
                    t[:, 0] = -1.0e30
                tabs[3 * g + slot] = t
        sl = [slice(c * upc[g], (c + 1) * upc[g]) for g in range(3)]
        in_maps.append({"qT": np.concatenate([qTs[g][sl[g]] for g in range(3)]),
                        "kT": np.concatenate([kTs[g][sl[g]] for g in range(3)]),
                        "v": np.concatenate([vs[g][sl[g]] for g in range(3)]),
                        "tab": tabs})
    res = run_bass_kernel_spmd(nc, in_maps, core_ids=list(range(NCORES)))
    og = []
    for g in range(3):
        off = sum(upc[:g])
        og.append(np.concatenate([np.asarray(r["o"])[off:off + upc[g]] for r in res.results], axis=0))
    return atta_host_post(og, S)


def run_post(h, o_or_og, modeA, wout, lnp, wr, br, wgu, wd):
    nc = _get(("post", modeA), lambda: build_post(TPC, modeA))
    consts = post_consts()
    in_maps = []
    for c in range(NCORES):
        rows = slice(c * TPC, (c + 1) * TPC)
        m = {"h": np.ascontiguousarray(h[rows]), "wout": wout, "lnp": lnp, "wr": wr, "br": br, "wgu": wgu, "wd": wd}
        m.update(consts)
        if modeA:
            m["og"] = np.ascontiguousarray(o_or_og[:, rows])
        else:
            m["o"] = np.ascontiguousarray(o_or_og[rows])
        in_maps.append(m)
    res = run_bass_kernel_spmd(nc, in_maps, core_ids=list(range(NCORES)))
    return np.concatenate([np.asarray(r["hout"]) for r in res.results], axis=0)


def layer_a(h, l, inp):
    qkv = run_proj(h, np.ascontiguousarray(inp["a_w_in"][l]))
    og = run_atta(qkv)
    lnp = np.stack([inp["ln_mix_g"][l], inp["ln_mix_b"][l], inp["ln_ffn_g"][l], inp["ln_ffn_b"][l]])
    return run_post(h, og, 1, np.ascontiguousarray(inp["a_w_out"][l]), lnp,
                    np.ascontiguousarray(inp["moe_w_router"][l]), np.ascontiguousarray(inp["moe_b_router"][l]),
                    np.ascontiguousarray(inp["moe_w_gate_up"][l]), np.ascontiguousarray(inp["moe_w_down"][l]))


BF = ml_dtypes.bfloat16


def run_proj_b(h, w, bias):
    N = w.shape[1]
    nc = _get(("projb", N), lambda: build_proj(TPC // 128, N, True, True))
    in_maps = [{"xT": np.ascontiguousarray(h[c * TPC:(c + 1) * TPC].T), "w": w, "bias": bias} for c in range(NCORES)]
    res = run_bass_kernel_spmd(nc, in_maps, core_ids=list(range(NCORES)))
    return np.concatenate([np.asarray(r["y"]) for r in res.results], axis=0)


def run_cmp(kv, inp):
    kv6 = kv.reshape(S, 6, 4, 64)
    nc = _get(("cmp",), build_cmp)
    in_maps = []
    nblk = S // 16
    for j in range(2):
        for g in range(4):
            u = kv6[:, j, g, :]
            c = u.reshape(nblk, 16, 64)
            blocks = np.concatenate([c[:-1], c[1:]], axis=1)
            X = np.zeros((nblk, 2048), kv.dtype)
            X[:nblk - 1] = blocks.reshape(nblk - 1, 2048)
            in_maps.append({"xT": np.ascontiguousarray(X.T), "pos": np.ascontiguousarray(inp["b_cmp_pos"][j].reshape(2048)),
                            "w1": np.ascontiguousarray(inp["b_cmp_w1"][j]), "b1": np.ascontiguousarray(inp["b_cmp_b1"][j]),
                            "w2": np.ascontiguousarray(inp["b_cmp_w2"][j]), "b2": np.ascontiguousarray(inp["b_cmp_b2"][j])})
    res = run_bass_kernel_spmd(nc, in_maps, core_ids=list(range(NCORES)))
    outs = [np.asarray(r["outT"]) for r in res.results]
    kcT = [outs[g] for g in range(4)]
    vc = [np.ascontiguousarray(outs[4 + g].T) for g in range(4)]
    return kcT, vc


def run_nsa(proj, kv, kcT, vc):
    nc = _get(("nsa",), lambda: build_nsa(S))
    kv6 = kv.reshape(S, 6, 4, 64)
    q = proj[:, :1024].reshape(S // 128, 128, 16, 64)
    in_maps = []
    for c in range(NCORES):
        g, half = c // 2, c % 2
        own = [4 * g + 2 * half, 4 * g + 2 * half + 1]
        oth = [4 * g + 2 * (1 - half), 4 * g + 2 * (1 - half) + 1]
        heads4 = own + oth
        m = nsa_tables(S, heads4)
        m["qT4"] = np.ascontiguousarray(q[:, :, heads4, :].transpose(0, 3, 2, 1))
        qsrc = np.zeros((S // 128, 128, 2, 128), BF)
        qsrc[:, 0:64] = m["qT4"][:, :, 0:2, :]
        qsrc[:, 96:102] = m.pop("augq")[:, :, :, None]
        m["qaugsrc"] = qsrc
        m["kcT"] = kcT[g]
        m["vc"] = vc[g]
        m["ksT"] = np.ascontiguousarray(kv6[:, 2, g, :].T)
        m["vs"] = np.ascontiguousarray(kv6[:, 3, g, :])
        m["kwT"] = np.ascontiguousarray(kv6[:, 4, g, :].T)
        m["vw"] = np.ascontiguousarray(kv6[:, 5, g, :])
        in_maps.append(m)
    res = run_bass_kernel_spmd(nc, in_maps, core_ids=list(range(NCORES)))
    og = np.zeros((3, S, 16, 65), np.float32)
    for c in range(NCORES):
        g, half = c // 2, c % 2
        o3 = np.asarray(res.results[c]["o3"]).reshape(S, 3, 2, 65).copy()
        o3[:, 1] = np.asarray(res.results[c]["o3s"]).transpose(0, 3, 2, 1).reshape(S, 2, 65)
        for hh in range(2):
            og[:, :, 4 * g + 2 * half + hh, :] = o3[:, :, hh, :].transpose(1, 0, 2)
    return og.reshape(3, S, 1040)


def layer_b(h, i, inp, shared):
    l = 2 + i
    proj = run_proj_b(h, np.ascontiguousarray(inp["b_w_q"][i]), np.ascontiguousarray(inp["b_b_q"][i]))
    kv, kcT, vc = shared
    og = run_nsa(proj, kv, kcT, vc)
    lnp = np.stack([inp["ln_mix_g"][l], inp["ln_mix_b"][l], inp["ln_ffn_g"][l], inp["ln_ffn_b"][l]])
    gl = np.ascontiguousarray(proj[:, 1024:1072])
    nc = _get(("post", 0), lambda: build_post(TPC, 0))
    consts = post_consts()
    in_maps = []
    for c in range(NCORES):
        rows = slice(c * TPC, (c + 1) * TPC)
        m = {"h": np.ascontiguousarray(h[rows]), "wout": np.ascontiguousarray(inp["b_w_out"][i]), "lnp": lnp,
             "wr": np.ascontiguousarray(inp["moe_w_router"][l]), "br": np.ascontiguousarray(inp["moe_b_router"][l]),
             "wgu": np.ascontiguousarray(inp["moe_w_gate_up"][l]), "wd": np.ascontiguousarray(inp["moe_w_down"][l]),
             "og": np.ascontiguousarray(og[:, rows]), "gl": np.ascontiguousarray(gl[rows])}
        m.update(consts)
        in_maps.append(m)
    res = run_bass_kernel_spmd(nc, in_maps, core_ids=list(range(NCORES)))
    return np.concatenate([np.asarray(r["hout"]) for r in res.results], axis=0)


def forward(inp):
    h = np.ascontiguousarray(inp["x"][0])
    for l in range(2):
        h = layer_a(h, l, inp)
    kv = run_proj(h, np.ascontiguousarray(inp["b_w_kv"]))
    kcT, vc = run_cmp(kv, inp)
    for i in range(2):
        h = layer_b(h, i, inp, (kv, kcT, vc))
    return h[None].astype(np.float32)


def kernel(**inputs):
    inp = {k: np.asarray(v) for k, v in inputs.items()}
    return forward(inp)
```

```python
import ml_dtypes
import contextlib
import numpy as np
import concourse.bass as bass
import concourse.mybir as mybir
from concourse.bass_utils import run_bass_kernel_spmd

F32 = mybir.dt.float32
BF16 = mybir.dt.bfloat16
I32 = mybir.dt.int32
U32 = mybir.dt.uint32
AF = mybir.ActivationFunctionType
ALU = mybir.AluOpType
AX = mybir.AxisListType

DMA_RING = 6


class Sched:
    def __init__(self, nc, same_engine_sync=None):
        if same_engine_sync is None:
            same_engine_sync = True
        self.nc = nc
        self.es = contextlib.ExitStack()
        self.engs = {"pe": nc.tensor, "dve": nc.vector, "act": nc.scalar,
                     "pool": nc.gpsimd, "sp": nc.sync}
        self.sem = {}
        self.cnt = {}
        for e in self.engs:
            self.sem[e] = self.es.enter_context(nc.semaphore("s_" + e))
            self.cnt[e] = 0
        self.dsem = {}
        self.dcnt = {}
        self.dissued = {}
        for q in ("sp", "act", "pool"):
            self.dsem[q] = [self.es.enter_context(nc.semaphore("d_%s%d" % (q, i)))
                            for i in range(DMA_RING)]
            self.dcnt[q] = [0] * DMA_RING
            self.dissued[q] = 0
        self.waited = {}
        self.lastw = {}
        self.readers = {}
        self.same = same_engine_sync
        self.excl = set()
        self.ninst = 0

    def sb(self, name, shape, dt):
        return self.es.enter_context(self.nc.sbuf_tensor(name, shape, dt))

    def ps(self, name, shape, dt=F32, key=None):
        self.excl.add(key if key is not None else name)
        return self.es.enter_context(self.nc.psum_tensor(name, shape, dt))

    def _semobj(self, key):
        if isinstance(key, tuple):
            return self.dsem[key[0]][key[1]]
        return self.sem[key]

    def _wait(self, eng, key, val):
        if key == eng and (not self.same or eng == "pe"):
            return
        w = self.waited.get((eng, key), 0)
        if val > w:
            self.engs[eng].wait_ge(self._semobj(key), val)
            self.waited[(eng, key)] = val

    def _deps(self, eng, reads, writes):
        for t in reads:
            lw = self.lastw.get(t)
            if lw is not None:
                self._wait(eng, lw[0], lw[1])
        for t in writes:
            lw = self.lastw.get(t)
            if lw is not None:
                self._wait(eng, lw[0], lw[1])
            for r in self.readers.get(t, ()):
                self._wait(eng, r[0], r[1])

    def _record(self, stamp, reads, writes):
        for t in writes:
            self.lastw[t] = stamp
            self.readers[t] = []
        for t in reads:
            self.readers.setdefault(t, []).append(stamp)

    def op(self, eng, fn, reads=(), writes=()):
        ex = [t for t in reads if t in self.excl]
        if ex:
            reads = [t for t in reads if t not in self.excl]
            writes = list(writes) + ex
        self._deps(eng, reads, writes)
        ins = fn(self.engs[eng])
        self.cnt[eng] += 1
        ins.then_inc(self.sem[eng], 1)
        self._record((eng, self.cnt[eng]), reads, writes)
        self.ninst += 1
        return ins

    def dma(self, q, out, in_, reads=(), writes=(), **kw):
        slot = self.dissued[q] % DMA_RING
        self.dissued[q] += 1
        key = (q, slot)
        if self.dcnt[q][slot] > 0:
            self._wait(q, key, self.dcnt[q][slot])
        self._deps(q, reads, writes)
        ins = self.engs[q].dma_start(out=out, in_=in_, **kw)
        self.dcnt[q][slot] += 16
        ins.then_inc(self.dsem[q][slot], 16)
        self._record((key, self.dcnt[q][slot]), reads, writes)
        self.ninst += 1
        return ins

    def finish(self, out_tiles):
        for t in out_tiles:
            lw = self.lastw.get(t)
            if lw is not None:
                self._wait("sp", lw[0], lw[1])

    def close(self):
        self.es.close()


def build_proj(NT, N, out_bf16=True, has_bias=False):
    nc = bass.Bass("TRN2", target_bir_lowering=False)
    T = NT * 128
    xT = nc.dram_tensor("xT", [1024, T], F32, kind="ExternalInput").ap()
    w = nc.dram_tensor("w", [1024, N], F32, kind="ExternalInput").ap()
    odt = BF16 if out_bf16 else F32
    y = nc.dram_tensor("y", [T, N], odt, kind="ExternalOutput").ap()
    if has_bias:
        bias = nc.dram_tensor("bias", [N], F32, kind="ExternalInput").ap()
    s = Sched(nc)
    if has_bias:
        bias_bc = s.sb("bias_bc", [128, N], F32)
        s.dma("sp", bias_bc[:, :], bias.partition_broadcast(128), writes=["bias_bc"])
    x_f = s.sb("x_f", [128, 8, 512], F32)
    x_b = s.sb("x_b", [128, 8, T], BF16)
    xTv = xT.rearrange("(kc p) t -> p kc t", p=128)
    for c in range(T // 512):
        s.dma("sp", x_f[:, :, :], xTv[:, :, c * 512:(c + 1) * 512], writes=["x_f"])
        s.op("dve", lambda e: e.tensor_copy(out=x_b[:, :, c * 512:(c + 1) * 512], in_=x_f[:, :, :]),
             reads=["x_f"], writes=[("x_b", c)])
    CW = 512
    nch = (N + CW - 1) // CW
    w_f = [s.sb("w_f%d" % i, [128, 8, CW], F32) for i in range(2)]
    w_b = [s.sb("w_b%d" % i, [128, 8, CW], BF16) for i in range(2)]
    pt = [s.ps("pt%d" % i, [128, CW], key=("pt", i)) for i in range(4)]
    ot = [s.sb("ot%d" % i, [128, CW], odt) for i in range(4)]
    wv = w.rearrange("(kc p) n -> p kc n", p=128)
    k = 0

    def load_chunk(ch):
        n0 = ch * CW
        cw = min(CW, N - n0)
        b = ch % 2
        s.dma("sp", w_f[b][:, :, :cw], wv[:, :, n0:n0 + cw], writes=[("w_f", b)])
        s.op("pool", lambda e: e.tensor_copy(out=w_b[b][:, :, :cw], in_=w_f[b][:, :, :cw]),
             reads=[("w_f", b)], writes=[("w_b", b)])

    load_chunk(0)
    for ch in range(nch):
        n0 = ch * CW
        cw = min(CW, N - n0)
        b = ch % 2
        if ch + 1 < nch:
            load_chunk(ch + 1)
        for t in range(NT):
            pb = k % 4
            k += 1
            for kc in range(8):
                s.op("pe", lambda e: e.matmul(pt[pb][:, :cw], lhsT=x_b[:, kc, t * 128:(t + 1) * 128],
                                              rhs=w_b[b][:, kc, :cw], start=(kc == 0), stop=(kc == 7)),
                     reads=[("x_b", t // 4), ("w_b", b)], writes=[("pt", pb)])
            if has_bias:
                s.op("dve", lambda e: e.tensor_tensor(out=ot[pb][:, :cw], in0=pt[pb][:, :cw], in1=bias_bc[:, n0:n0 + cw], op=ALU.add),
                     reads=[("pt", pb), "bias_bc"], writes=[("ot", pb)])
            elif pb % 2 == 0:
                s.op("act", lambda e: e.copy(out=ot[pb][:, :cw], in_=pt[pb][:, :cw]),
                     reads=[("pt", pb)], writes=[("ot", pb)])
            else:
                s.op("dve", lambda e: e.tensor_copy(out=ot[pb][:, :cw], in_=pt[pb][:, :cw]),
                     reads=[("pt", pb)], writes=[("ot", pb)])
            s.dma("sp" if pb % 2 else "pool", y[t * 128:(t + 1) * 128, n0:n0 + cw], ot[pb][:, :cw],
                  reads=[("ot", pb)], writes=[("y", t, ch)])
    s.finish([("y", t, ch) for t in range(NT) for ch in range(nch)])
    print("ninst", s.ninst)
    s.close()
    return nc


A_DIL = (1, 4, 16)


def atta_bias_table():
    slopes = (2.0 ** (-8.0 * np.arange(1, 17) / 16)).astype(np.float32)
    k = np.arange(128)[:, None]
    q = np.arange(128)[None, :]
    tab = np.zeros((3, 128, 2, 16, 128), np.float32)
    for g, d in enumerate(A_DIL):
        for ch in range(2):
            dist = (q - k + (128 if ch == 0 else 0))
            valid = (dist >= 0) & (dist <= 128)
            for hh in range(16):
                b = -(slopes[hh] * (dist * d).astype(np.float32))
                tab[g, :, ch, hh, :] = np.where(valid, b, -1.0e30)
    return tab


def build_atta(units, nslot):
    nc = bass.Bass("TRN2", target_bir_lowering=False)
    U = len(units)
    qT = nc.dram_tensor("qT", [U, 64, 16, 128], BF16, kind="ExternalInput").ap()
    kT = nc.dram_tensor("kT", [U, 64, 16, 256], BF16, kind="ExternalInput").ap()
    v = nc.dram_tensor("v", [U, 2, 128, 16, 64], BF16, kind="ExternalInput").ap()
    tab = nc.dram_tensor("tab", [nslot, 128, 2, 16, 128], F32, kind="ExternalInput").ap()
    o = nc.dram_tensor("o", [U, 128, 16 * 65], F32, kind="ExternalOutput").ap()
    s = Sched(nc)
    tb = s.sb("tb", [128, 2, 16, 128], F32)
    q_sb = [s.sb("q_sb%d" % i, [64, 16, 128], BF16) for i in range(2)]
    k_sb = [s.sb("k_sb%d" % i, [64, 16, 256], BF16) for i in range(2)]
    v_sb = [s.sb("v_sb%d" % i, [128, 2, 16, 65], BF16) for i in range(2)]
    tmp = [s.sb("tmp%d" % i, [128, 2, 512], F32) for i in range(2)]
    pT = [s.sb("pT%d" % i, [128, 2, 512], BF16) for i in range(2)]
    o_sb = [s.sb("o_sb%d" % i, [128, 16, 65], F32) for i in range(2)]
    ps_s = [[s.ps("ps_s%d_%d" % (i, c), [128, 512], key=("ps_s", i, c)) for c in range(2)] for i in range(2)]
    ps_o = [s.ps("ps_o%d" % i, [128, 4, 128], key=("ps_o", i)) for i in range(2)]
    for i in range(2):
        s.op("pool", lambda e: e.memset(v_sb[i][:, :, :, 64:65], 1.0), writes=[("v_sb", i)])
    okeys = []
    state = {"cur_g": -1}
    items = [(u, hg) for u in range(U) for hg in range(4)]

    def S(n):
        u, hg = items[n]
        g = units[u]
        b = u % 2
        i = n % 2
        if hg == 0:
            if g != state["cur_g"]:
                s.dma("sp", tb[:, :, :, :], tab[g], writes=["tb"])
                state["cur_g"] = g
            s.dma("sp", q_sb[b][:, :, :], qT[u], writes=[("q_sb", b)])
            s.dma("sp", k_sb[b][:, :, :], kT[u], writes=[("k_sb", b)])
            for c in range(2):
                s.dma("sp", v_sb[b][:, c, :, 0:64], v[u, c], writes=[("v_sb", b)], reads=[("v_sb", b)])
        for c in range(2):
            for hh in range(4):
                hd = hg * 4 + hh
                s.op("pe", lambda e: e.matmul(ps_s[i][c][:, hh * 128:(hh + 1) * 128],
                                              lhsT=k_sb[b][:, hd, c * 128:(c + 1) * 128], rhs=q_sb[b][:, hd, :],
                                              start=True, stop=True),
                     reads=[("k_sb", b), ("q_sb", b)], writes=[("ps_s", i, c)])
            s.op("dve", lambda e: e.scalar_tensor_tensor(
                out=tmp[i][:, c, :], in0=ps_s[i][c][:, :], scalar=0.125,
                in1=tb[:, c, hg * 4:hg * 4 + 4, :].rearrange("p a b -> p (a b)"),
                op0=ALU.mult, op1=ALU.add),
                reads=[("ps_s", i, c), "tb"], writes=[("tmp", i, c)])
            s.op("act", lambda e: e.activation(out=pT[i][:, c, :], in_=tmp[i][:, c, :], func=AF.Exp),
                 reads=[("tmp", i, c)], writes=[("pT", i, c)])

    def PV(n):
        u, hg = items[n]
        b = u % 2
        i = n % 2
        for hh in range(4):
            hd = hg * 4 + hh
            for c in range(2):
                s.op("pe", lambda e: e.matmul(ps_o[i][:, hh, 0:65], lhsT=pT[i][:, c, hh * 128:(hh + 1) * 128],
                                              rhs=v_sb[b][:, c, hd, :], start=(c == 0), stop=(c == 1)),
                     reads=[("pT", i, c), ("v_sb", b)], writes=[("ps_o", i)])
        s.op("dve" if hg % 2 else "act", (lambda e: e.tensor_copy(out=o_sb[b][:, hg * 4:hg * 4 + 4, :], in_=ps_o[i][:, :, 0:65])) if hg % 2
             else (lambda e: e.copy(out=o_sb[b][:, hg * 4:hg * 4 + 4, :], in_=ps_o[i][:, :, 0:65])),
             reads=[("ps_o", i)], writes=[("o_sb", b)])
        if hg == 3:
            s.dma("sp", o[u], o_sb[b][:, :, :].rearrange("p a b -> p (a b)"), reads=[("o_sb", b)], writes=[("o", u)])
            okeys.append(("o", u))

    S(0)
    for n in range(len(items)):
        if n + 1 < len(items):
            S(n + 1)
        PV(n)
    s.finish(okeys)
    print("atta ninst", s.ninst)
    s.close()
    return nc


def atta_host_prep(qkv):
    S = qkv.shape[0]
    x = qkv.reshape(S, 3, 3, 16, 64)
    qTs, kTs, vs, meta = [], [], [], []
    for g, d in enumerate(A_DIL):
        L = S // d
        nb = L // 128
        def perm(a):
            return a.reshape(L, d, 16, 64).transpose(1, 0, 2, 3)
        q = perm(x[:, g, 0]).reshape(d, nb, 128, 16, 64)
        k = perm(x[:, g, 1])
        vv = perm(x[:, g, 2])
        z = np.zeros((d, 128, 16, 64), qkv.dtype)
        kp = np.concatenate([z, k], axis=1).reshape(d, nb + 1, 128, 16, 64)
        vp = np.concatenate([z, vv], axis=1).reshape(d, nb + 1, 128, 16, 64)
        qT = q.transpose(0, 1, 4, 3, 2).reshape(d * nb, 64, 16, 128)
        k2 = np.stack([kp[:, :-1], kp[:, 1:]], axis=2)
        kT = k2.transpose(0, 1, 5, 4, 2, 3).reshape(d * nb, 64, 16, 256)
        v2 = np.stack([vp[:, :-1], vp[:, 1:]], axis=2).reshape(d * nb, 2, 128, 16, 64)
        qTs.append(np.ascontiguousarray(qT))
        kTs.append(np.ascontiguousarray(kT))
        vs.append(np.ascontiguousarray(v2))
        meta.append([(g, (i % nb) == 0) for i in range(d * nb)])
    return qTs, kTs, vs, meta


def atta_host_post(o_groups, S):
    out = []
    for g, d in enumerate(A_DIL):
        L = S // d
        a = o_groups[g].reshape(d, L, 1040).transpose(1, 0, 2).reshape(S, 1040)
        out.append(a)
    return np.stack(out)


ALPHA = 8.0 ** 0.25
LN_EPS = 1e-5
BIG = 1.0e30


def build_post(T, modeA):
    nc = bass.Bass("TRN2", target_bir_lowering=False)
    D = 1024
    NTL = T // 128
    SG = min(1024, T)
    NSG = T // SG
    TPS = SG // 128

    def din(name, shape, dt=F32):
        return nc.dram_tensor(name, shape, dt, kind="ExternalInput").ap()

    h = din("h", [T, D])
    if modeA:
        og = din("og", [3, T, 16 * 65])
    else:
        og = din("og", [3, T, 16 * 65])
        gl = din("gl", [T, 48], BF16)
    wout = din("wout", [D, D])
    lnp = din("lnp", [4, D])
    wr = din("wr", [D, 20])
    br = din("br", [20])
    wgu = din("wgu", [16, D, 512])
    wd = din("wd", [16, 256, D])
    ident = din("ident", [128, 128])
    sel = din("sel", [128, 16 * 128])
    hout = nc.dram_tensor("hout", [T, D], F32, kind="ExternalOutput").ap()

    s = Sched(nc)
    ident_f = s.sb("ident_f", [128, 128], F32)
    ident_b = s.sb("ident_b", [128, 128], BF16)
    sel_f = s.sb("sel_f", [128, 16 * 128], F32)
    lnbc = s.sb("lnbc", [128, 4, D], F32)
    br_bc = s.sb("br_bc", [128, 20], F32)
    wr_f = s.sb("wr_f", [128, 8, 20], F32)
    wout_b = s.sb("wout_b", [128, 8, D], BF16)
    eps_t = s.sb("eps_t", [128, 1], F32)
    stage = [s.sb("stage%d" % i, [128, 4096], F32) for i in range(2)]
    wgu_b = [s.sb("wgu_b%d" % i, [128, 8, 512], BF16) for i in range(2)]
    wd_b = [s.sb("wd_b%d" % i, [128, 2, D], BF16) for i in range(2)]
    nstage = [0]

    def stage_load(src_ap, n_inner, dst_ap, dkey):
        b = nstage[0] % 2
        nstage[0] += 1
        sv = stage[b][:, :src_ap.shape[1] * src_ap.shape[2]].rearrange("p (a b) -> p a b", a=src_ap.shape[1])
        s.dma("sp", sv, src_ap, writes=[("stage", b)])
        s.op("pool", lambda e: e.tensor_copy(out=dst_ap, in_=sv), reads=[("stage", b)], writes=[dkey])

    s.dma("sp", ident_f[:, :], ident, writes=["ident_f"])
    s.op("dve", lambda e: e.tensor_copy(out=ident_b[:, :], in_=ident_f[:, :]), reads=["ident_f"], writes=["ident_b"])
    s.dma("sp", sel_f[:, :], sel, writes=["sel_f"])
    for i in range(4):
        s.dma("sp", lnbc[:, i, :], lnp[i].partition_broadcast(128), writes=[("lnbc", i)])
    s.dma("sp", br_bc[:, :], br.partition_broadcast(128), writes=["br_bc"])
    s.dma("sp", wr_f[:, :, :], wr.rearrange("(kc p) n -> p kc n", p=128), writes=["wr_f"])
    s.op("dve", lambda e: e.memset(eps_t[:, :], LN_EPS), writes=["eps_t"])
    woutv = wout.rearrange("(kc p) n -> p kc n", p=128)
    for i in range(2):
        stage_load(woutv[:, 4 * i:4 * i + 4, :], None, wout_b[:, 4 * i:4 * i + 4, :], ("wout_b", i))

    h_t = s.sb("h_t", [128, D], F32)
    if modeA:
        og_t = s.sb("og_t", [128, 3, 16 * 65], F32)
        acc = s.sb("acc", [128, 16, 65], F32)
        rl = s.sb("rl", [128, 16], F32)
    else:
        og_t = s.sb("og_t", [128, 3, 16 * 65], F32)
        gl_t = s.sb("gl_t", [128, 48], BF16)
        gate = s.sb("gate", [128, 3, 16], F32)
        rl3 = s.sb("rl3", [128, 3, 16], F32)

    o_b = s.sb("o_b", [128, D], BF16)
    oT = s.sb("oT", [128, 8, 128], BF16)
    r = s.sb("r", [128, D], F32)
    xn = s.sb("xn", [128, D], F32)
    tA = r[:, :].rearrange("p (a b) -> p a b", a=16)
    tB = xn[:, :].rearrange("p (a b) -> p a b", a=16)
    h1 = s.sb("h1", [128, D], F32)
    st = s.sb("st", [128, 2, 6], F32)
    mv = s.sb("mv", [128, 2], F32)
    rstd = s.sb("rstd", [128, 1], F32)
    yacc = s.sb("yacc", [128, TPS, D], F32)
    h1T_b = s.sb("h1T_b", [128, 8, SG], BF16)
    h1T_f = s.sb("h1T_f", [128, 8, 128], F32)
    combT = s.sb("combT", [128, SG], F32)
    L = s.sb("L", [128, 20], F32)
    sm = s.sb("sm", [128, 16], F32)
    goh = s.sb("goh", [128, 4], F32)
    em = s.sb("em", [128, 4, 4], F32)
    top8 = s.sb("top8", [128, 8], F32)
    c0 = s.sb("c0", [128, 16], F32)
    c1 = s.sb("c1", [128, 16], F32)
    comb = s.sb("comb", [128, 128], F32)
    s.op("dve", lambda e: e.memset(comb[:, :], 0.0), writes=["comb"])
    sg = [s.sb("sg%d" % i, [128, 512], F32) for i in range(2)]
    tt_ = [s.sb("tt%d" % i, [128, 512], F32) for i in range(2)]
    hid2 = [[s.sb("hid%d_%d" % (p, i), [128, 512], BF16) for i in range(2)] for p in range(2)]
    cb2 = [s.sb("cb2_%d" % p, [128, 512], F32) for p in range(2)]
    gu = [s.ps("gu%d" % i, [128, 512], key=("gu", i)) for i in range(4)]
    pcb = s.ps("pcb", [128, 512])
    py = [s.ps("py%d" % i, [128, 512], key=("py", i)) for i in range(2)]
    pT_b = s.ps("pT_b", [128, 1024], BF16)

    def layer_norm(src, src_key, gi, dst, dst_key):
        for i in range(2):
            s.op("dve", lambda e: e.bn_stats(out=st[:, i, :], in_=src[:, i * 512:(i + 1) * 512]),
                 reads=[src_key], writes=[("st", i)])
        s.op("dve", lambda e: e.bn_aggr(out=mv[:, :], in_=st[:, :, :]), reads=[("st", 0), ("st", 1)], writes=["mv"])
        s.op("act", lambda e: e.activation(out=rstd[:, :], in_=mv[:, 1:2], func=AF.Sqrt, bias=eps_t[:, :], scale=1.0),
             reads=["mv", "eps_t"], writes=["rstd"])
        s.op("dve", lambda e: e.reciprocal(out=rstd[:, :], in_=rstd[:, :]), reads=["rstd"], writes=["rstd"])
        s.op("dve", lambda e: e.tensor_scalar(out=xn[:, :], in0=src[:, :], scalar1=mv[:, 0:1], scalar2=rstd[:, 0:1],
                                              op0=ALU.subtract, op1=ALU.mult),
             reads=[src_key, "mv", "rstd"], writes=["xn"])
        s.op("pool", lambda e: e.tensor_tensor(out=xn[:, :], in0=xn[:, :], in1=lnbc[:, gi, :], op=ALU.mult),
             reads=["xn", ("lnbc", gi)], writes=["xn"])
        s.op("pool", lambda e: e.tensor_tensor(out=dst, in0=xn[:, :], in1=lnbc[:, gi + 1, :], op=ALU.add),
             reads=["xn", ("lnbc", gi + 1)], writes=[dst_key])

    STOP = 99

    class StopBuild(Exception):
        pass

    def ck(n):
        if STOP == n:
            raise StopBuild()

    out_keys = []
    try:
      ck(1)
      for sgi in range(NSG):
          for ti in range(TPS):
              gt = sgi * TPS + ti
              rows = slice(gt * 128, (gt + 1) * 128)
              s.dma("sp", h_t[:, :], h[rows, :], writes=["h_t"])
              if modeA:
                  s.dma("sp", og_t[:, :, :], og[:, rows, :].rearrange("g t c -> t g c"), writes=["og_t"])
                  accf = acc[:, :, :].rearrange("p a b -> p (a b)")
                  s.op("dve", lambda e: e.tensor_tensor(out=accf, in0=og_t[:, 0, :], in1=og_t[:, 1, :], op=ALU.add),
                       reads=["og_t"], writes=["acc"])
                  s.op("dve", lambda e: e.tensor_tensor(out=accf, in0=accf, in1=og_t[:, 2, :], op=ALU.add),
                       reads=["og_t", "acc"], writes=["acc"])
                  s.op("dve", lambda e: e.reciprocal(out=rl[:, :], in_=acc[:, :, 64]), reads=["acc"], writes=["rl"])
                  s.op("dve", lambda e: e.tensor_tensor(out=o_b[:, :].rearrange("p (a b) -> p a b", a=16),
                                                        in0=acc[:, :, 0:64],
                                                        in1=rl[:, :].unsqueeze(2).to_broadcast([128, 16, 64]),
                                                        op=ALU.mult),
                       reads=["acc", "rl"], writes=["o_b"])
              else:
                  s.dma("sp", og_t[:, :, :], og[:, rows, :].rearrange("g t c -> t g c"), writes=["og_t"])
                  s.dma("sp", gl_t[:, :], gl[rows, :], writes=["gl_t"])
                  s.op("act", lambda e: e.activation(out=gate[:, :, :].rearrange("p a b -> p (a b)"), in_=gl_t[:, :], func=AF.Sigmoid),
                       reads=["gl_t"], writes=["gate"])
                  ogv = og_t[:, :, :].rearrange("p g (h c) -> p g h c", c=65)
                  s.op("dve", lambda e: e.tensor_scalar(out=rl3[:, :, :], in0=ogv[:, :, :, 64], scalar1=1.0e-30, scalar2=None, op0=ALU.max),
                       reads=["og_t"], writes=["rl3"])
                  s.op("dve", lambda e: e.reciprocal(out=rl3[:, :, :], in_=rl3[:, :, :]), reads=["rl3"], writes=["rl3"])
                  s.op("dve", lambda e: e.tensor_tensor(out=rl3[:, :, :], in0=rl3[:, :, :], in1=gate[:, :, :], op=ALU.mult),
                       reads=["rl3", "gate"], writes=["rl3"])
                  s.op("dve", lambda e: e.tensor_tensor(out=tA[:, :, :], in0=ogv[:, 0, :, 0:64],
                                                        in1=rl3[:, 0, :].unsqueeze(2).to_broadcast([128, 16, 64]), op=ALU.mult),
                       reads=["og_t", "rl3"], writes=["r"])
                  s.op("dve", lambda e: e.tensor_tensor(out=tB[:, :, :], in0=ogv[:, 1, :, 0:64],
                                                        in1=rl3[:, 1, :].unsqueeze(2).to_broadcast([128, 16, 64]), op=ALU.mult),
                       reads=["og_t", "rl3"], writes=["xn"])
                  s.op("dve", lambda e: e.tensor_tensor(out=tA[:, :, :], in0=tA[:, :, :], in1=tB[:, :, :], op=ALU.add),
                       reads=["r", "xn"], writes=["r"])
                  s.op("dve", lambda e: e.tensor_tensor(out=tB[:, :, :], in0=ogv[:, 2, :, 0:64],
                                                        in1=rl3[:, 2, :].unsqueeze(2).to_broadcast([128, 16, 64]), op=ALU.mult),
                       reads=["og_t", "rl3"], writes=["xn"])
                  s.op("dve", lambda e: e.tensor_tensor(out=o_b[:, :].rearrange("p (a b) -> p a b", a=16), in0=tA[:, :, :], in1=tB[:, :, :], op=ALU.add),
                       reads=["r", "xn"], writes=["o_b"])
              for kc in range(8):
                  s.op("pe", lambda e: e.transpose(out=pT_b[:, kc * 128:(kc + 1) * 128],
                                                   in_=o_b[:, kc * 128:(kc + 1) * 128], identity=ident_b[:, :]),
                       reads=["o_b", "ident_b"], writes=["pT_b"])
              s.op("act", lambda e: e.copy(out=oT[:, :, :].rearrange("p a b -> p (a b)"), in_=pT_b[:, :]),
                   reads=["pT_b"], writes=["oT"])
              for hh in range(2):
                  for kc in range(8):
                      s.op("pe", lambda e: e.matmul(gu[hh][:, :], lhsT=oT[:, kc, :],
                                                    rhs=wout_b[:, kc, hh * 512:(hh + 1) * 512],
                                                    start=(kc == 0), stop=(kc == 7)),
                           reads=["oT", ("wout_b", kc // 4)], writes=[("gu", hh)])
                  s.op("dve", lambda e: e.scalar_tensor_tensor(out=r[:, hh * 512:(hh + 1) * 512],
                                                               in0=h_t[:, hh * 512:(hh + 1) * 512], scalar=ALPHA,
                                                               in1=gu[hh][:, :], op0=ALU.mult, op1=ALU.add),
                       reads=["h_t", ("gu", hh)], writes=["r"])
              ck(2)
              layer_norm(r, "r", 0, h1[:, :], "h1")
              ck(3)
              s.op("act", lambda e: e.mul(out=yacc[:, ti, :], in_=h1[:, :], mul=ALPHA),
                   reads=["h1"], writes=[("yacc", ti)])
              ck(31)
              for q in range(2):
                  for j in range(4):
                      kc = q * 4 + j
                      s.op("pe", lambda e: e.transpose(out=gu[2 + q][:, j * 128:(j + 1) * 128],
                                                       in_=h1[:, kc * 128:(kc + 1) * 128], identity=ident_f[:, :]),
                           reads=["h1", "ident_f"], writes=[("gu", 2 + q)])
                  ck(32)
                  s.op("act", lambda e: e.copy(out=h1T_f[:, q * 4:q * 4 + 4, :],
                                               in_=gu[2 + q][:, :].rearrange("p (a b) -> p a b", a=4)),
                       reads=[("gu", 2 + q)], writes=[("h1T_f", q)])
                  ck(33)
                  s.op("dve", lambda e: e.tensor_copy(out=h1T_b[:, q * 4:q * 4 + 4, ti * 128:(ti + 1) * 128],
                                                      in_=gu[2 + q][:, :].rearrange("p (a b) -> p a b", a=4)),
                       reads=[("gu", 2 + q)], writes=[("h1T_b", ti)])
              ck(4)
              for kc in range(8):
                  s.op("pe", lambda e: e.matmul(pcb[:, 0:20], lhsT=h1T_f[:, kc, :], rhs=wr_f[:, kc, :],
                                                start=(kc == 0), stop=(kc == 7)),
                       reads=[("h1T_f", kc // 4), "wr_f"], writes=["pcb"])
              s.op("dve", lambda e: e.tensor_tensor(out=L[:, :], in0=pcb[:, 0:20], in1=br_bc[:, :], op=ALU.add),
                   reads=["pcb", "br_bc"], writes=["L"])
              ck(5)
              V = lambda eng, fn, rd, wr_: s.op(eng, fn, reads=rd, writes=wr_)
              V("dve", lambda e: e.reduce_max(out=sm[:, 0:1], in_=L[:, 0:4], axis=AX.X), ["L"], ["sm"])
              V("dve", lambda e: e.tensor_scalar(out=sm[:, 1:2], in0=sm[:, 0:1], scalar1=-1.0, scalar2=None, op0=ALU.mult),
                ["sm"], ["sm"])
              V("act", lambda e: e.activation(out=goh[:, :], in_=L[:, 0:4], func=AF.Exp, bias=sm[:, 1:2], scale=1.0),
                ["L", "sm"], ["goh"])
              V("dve", lambda e: e.reduce_sum(out=sm[:, 2:3], in_=goh[:, :], axis=AX.X), ["goh"], ["sm"])
              V("dve", lambda e: e.reciprocal(out=sm[:, 3:4], in_=sm[:, 2:3]), ["sm"], ["sm"])
              V("dve", lambda e: e.tensor_scalar(out=goh[:, :], in0=L[:, 0:4], scalar1=sm[:, 0:1], scalar2=None,
                                                 op0=ALU.is_equal), ["L", "sm", "goh"], ["goh"])
              V("dve", lambda e: e.tensor_scalar(out=goh[:, :], in0=goh[:, :], scalar1=BIG, scalar2=BIG,
                                                 op0=ALU.mult, op1=ALU.subtract), ["goh"], ["goh"])
              V("dve", lambda e: e.tensor_tensor(out=em[:, :, :], in0=L[:, 4:20].rearrange("p (a b) -> p a b", a=4),
                                                 in1=goh[:, :].unsqueeze(2).to_broadcast([128, 4, 4]), op=ALU.add),
                ["L", "goh"], ["em"])
              emf = em[:, :, :].rearrange("p a b -> p (a b)")
              V("dve", lambda e: e.max(out=top8[:, :], in_=emf), ["em"], ["top8"])
              V("dve", lambda e: e.tensor_tensor(out=sm[:, 4:5], in0=top8[:, 1:2], in1=top8[:, 0:1], op=ALU.subtract),
                ["top8", "sm"], ["sm"])
              V("act", lambda e: e.activation(out=sm[:, 5:6], in_=sm[:, 4:5], func=AF.Exp), ["sm"], ["sm"])
              V("dve", lambda e: e.tensor_scalar(out=sm[:, 6:7], in0=sm[:, 5:6], scalar1=1.0, scalar2=None, op0=ALU.add),
                ["sm"], ["sm"])
              V("dve", lambda e: e.reciprocal(out=sm[:, 6:7], in_=sm[:, 6:7]), ["sm"], ["sm"])
              V("dve", lambda e: e.tensor_tensor(out=sm[:, 7:8], in0=sm[:, 6:7], in1=sm[:, 3:4], op=ALU.mult), ["sm"], ["sm"])
              V("dve", lambda e: e.tensor_tensor(out=sm[:, 8:9], in0=sm[:, 7:8], in1=sm[:, 5:6], op=ALU.mult), ["sm"], ["sm"])
              V("dve", lambda e: e.tensor_scalar(out=c0[:, :], in0=emf, scalar1=top8[:, 0:1], scalar2=sm[:, 7:8],
                                                 op0=ALU.is_equal, op1=ALU.mult), ["em", "top8", "sm"], ["c0"])
              V("dve", lambda e: e.tensor_scalar(out=c1[:, :], in0=emf, scalar1=top8[:, 1:2], scalar2=sm[:, 8:9],
                                                 op0=ALU.is_equal, op1=ALU.mult), ["em", "top8", "sm"], ["c1"])
              V("dve", lambda e: e.tensor_tensor(out=comb[:, 0:16], in0=c0[:, :], in1=c1[:, :], op=ALU.add),
                ["c0", "c1"], ["comb"])
              ck(6)
              V("pe", lambda e: e.transpose(out=pcb[:, 128:256], in_=comb[:, :], identity=ident_f[:, :]),
                ["comb", "ident_f"], ["pcb"])
              V("act", lambda e: e.copy(out=combT[:, ti * 128:(ti + 1) * 128], in_=pcb[:, 128:256]),
                ["pcb"], [("combT", ti)])

          ck(7)
          NSUB = SG // 512
          units = [(ex, sub) for ex in range(16) for sub in range(NSUB)]

          def load_w(ex):
              wb = ex % 2
              stage_load(wgu[ex].rearrange("(kc p) n -> p kc n", p=128), None, wgu_b[wb][:, :, :], ("wgu_b", wb))
              stage_load(wd[ex].rearrange("(fc p) n -> p fc n", p=128), None, wd_b[wb][:, :, :], ("wd_b", wb))

          def GU(n, fc):
              ex, sub = units[n]
              wb = ex % 2
              tok = slice(sub * 512, (sub + 1) * 512)
              tkeys = [("h1T_b", sub * 4 + i) for i in range(4)]
              for j in (fc, 2 + fc):
                  for kc in range(8):
                      s.op("pe", lambda e: e.matmul(gu[j][:, :], lhsT=wgu_b[wb][:, kc, j * 128:(j + 1) * 128],
                                                    rhs=h1T_b[:, kc, tok], start=(kc == 0), stop=(kc == 7)),
                           reads=[("wgu_b", wb)] + tkeys, writes=[("gu", j)])

          def CB(n):
              ex, sub = units[n]
              tok = slice(sub * 512, (sub + 1) * 512)
              s.op("pe", lambda e: e.matmul(pcb[:, :], lhsT=sel_f[:, ex * 128:(ex + 1) * 128], rhs=combT[:, tok],
                                            start=True, stop=True),
                   reads=["sel_f"] + [("combT", sub * 4 + i) for i in range(4)], writes=["pcb"])
              s.op("act", lambda e: e.copy(out=cb2[n % 2][:, :], in_=pcb[:, :]), reads=["pcb"], writes=[("cb", n % 2)])

          def EW(n, fc):
              p = n % 2
              s.op("act", lambda e: e.activation(out=sg[fc][:, :], in_=gu[fc][:, :], func=AF.Silu),
                   reads=[("gu", fc)], writes=[("sg", fc)])
              s.op("dve", lambda e: e.tensor_tensor(out=tt_[fc][:, :], in0=gu[2 + fc][:, :], in1=cb2[p][:, :], op=ALU.mult),
                   reads=[("gu", 2 + fc), ("cb", p)], writes=[("tt", fc)])
              s.op("pool", lambda e: e.tensor_tensor(out=hid2[p][fc][:, :], in0=sg[fc][:, :], in1=tt_[fc][:, :], op=ALU.mult),
                   reads=[("sg", fc), ("tt", fc)], writes=[("hid", p, fc)])

          def DOWN(n):
              ex, sub = units[n]
              wb = ex % 2
              p = n % 2
              for t4 in range(4):
                  ti = sub * 4 + t4
                  for hh in range(2):
                      for fc in range(2):
                          s.op("pe", lambda e: e.matmul(py[hh][:, :], lhsT=hid2[p][fc][:, t4 * 128:(t4 + 1) * 128],
                                                        rhs=wd_b[wb][:, fc, hh * 512:(hh + 1) * 512],
                                                        start=(fc == 0), stop=(fc == 1)),
                               reads=[("hid", p, fc), ("wd_b", wb)], writes=[("py", hh)])
                      s.op("dve", lambda e: e.tensor_tensor(out=yacc[:, ti, hh * 512:(hh + 1) * 512],
                                                            in0=yacc[:, ti, hh * 512:(hh + 1) * 512],
                                                            in1=py[hh][:, :], op=ALU.add),
                           reads=[("yacc", ti), ("py", hh)], writes=[("yacc", ti)])

          load_w(0)
          GU(0, 0); CB(0); EW(0, 0); GU(0, 1); EW(0, 1)
          for n in range(len(units)):
              ex, sub = units[n]
              if sub == 0 and ex + 1 < 16:
                  load_w(ex + 1)
              if n + 1 < len(units):
                  GU(n + 1, 0); CB(n + 1); EW(n + 1, 0)
              DOWN(n)
              if n + 1 < len(units):
                  GU(n + 1, 1); EW(n + 1, 1)
          ck(8)
          for ti in range(TPS):
              gt = sgi * TPS + ti
              layer_norm(yacc[:, ti, :], ("yacc", ti), 2, h1[:, :], "h1")
              s.dma("sp", hout[gt * 128:(gt + 1) * 128, :], h1[:, :], reads=["h1"], writes=[("hout", gt)])
              out_keys.append(("hout", gt))
    except StopBuild:
        pass
    s.finish(out_keys)
    print("post ninst", s.ninst)
    s.close()
    return nc


def post_consts():
    ident = np.eye(128, dtype=np.float32)
    sel = np.zeros((128, 16 * 128), np.float32)
    for e in range(16):
        sel[e, e * 128:(e + 1) * 128] = 1.0
    return {"ident": ident, "sel": sel}


def build_cmp():
    nc = bass.Bass("TRN2", target_bir_lowering=False)

    def din(name, shape, dt=F32):
        return nc.dram_tensor(name, shape, dt, kind="ExternalInput").ap()
    xT = din("xT", [2048, 1024], BF16)
    pos = din("pos", [2048])
    w1 = din("w1", [2048, 256])
    b1 = din("b1", [256])
    w2 = din("w2", [256, 64])
    b2 = din("b2", [64])
    outT = nc.dram_tensor("outT", [64, 1024], BF16, kind="ExternalOutput").ap()
    s = Sched(nc)
    x_in = s.sb("x_in", [128, 16, 1024], BF16)
    x_b = s.sb("x_b", [128, 16, 1024], BF16)
    pos_sb = s.sb("pos_sb", [128, 16], F32)
    w1_f = s.sb("w1_f", [128, 16, 256], F32)
    w1_b = s.sb("w1_b", [128, 16, 256], BF16)
    b1_sb = s.sb("b1_sb", [128, 2], F32)
    w2_f = s.sb("w2_f", [128, 2, 64], F32)
    w2_b = s.sb("w2_b", [128, 2, 64], BF16)
    b2_sb = s.sb("b2_sb", [64, 1], F32)
    xh = s.sb("xh", [128, 512], F32)
    x2 = s.sb("x2", [128, 512], F32)
    sg = s.sb("sg", [128, 512], F32)
    hid = s.sb("hid", [128, 2, 1024], BF16)
    o_sb = s.sb("o_sb", [64, 1024], BF16)
    ph = [s.ps("ph%d" % i, [128, 512], key=("ph", i)) for i in range(2)]
    po = s.ps("po", [128, 512], key="po")
    s.dma("sp", x_in[:, :, :], xT.rearrange("(kc p) n -> p kc n", p=128), writes=["x_in"])
    s.dma("sp", pos_sb[:, :], pos.rearrange("(kc p) -> p kc", p=128), writes=["pos_sb"], allow_slow_non_contiguous=True)
    s.dma("sp", w1_f[:, :, :], w1.rearrange("(kc p) n -> p kc n", p=128), writes=["w1_f"])
    s.dma("sp", b1_sb[:, :], b1.rearrange("(fc p) -> p fc", p=128), writes=["b1_sb"], allow_slow_non_contiguous=True)
    s.dma("sp", w2_f[:, :, :], w2.rearrange("(fc p) n -> p fc n", p=128), writes=["w2_f"])
    s.dma("sp", b2_sb[:, :], b2.rearrange("(p o) -> p o", o=1), writes=["b2_sb"])
    s.op("pool", lambda e: e.tensor_copy(out=w1_b[:, :, :], in_=w1_f[:, :, :]), reads=["w1_f"], writes=["w1_b"])
    s.op("pool", lambda e: e.tensor_copy(out=w2_b[:, :, :], in_=w2_f[:, :, :]), reads=["w2_f"], writes=["w2_b"])
    for kc in range(16):
        s.op("dve", lambda e: e.tensor_scalar(out=x_b[:, kc, :], in0=x_in[:, kc, :], scalar1=pos_sb[:, kc:kc + 1], scalar2=None,
                                              op0=ALU.add), reads=["x_in", "pos_sb"], writes=[("x_b", kc)])
    xkeys = [("x_b", kc) for kc in range(16)]
    k = 0
    for nch in range(2):
        ns = slice(nch * 512, (nch + 1) * 512)
        for fc in range(2):
            p = ph[k % 2]
            pk = ("ph", k % 2)
            k += 1
            for kc in range(16):
                s.op("pe", lambda e: e.matmul(p[:, :], lhsT=w1_b[:, kc, fc * 128:(fc + 1) * 128], rhs=x_b[:, kc, ns],
                                              start=(kc == 0), stop=(kc == 15)), reads=["w1_b"] + xkeys, writes=[pk])
            s.op("dve", lambda e: e.tensor_scalar(out=xh[:, :], in0=p[:, :], scalar1=b1_sb[:, fc:fc + 1], scalar2=None, op0=ALU.add),
                 reads=[pk, "b1_sb"], writes=["xh"])
            s.op("pool", lambda e: e.tensor_tensor(out=x2[:, :], in0=xh[:, :], in1=xh[:, :], op=ALU.mult), reads=["xh"], writes=["x2"])
            s.op("dve", lambda e: e.tensor_scalar(out=x2[:, :], in0=x2[:, :], scalar1=0.044715, scalar2=1.0, op0=ALU.mult, op1=ALU.add),
                 reads=["x2"], writes=["x2"])
            s.op("pool", lambda e: e.tensor_tensor(out=x2[:, :], in0=x2[:, :], in1=xh[:, :], op=ALU.mult), reads=["x2", "xh"], writes=["x2"])
            s.op("act", lambda e: e.activation(out=sg[:, :], in_=x2[:, :], func=AF.Sigmoid, scale=1.5957691216057308),
                 reads=["x2"], writes=["sg"])
            s.op("dve", lambda e: e.tensor_tensor(out=hid[:, fc, ns], in0=xh[:, :], in1=sg[:, :], op=ALU.mult),
                 reads=["xh", "sg"], writes=[("hid", fc, nch)])
        for fc in range(2):
            s.op("pe", lambda e: e.matmul(po[0:64, :], lhsT=w2_b[:, fc, :], rhs=hid[:, fc, ns], start=(fc == 0), stop=(fc == 1)),
                 reads=["w2_b", ("hid", fc, nch)], writes=["po"])
        s.op("dve", lambda e: e.tensor_scalar(out=o_sb[:, ns], in0=po[0:64, :], scalar1=b2_sb[:, 0:1], scalar2=None, op0=ALU.add),
             reads=["po", "b2_sb"], writes=[("o_sb", nch)])
    s.dma("sp", outT, o_sb[:, :], reads=[("o_sb", 0), ("o_sb", 1)], writes=["outT"])
    s.finish(["outT"])
    s.close()
    return nc


NEG = -1.0e30
MNEG = -240000.0


def nsa_tables(S, heads4):
    slopes = (2.0 ** (-8.0 * np.arange(1, 17) / 16)).astype(np.float64)
    sl4 = slopes[list(heads4)]
    kl = np.arange(128)[:, None].astype(np.float64)
    ql = np.arange(128)[None, :].astype(np.float64)
    t = {}
    tabW = np.zeros((5, 128, 2, 128), np.float32)
    for dlt in range(5):
        dist = ql - kl + 128 * dlt
        valid = (dist >= 0) & (dist < 512)
        for h in range(2):
            tabW[dlt, :, h, :] = np.where(valid, -sl4[h] * dist, NEG)
    t["tabW"] = tabW
    tabS = np.zeros((2, 128, 2, 128), np.float32)
    for h in range(2):
        tabS[1, :, h, :] = -sl4[h] * (ql - kl)
        tabS[0, :, h, :] = np.where(ql - kl >= 0, -sl4[h] * (ql - kl), NEG)
    t["tabS"] = tabS
    cst = np.zeros((128, 4, 128), np.float32)
    for h in range(4):
        cst[:, h, :] = (-sl4[h] * 128.0 * np.arange(128))[None, :]
    t["cst"] = cst
    tabC = np.zeros((128, 4, 128), np.float32)
    for h in range(4):
        tabC[:, h, :] = -sl4[h] * (ql - 16 * kl - 31)
    t["tabC"] = tabC
    maskC = np.zeros((17, 128, 128), np.float32)
    for dlt in range(17):
        maskC[dlt] = np.where(128 * dlt + ql - 16 * kl - 31 >= 0, 0.0, MNEG)
    t["maskC"] = maskC
    NQB = S // 128
    nblk = S // 64
    keep = np.ones((NQB, 128, nblk), np.float32)
    force = np.zeros((NQB, 128, nblk), np.float32)
    n = np.arange(nblk)[None, :]
    for i in range(NQB):
        cur = (2 * i + (np.arange(128) >= 64).astype(np.int64))[:, None]
        forced = (n == 0) | (n == cur) | (n == cur - 1)
        fut = n > cur
        keep[i] = np.where(forced | fut, 0.0, 1.0)
        force[i] = np.where(fut, -1.0e4, np.where(forced, 1.0e4, 0.0))
    t["keep"] = keep
    t["force"] = force
    t["identb"] = np.eye(128, dtype=np.float32)
    oh = np.zeros((64, S), np.float32)
    for j in range(S // 128):
        for k in range(128):
            oh[2 * (j % 16) + k // 64, j * 128 + k] = 1.0
        oh[32:35, j * 128:(j + 1) * 128] = 1.0
        oh[35:38, j * 128:(j + 1) * 128] = float(j)
    t["oh"] = oh.astype(ml_dtypes.bfloat16)
    bf = ml_dtypes.bfloat16
    def split3(x):
        x = np.asarray(x, np.float64)
        h1 = x.astype(np.float32).astype(bf).astype(np.float64)
        h2 = (x - h1).astype(np.float32).astype(bf).astype(np.float64)
        h3 = (x - h1 - h2).astype(np.float32).astype(bf).astype(np.float64)
        return [h1, h2, h3]
    augq = np.zeros((NQB, 6, 2), np.float64)
    for h in range(2):
        A = -sl4[h] * 1024.0 * np.arange(NQB)
        Bv = np.full(NQB, sl4[h] * 1024.0)
        for r, comp in enumerate(split3(A) + split3(Bv)):
            augq[:, r, h] = comp
    t["augq"] = augq.astype(np.float32).astype(bf)
    return t


def build_nsa(S):
    nc = bass.Bass("TRN2", target_bir_lowering=False)
    NQB = S // 128
    NCH = S // 128
    NBLK = S // 64
    NHALF = (NBLK + 127) // 128
    NCMP = S // 16
    NCC = (NCMP + 127) // 128
    CW = min(128, NCMP)

    def din(name, shape, dt=F32):
        return nc.dram_tensor(name, shape, dt, kind="ExternalInput").ap()

    qT4 = din("qT4", [NQB, 64, 4, 128], BF16)
    kcT = din("kcT", [64, NCC * 128], BF16)
    vc = din("vc", [NCC * 128, 64], BF16)
    ksT = din("ksT", [64, S], BF16)
    vs = din("vs", [S, 64], BF16)
    kwT = din("kwT", [64, S], BF16)
    vw = din("vw", [S, 64], BF16)
    tabW_d = din("tabW", [5, 128, 2, 128])
    tabS_d = din("tabS", [2, 128, 2, 128])
    cst_d = din("cst", [128, 4, 128])
    tabC_d = din("tabC", [128, 4, 128])
    maskC_d = din("maskC", [17, 128, 128])
    keep_d = din("keep", [NQB, 128, NBLK])
    force_d = din("force", [NQB, 128, NBLK])
    ident_d = din("identb", [128, 128])
    oh_d = din("oh", [64, S], BF16)
    qaug_d = din("qaugsrc", [NQB, 128, 2, 128], BF16)
    o3s = nc.dram_tensor("o3s", [NQB, 65, 2, 128], F32, kind="ExternalOutput").ap()
    o3 = nc.dram_tensor("o3", [NQB, 128, 3, 2, 65], F32, kind="ExternalOutput").ap()

    s = Sched(nc)
    stg = s.sb("stg", [128, 17 * 128], F32)
    tabW = s.sb("tabW_s", [128, 5, 256], F32)
    tabS = s.sb("tabS_s", [128, 2, 256], F32)
    cst = s.sb("cst_s", [128, 4, 128], F32)
    tabC = s.sb("tabC_s", [128, 512], F32)
    maskC = s.sb("maskC_s", [128, 17, 128], BF16)
    identb = s.sb("identb_s", [128, 128], BF16)
    kc_sb = s.sb("kc_sb", [64, NCC * 128], BF16)
    vc_sb = s.sb("vc_sb", [128, NCC, 65], BF16)
    ks_sb = s.sb("ks_sb", [128, S], BF16)
    vs_sb = s.sb("vs_sb", [128, NCH, 65], BF16)
    kw_sb = s.sb("kw_sb", [64, S], BF16)
    vw_sb = s.sb("vw_sb", [128, NCH, 65], BF16)
    s.dma("sp", tabW[:, :, :], tabW_d.rearrange("v p h q -> p v (h q)"), writes=["tabW"])
    s.dma("sp", tabS[:, :, :], tabS_d.rearrange("v p h q -> p v (h q)"), writes=["tabS"])
    s.dma("sp", cst[:, :, :], cst_d, writes=["cst"])
    s.dma("sp", tabC[:, :], tabC_d.rearrange("p h q -> p (h q)"), writes=["tabC"])
    s.dma("sp", stg[:, 0:17 * 128].rearrange("p (v q) -> p v q", v=17), maskC_d.rearrange("v p q -> p v q"), writes=["stg"])
    s.op("dve", lambda e: e.tensor_copy(out=maskC[:, :, :], in_=stg[:, 0:17 * 128].rearrange("p (v q) -> p v q", v=17)),
         reads=["stg"], writes=["maskC"])
    s.dma("sp", stg[:, 0:128], ident_d, writes=["stg"])
    s.op("dve", lambda e: e.tensor_copy(out=identb[:, :], in_=stg[:, 0:128]), reads=["stg"], writes=["identb"])
    s.dma("sp", kc_sb[:, :], kcT, writes=["kc_sb"])
    s.dma("sp", ks_sb[0:64, :], ksT, writes=["ks_sb"])
    s.dma("sp", ks_sb[64:128, :], oh_d, writes=["ks_sb"])
    s.dma("sp", kw_sb[:, :], kwT, writes=["kw_sb"])
    for (vsb, vd, nm, n_) in ((vc_sb, vc, "vc_sb", NCC), (vs_sb, vs, "vs_sb", NCH), (vw_sb, vw, "vw_sb", NCH)):
        s.op("pool", lambda e: e.memset(vsb[:, :, 64:65], 1.0), writes=[nm])
        s.dma("sp", vsb[:, :, 0:64], vd.rearrange("(c p) d -> p c d", p=128), writes=[nm])

    NB = 5
    LA = 3
    q_sb = [s.sb("q_sb%d" % i, [64, 4, 128], BF16) for i in range(2)]
    keep_sb = [s.sb("keep_sb%d" % i, [128, NBLK], F32) for i in range(2)]
    force_sb = [s.sb("force_sb%d" % i, [128, NBLK], F32) for i in range(2)]
    tmp = [s.sb("tmp%d" % i, [128, 512], F32) for i in range(NB)]
    pc = s.sb("pc", [128, NCC, 512], BF16)
    pw = s.sb("pw", [128, 5, 256], BF16)
    psl = [s.sb("psl%d" % i, [128, 256], BF16) for i in range(NB)]
    oc_sb = s.sb("oc_sb", [128, 4, 65], F32)
    rl = s.sb("rl", [128, 4], F32)
    imp = s.sb("imp", [128, NCC * 128], F32)
    sc = s.sb("sc", [128, NBLK], F32)
    sc2 = s.sb("sc2", [128, NBLK], F32)
    t8a = s.sb("t8a", [128, 8], F32)
    t8b = s.sb("t8b", [128, 8], F32)
    NV = (NCH + 15) // 16
    selb = s.sb("selb", [128, max(64 + NBLK, 32 * (NV - 1) + 128)], BF16)
    qaug = [s.sb("qaug%d" % i, [128, NV, 2, 128], BF16) for i in range(2)]
    osT = [s.sb("osT%d" % i, [65, 2, 128], F32) for i in range(2)]
    o_sb = [s.sb("o_sb%d" % i, [128, 3, 2, 65], F32) for i in range(2)]
    ps_s = [s.ps("ps_s%d" % i, [128, 512], key=("ps_s", i)) for i in range(NB)]
    ps_os = [s.ps("ps_os%d" % i, [128, 512], key=("ps_os", i)) for i in range(1)]
    ps_o = s.ps("ps_o", [128, 4, 128], key="ps_o")
    ps_t = s.ps("ps_t", [128, 1024], BF16, key="ps_t")
    s.op("dve", lambda e: e.memset(selb[:, :], 0.0), writes=["selb"])
    for i_ in range(2):
        s.op("pool", lambda e: e.memset(o_sb[i_][:, :, :, :], 0.0), writes=[("o_sb", i_, 0), ("o_sb", i_, 2)])

    sidx = [0]
    okeys = []

    def nextbuf():
        k = sidx[0] % NB
        sidx[0] += 1
        return k

    def sel_steps(i):
        b = i % 2
        steps = []

        def ld():
            s.dma("sp", q_sb[b][:, :, :], qT4[i], writes=[("q_sb", b)])
            s.dma("sp", qaug[b][:, :, :, :], qaug_d[i].unsqueeze(1).to_broadcast([128, NV, 2, 128]), writes=[("qaug", b)])
            s.dma("sp", keep_sb[b][:, :], keep_d[i], writes=[("keep", b)])
            s.dma("sp", force_sb[b][:, :], force_d[i], writes=[("force", b)])
        steps.append(ld)
        mlist = [m for m in range(NCC) if 16 * CW * m + 31 <= 128 * i + 127]

        def cmp_chunk(m):
            k = nextbuf()
            dl = i - 16 * m
            if dl <= 16:
                s.op("pe", lambda e: e.matmul(ps_s[k][:, :], lhsT=identb[:, :],
                                              rhs=maskC[:, dl, :].unsqueeze(1).to_broadcast([128, 4, 128]),
                                              start=True, stop=False),
                     reads=["identb", "maskC"], writes=[("ps_s", k)])
            s.op("pe", lambda e: e.matmul(ps_s[k][:, :], lhsT=kc_sb[:, m * 128:(m + 1) * 128],
                                          rhs=q_sb[b][:, :, :].rearrange("p a b -> p (a b)"), start=(dl > 16), stop=True),
                 reads=["kc_sb", ("q_sb", b)], writes=[("ps_s", k)])
            s.op("dve", lambda e: e.scalar_tensor_tensor(out=tmp[k][:, :], in0=ps_s[k][:, :], scalar=0.125,
                                                         in1=tabC[:, :], op0=ALU.mult, op1=ALU.add),
                 reads=[("ps_s", k), "tabC"], writes=[("tmp", k)])
            for h in range(4):
                s.op("act", lambda e: e.activation(out=pc[:, m, h * 128:(h + 1) * 128], in_=tmp[k][:, h * 128:(h + 1) * 128],
                                                   func=AF.Exp, bias=cst[:, h, dl:dl + 1], scale=1.0),
                     reads=[("tmp", k), "cst"], writes=[("pc", m, h)])
        for m in mlist:
            steps.append(lambda m=m: cmp_chunk(m))

        def cmp_pv():
            for h in range(4):
                for mi, m in enumerate(mlist):
                    s.op("pe", lambda e: e.matmul(ps_o[:, h, 0:65], lhsT=pc[:, m, h * 128:(h + 1) * 128], rhs=vc_sb[:, m, :],
                                                  start=(mi == 0), stop=(mi == len(mlist) - 1)),
                         reads=[("pc", m, h), "vc_sb"], writes=["ps_o"])
            s.op("act", lambda e: e.copy(out=oc_sb[:, :, :], in_=ps_o[:, :, 0:65]), reads=["ps_o"], writes=["oc_sb"])
            s.op("pool", lambda e: e.tensor_copy(out=o_sb[b][:, 0, :, :], in_=oc_sb[:, 0:2, :]), reads=["oc_sb"], writes=[("o_sb", b, 0)])
            s.op("dve", lambda e: e.tensor_scalar(out=rl[:, :], in0=oc_sb[:, :, 64], scalar1=1.0e-30, scalar2=None, op0=ALU.max),
                 reads=["oc_sb"], writes=["rl"])
            s.op("dve", lambda e: e.reciprocal(out=rl[:, :], in_=rl[:, :]), reads=["rl"], writes=["rl"])
            s.op("pool", lambda e: e.memset(imp[:, :], 0.0), writes=["imp"])
        steps.append(cmp_pv)

        def imp_head(h):
            for m in mlist:
                s.op("pe", lambda e: e.transpose(out=ps_t[:, m * 128:(m + 1) * 128], in_=pc[:, m, h * 128:(h + 1) * 128],
                                                 identity=identb[:, :]),
                     reads=[("pc", m, h), "identb"], writes=["ps_t"])
            w = len(mlist) * 128
            s.op("dve", lambda e: e.scalar_tensor_tensor(out=imp[:, 0:w], in0=ps_t[:, 0:w], scalar=rl[:, h:h + 1],
                                                         in1=imp[:, 0:w], op0=ALU.mult, op1=ALU.add),
                 reads=["ps_t", "rl", "imp"], writes=["imp"])
        for h in range(4):
            steps.append(lambda h=h: imp_head(h))
        A = imp[:, :].rearrange("p (n f) -> p n f", f=4)
        nb = NBLK

        def score1():
            s.op("dve", lambda e: e.tensor_tensor(out=sc[:, :], in0=A[:, 0:nb, 0], in1=A[:, 0:nb, 1], op=ALU.add), reads=["imp"], writes=["sc"])
            s.op("dve", lambda e: e.tensor_tensor(out=sc[:, :], in0=sc[:, :], in1=A[:, 0:nb, 2], op=ALU.add), reads=["imp", "sc"], writes=["sc"])
            s.op("dve", lambda e: e.scalar_tensor_tensor(out=sc[:, :], in0=sc[:, :], scalar=2.0, in1=A[:, 0:nb, 3],
                                                         op0=ALU.mult, op1=ALU.add), reads=["imp", "sc"], writes=["sc"])
            s.op("dve", lambda e: e.tensor_tensor(out=sc[:, 1:nb], in0=sc[:, 1:nb], in1=A[:, 0:nb - 1, 3], op=ALU.add),
                 reads=["imp", "sc"], writes=["sc"])

        def score2():
            s.op("dve", lambda e: e.tensor_tensor(out=sc[:, :], in0=sc[:, :], in1=keep_sb[b][:, :], op=ALU.mult),
                 reads=["sc", ("keep", b)], writes=["sc"])
            s.op("dve", lambda e: e.tensor_tensor(out=sc[:, :], in0=sc[:, :], in1=force_sb[b][:, :], op=ALU.add),
                 reads=["sc", ("force", b)], writes=["sc"])
            s.op("dve", lambda e: e.max(out=t8a[:, :], in_=sc[:, :]), reads=["sc"], writes=["t8a"])

        def score3():
            s.op("dve", lambda e: e.match_replace(out=sc2[:, :], in_to_replace=t8a[:, :], in_values=sc[:, :], imm_value=-3.0e4),
                 reads=["sc", "t8a"], writes=["sc2"])
            s.op("dve", lambda e: e.max(out=t8b[:, :], in_=sc2[:, :]), reads=["sc2"], writes=["t8b"])
            s.op("dve", lambda e: e.tensor_scalar(out=sc2[:, :], in0=sc[:, :], scalar1=t8b[:, 7:8], scalar2=-MNEG,
                                                  op0=ALU.is_ge, op1=ALU.mult), reads=["sc", "t8b"], writes=["sc2"])
            s.op("dve", lambda e: e.tensor_scalar(out=selb[:, 64:64 + nb], in0=sc2[:, :], scalar1=MNEG, scalar2=None, op0=ALU.add),
                 reads=["sc2"], writes=["selb"])

        def score4():
            for v_ in range(NV):
                s.op("pe", lambda e: e.transpose(out=ps_t[:, v_ * 128:(v_ + 1) * 128], in_=selb[:, 32 * v_:32 * v_ + 128],
                                                 identity=identb[:, :]), reads=["selb", "identb"], writes=["ps_t"])
            s.op("act", lambda e: e.copy(out=qaug[b][64:96, :, :, :],
                                         in_=ps_t[64:96, 0:NV * 128].rearrange("p (v q) -> p v q", v=NV).unsqueeze(2).to_broadcast([32, NV, 2, 128])),
                 reads=["ps_t"], writes=[("qaug", b)])
        steps += [score1, score2, score3, score4]
        return steps

    def s_stage(i, j, k):
        b = i % 2
        v_ = j // 16
        s.op("pe", lambda e: e.matmul(ps_s[k][:, 0:256], lhsT=ks_sb[:, j * 128:(j + 1) * 128],
                                      rhs=qaug[b][:, v_, :, :].rearrange("p a b -> p (a b)"), start=True, stop=True),
             reads=["ks_sb", ("qaug", b)], writes=[("ps_s", k)])
        s.op("dve", lambda e: e.scalar_tensor_tensor(out=tmp[k][:, 0:256], in0=ps_s[k][:, 0:256], scalar=0.125,
                                                     in1=tabS[:, 0 if j == i else 1, :], op0=ALU.mult, op1=ALU.add),
             reads=[("ps_s", k), "tabS"], writes=[("tmp", k)])
        s.op("act", lambda e: e.activation(out=psl[k][:, 0:256], in_=tmp[k][:, 0:256], func=AF.Exp),
             reads=[("tmp", k)], writes=[("psl", k, 0)])

    def pv_stage(i, j, k):
        s.op("pe", lambda e: e.matmul(ps_os[0][0:65, 0:256], lhsT=vs_sb[:, j, :], rhs=psl[k][:, 0:256],
                                      start=(j == 0), stop=(j == i)),
             reads=[("psl", k, 0), "vs_sb"], writes=[("ps_os", 0)])

    def window(i):
        b = i % 2
        wl = [j for j in range(i - 4, i + 1) if j >= 0]
        for wi, j in enumerate(wl):
            k = nextbuf()
            s.op("pe", lambda e: e.matmul(ps_s[k][:, 0:256], lhsT=kw_sb[:, j * 128:(j + 1) * 128],
                                          rhs=q_sb[b][:, 0:2, :].rearrange("p a b -> p (a b)"), start=True, stop=True),
                 reads=["kw_sb", ("q_sb", b)], writes=[("ps_s", k)])
            s.op("dve", lambda e: e.scalar_tensor_tensor(out=tmp[k][:, 0:256], in0=ps_s[k][:, 0:256], scalar=0.125,
                                                         in1=tabW[:, i - j, :], op0=ALU.mult, op1=ALU.add),
                 reads=[("ps_s", k), "tabW"], writes=[("tmp", k)])
            s.op("act", lambda e: e.activation(out=pw[:, wi, :], in_=tmp[k][:, 0:256], func=AF.Exp),
                 reads=[("tmp", k)], writes=[("pw", wi)])
        for h in range(2):
            for wi, j in enumerate(wl):
                s.op("pe", lambda e: e.matmul(ps_o[:, h, 0:65], lhsT=pw[:, wi, h * 128:(h + 1) * 128], rhs=vw_sb[:, j, :],
                                              start=(wi == 0), stop=(wi == len(wl) - 1)),
                     reads=[("pw", wi), "vw_sb"], writes=["ps_o"])
        s.op("act", lambda e: e.copy(out=o_sb[b][:, 2, :, :], in_=ps_o[:, 0:2, 0:65]), reads=["ps_o"], writes=[("o_sb", b, 2)])

    for st in sel_steps(0):
        st()
    for i in range(NQB):
        b = i % 2
        nxt = sel_steps(i + 1) if i + 1 < NQB else []
        n = i + 1
        bufs = {}
        for step in range(n + LA):
            if step < n:
                bufs[step] = nextbuf()
                s_stage(i, step, bufs[step])
            if step - LA >= 0:
                pv_stage(i, step - LA, bufs[step - LA])
            if nxt and step % 2 == 1:
                nxt.pop(0)()
        s.op("act", lambda e: e.copy(out=osT[b][:, :, :].rearrange("p a b -> p (a b)"), in_=ps_os[0][0:65, 0:256]),
             reads=[("ps_os", 0)], writes=[("osT", b)])
        s.dma("sp", o3s[i], osT[b][:, :, :], reads=[("osT", b)], writes=[("o3s", i)])
        okeys.append(("o3s", i))
        window(i)
        while nxt:
            nxt.pop(0)()
        s.dma("sp", o3[i], o_sb[b][:, :, :, :], reads=[("o_sb", b, 0), ("o_sb", b, 2)], writes=[("o3", i)])
        okeys.append(("o3", i))
    s.finish(okeys)
    print("nsa ninst", s.ninst)
    s.close()
    return nc


import numpy as np

NCORES = 8
S = 16384
TPC = S // NCORES
_cache = {}


def _get(key, fn):
    if key not in _cache:
        _cache[key] = fn()
    return _cache[key]


def run_proj(h, w, out_bf16=True):
    N = w.shape[1]
    nc = _get(("proj", N, out_bf16), lambda: build_proj(TPC // 128, N, out_bf16))
    in_maps = [{"xT": np.ascontiguousarray(h[c * TPC:(c + 1) * TPC].T), "w": w} for c in range(NCORES)]
    res = run_bass_kernel_spmd(nc, in_maps, core_ids=list(range(NCORES)))
    return np.concatenate([np.asarray(r["y"]) for r in res.results], axis=0)


def run_atta(qkv):
    qTs, kTs, vs, meta = atta_host_prep(qkv)
    upc = [len(m) // NCORES for m in meta]
    units = []
    for g in range(3):
        for p in range(upc[g]):
            units.append(3 * g + (0 if p == 0 else (2 if p == 8 else 1)))
    nc = _get(("atta",), lambda: build_atta(units, 9))
    tab = atta_bias_table()
    in_maps = []
    for c in range(NCORES):
        tabs = np.zeros((9, 128, 2, 16, 128), np.float32)
        for g in range(3):
            for slot, p in ((0, 0), (1, 1), (2, 8)):
                t = tab[g].copy()
                if meta[g][c * upc[g] + p][1]:
                    t[:, 0] = -1.0e30
                tabs[3 * g + slot] = t
        sl = [slice(c * upc[g], (c + 1) * upc[g]) for g in range(3)]
        in_maps.append({"qT": np.concatenate([qTs[g][sl[g]] for g in range(3)]),
                        "kT": np.concatenate([kTs[g][sl[g]] for g in range(3)]),
                        "v": np.concatenate([vs[g][sl[g]] for g in range(3)]),
                        "tab": tabs})
    res = run_bass_kernel_spmd(nc, in_maps, core_ids=list(range(NCORES)))
    og = []
    for g in range(3):
        off = sum(upc[:g])
        og.append(np.concatenate([np.asarray(r["o"])[off:off + upc[g]] for r in res.results], axis=0))
    return atta_host_post(og, S)


def run_post(h, o_or_og, modeA, wout, lnp, wr, br, wgu, wd):
    nc = _get(("post", modeA), lambda: build_post(TPC, modeA))
    consts = post_consts()
    in_maps = []
    for c in range(NCORES):
        rows = slice(c * TPC, (c + 1) * TPC)
        m = {"h": np.ascontiguousarray(h[rows]), "wout": wout, "lnp": lnp, "wr": wr, "br": br, "wgu": wgu, "wd": wd}
        m.update(consts)
        if modeA:
            m["og"] = np.ascontiguousarray(o_or_og[:, rows])
        else:
            m["o"] = np.ascontiguousarray(o_or_og[rows])
        in_maps.append(m)
    res = run_bass_kernel_spmd(nc, in_maps, core_ids=list(range(NCORES)))
    return np.concatenate([np.asarray(r["hout"]) for r in res.results], axis=0)


def layer_a(h, l, inp):
    qkv = run_proj(h, np.ascontiguousarray(inp["a_w_in"][l]))
    og = run_atta(qkv)
    lnp = np.stack([inp["ln_mix_g"][l], inp["ln_mix_b"][l], inp["ln_ffn_g"][l], inp["ln_ffn_b"][l]])
    return run_post(h, og, 1, np.ascontiguousarray(inp["a_w_out"][l]), lnp,
                    np.ascontiguousarray(inp["moe_w_router"][l]), np.ascontiguousarray(inp["moe_b_router"][l]),
                    np.ascontiguousarray(inp["moe_w_gate_up"][l]), np.ascontiguousarray(inp["moe_w_down"][l]))


BF = ml_dtypes.bfloat16


def run_proj_b(h, w, bias):
    N = w.shape[1]
    nc = _get(("projb", N), lambda: build_proj(TPC // 128, N, True, True))
    in_maps = [{"xT": np.ascontiguousarray(h[c * TPC:(c + 1) * TPC].T), "w": w, "bias": bias} for c in range(NCORES)]
    res = run_bass_kernel_spmd(nc, in_maps, core_ids=list(range(NCORES)))
    return np.concatenate([np.asarray(r["y"]) for r in res.results], axis=0)


def run_cmp(kv, inp):
    kv6 = kv.reshape(S, 6, 4, 64)
    nc = _get(("cmp",), build_cmp)
    in_maps = []
    nblk = S // 16
    for j in range(2):
        for g in range(4):
            u = kv6[:, j, g, :]
            c = u.reshape(nblk, 16, 64)
            blocks = np.concatenate([c[:-1], c[1:]], axis=1)
            X = np.zeros((nblk, 2048), kv.dtype)
            X[:nblk - 1] = blocks.reshape(nblk - 1, 2048)
            in_maps.append({"xT": np.ascontiguousarray(X.T), "pos": np.ascontiguousarray(inp["b_cmp_pos"][j].reshape(2048)),
                            "w1": np.ascontiguousarray(inp["b_cmp_w1"][j]), "b1": np.ascontiguousarray(inp["b_cmp_b1"][j]),
                            "w2": np.ascontiguousarray(inp["b_cmp_w2"][j]), "b2": np.ascontiguousarray(inp["b_cmp_b2"][j])})
    res = run_bass_kernel_spmd(nc, in_maps, core_ids=list(range(NCORES)))
    outs = [np.asarray(r["outT"]) for r in res.results]
    kcT = [outs[g] for g in range(4)]
    vc = [np.ascontiguousarray(outs[4 + g].T) for g in range(4)]
    return kcT, vc


def run_nsa(proj, kv, kcT, vc):
    nc = _get(("nsa",), lambda: build_nsa(S))
    kv6 = kv.reshape(S, 6, 4, 64)
    q = proj[:, :1024].reshape(S // 128, 128, 16, 64)
    in_maps = []
    for c in range(NCORES):
        g, half = c // 2, c % 2
        own = [4 * g + 2 * half, 4 * g + 2 * half + 1]
        oth = [4 * g + 2 * (1 - half), 4 * g + 2 * (1 - half) + 1]
        heads4 = own + oth
        m = nsa_tables(S, heads4)
        m["qT4"] = np.ascontiguousarray(q[:, :, heads4, :].transpose(0, 3, 2, 1))
        qsrc = np.zeros((S // 128, 128, 2, 128), BF)
        qsrc[:, 0:64] = m["qT4"][:, :, 0:2, :]
        qsrc[:, 96:102] = m.pop("augq")[:, :, :, None]
        m["qaugsrc"] = qsrc
        m["kcT"] = kcT[g]
        m["vc"] = vc[g]
        m["ksT"] = np.ascontiguousarray(kv6[:, 2, g, :].T)
        m["vs"] = np.ascontiguousarray(kv6[:, 3, g, :])
        m["kwT"] = np.ascontiguousarray(kv6[:, 4, g, :].T)
        m["vw"] = np.ascontiguousarray(kv6[:, 5, g, :])
        in_maps.append(m)
    res = run_bass_kernel_spmd(nc, in_maps, core_ids=list(range(NCORES)))
    og = np.zeros((3, S, 16, 65), np.float32)
    for c in range(NCORES):
        g, half = c // 2, c % 2
        o3 = np.asarray(res.results[c]["o3"]).reshape(S, 3, 2, 65).copy()
        o3[:, 1] = np.asarray(res.results[c]["o3s"]).transpose(0, 3, 2, 1).reshape(S, 2, 65)
        for hh in range(2):
            og[:, :, 4 * g + 2 * half + hh, :] = o3[:, :, hh, :].transpose(1, 0, 2)
    return og.reshape(3, S, 1040)


def layer_b(h, i, inp, shared):
    l = 2 + i
    proj = run_proj_b(h, np.ascontiguousarray(inp["b_w_q"][i]), np.ascontiguousarray(inp["b_b_q"][i]))
    kv, kcT, vc = shared
    og = run_nsa(proj, kv, kcT, vc)
    lnp = np.stack([inp["ln_mix_g"][l], inp["ln_mix_b"][l], inp["ln_ffn_g"][l], inp["ln_ffn_b"][l]])
    gl = np.ascontiguousarray(proj[:, 1024:1072])
    nc = _get(("post", 0), lambda: build_post(TPC, 0))
    consts = post_consts()
    in_maps = []
    for c in range(NCORES):
        rows = slice(c * TPC, (c + 1) * TPC)
        m = {"h": np.ascontiguousarray(h[rows]), "wout": np.ascontiguousarray(inp["b_w_out"][i]), "lnp": lnp,
             "wr": np.ascontiguousarray(inp["moe_w_router"][l]), "br": np.ascontiguousarray(inp["moe_b_router"][l]),
             "wgu": np.ascontiguousarray(inp["moe_w_gate_up"][l]), "wd": np.ascontiguousarray(inp["moe_w_down"][l]),
             "og": np.ascontiguousarray(og[:, rows]), "gl": np.ascontiguousarray(gl[rows])}
        m.update(consts)
        in_maps.append(m)
    res = run_bass_kernel_spmd(nc, in_maps, core_ids=list(range(NCORES)))
    return np.concatenate([np.asarray(r["hout"]) for r in res.results], axis=0)


def forward(inp):
    h = np.ascontiguousarray(inp["x"][0])
    for l in range(2):
        h = layer_a(h, l, inp)
    kv = run_proj(h, np.ascontiguousarray(inp["b_w_kv"]))
    kcT, vc = run_cmp(kv, inp)
    for i in range(2):
        h = layer_b(h, i, inp, (kv, kcT, vc))
    return h[None].astype(np.float32)


def kernel(**inputs):
    inp = {k: np.asarray(v) for k, v in inputs.items()}
    return forward(inp)
```

```python
import ml_dtypes
import contextlib
import numpy as np
import concourse.bass as bass
import concourse.mybir as mybir
from concourse.bass_utils import run_bass_kernel_spmd

F32 = mybir.dt.float32
BF16 = mybir.dt.bfloat16
I32 = mybir.dt.int32
U32 = mybir.dt.uint32
AF = mybir.ActivationFunctionType
ALU = mybir.AluOpType
AX = mybir.AxisListType

DMA_RING = 6


class Sched:
    def __init__(self, nc, same_engine_sync=None):
        if same_engine_sync is None:
            same_engine_sync = True
        self.nc = nc
        self.es = contextlib.ExitStack()
        self.engs = {"pe": nc.tensor, "dve": nc.vector, "act": nc.scalar,
                     "pool": nc.gpsimd, "sp": nc.sync}
        self.sem = {}
        self.cnt = {}
        for e in self.engs:
            self.sem[e] = self.es.enter_context(nc.semaphore("s_" + e))
            self.cnt[e] = 0
        self.dsem = {}
        self.dcnt = {}
        self.dissued = {}
        for q in ("sp", "act", "pool"):
            self.dsem[q] = [self.es.enter_context(nc.semaphore("d_%s%d" % (q, i)))
                            for i in range(DMA_RING)]
            self.dcnt[q] = [0] * DMA_RING
            self.dissued[q] = 0
        self.waited = {}
        self.lastw = {}
        self.readers = {}
        self.same = same_engine_sync
        self.excl = set()
        self.ninst = 0

    def sb(self, name, shape, dt):
        return self.es.enter_context(self.nc.sbuf_tensor(name, shape, dt))

    def ps(self, name, shape, dt=F32, key=None):
        self.excl.add(key if key is not None else name)
        return self.es.enter_context(self.nc.psum_tensor(name, shape, dt))

    def _semobj(self, key):
        if isinstance(key, tuple):
            return self.dsem[key[0]][key[1]]
        return self.sem[key]

    def _wait(self, eng, key, val):
        if key == eng and (not self.same or eng == "pe"):
            return
        w = self.waited.get((eng, key), 0)
        if val > w:
            self.engs[eng].wait_ge(self._semobj(key), val)
            self.waited[(eng, key)] = val

    def _deps(self, eng, reads, writes):
        for t in reads:
            lw = self.lastw.get(t)
            if lw is not None:
                self._wait(eng, lw[0], lw[1])
        for t in writes:
            lw = self.lastw.get(t)
            if lw is not None:
                self._wait(eng, lw[0], lw[1])
            for r in self.readers.get(t, ()):
                self._wait(eng, r[0], r[1])

    def _record(self, stamp, reads, writes):
        for t in writes:
            self.lastw[t] = stamp
            self.readers[t] = []
        for t in reads:
            self.readers.setdefault(t, []).append(stamp)

    def op(self, eng, fn, reads=(), writes=()):
        ex = [t for t in reads if t in self.excl]
        if ex:
            reads = [t for t in reads if t not in self.excl]
            writes = list(writes) + ex
        self._deps(eng, reads, writes)
        ins = fn(self.engs[eng])
        self.cnt[eng] += 1
        ins.then_inc(self.sem[eng], 1)
        self._record((eng, self.cnt[eng]), reads, writes)
        self.ninst += 1
        return ins

    def dma(self, q, out, in_, reads=(), writes=(), **kw):
        slot = self.dissued[q] % DMA_RING
        self.dissued[q] += 1
        key = (q, slot)
        if self.dcnt[q][slot] > 0:
            self._wait(q, key, self.dcnt[q][slot])
        self._deps(q, reads, writes)
        ins = self.engs[q].dma_start(out=out, in_=in_, **kw)
        self.dcnt[q][slot] += 16
        ins.then_inc(self.dsem[q][slot], 16)
        self._record((key, self.dcnt[q][slot]), reads, writes)
        self.ninst += 1
        return ins

    def finish(self, out_tiles):
        for t in out_tiles:
            lw = self.lastw.get(t)
            if lw is not None:
                self._wait("sp", lw[0], lw[1])

    def close(self):
        self.es.close()


def build_proj(NT, N, out_bf16=True, has_bias=False):
    nc = bass.Bass("TRN2", target_bir_lowering=False)
    T = NT * 128
    xT = nc.dram_tensor("xT", [1024, T], F32, kind="ExternalInput").ap()
    w = nc.dram_tensor("w", [1024, N], F32, kind="ExternalInput").ap()
    odt = BF16 if out_bf16 else F32
    y = nc.dram_tensor("y", [T, N], odt, kind="ExternalOutput").ap()
    if has_bias:
        bias = nc.dram_tensor("bias", [N], F32, kind="ExternalInput").ap()
    s = Sched(nc)
    if has_bias:
        bias_bc = s.sb("bias_bc", [128, N], F32)
        s.dma("sp", bias_bc[:, :], bias.partition_broadcast(128), writes=["bias_bc"])
    x_f = s.sb("x_f", [128, 8, 512], F32)
    x_b = s.sb("x_b", [128, 8, T], BF16)
    xTv = xT.rearrange("(kc p) t -> p kc t", p=128)
    for c in range(T // 512):
        s.dma("sp", x_f[:, :, :], xTv[:, :, c * 512:(c + 1) * 512], writes=["x_f"])
        s.op("dve", lambda e: e.tensor_copy(out=x_b[:, :, c * 512:(c + 1) * 512], in_=x_f[:, :, :]),
             reads=["x_f"], writes=[("x_b", c)])
    CW = 512
    nch = (N + CW - 1) // CW
    w_f = [s.sb("w_f%d" % i, [128, 8, CW], F32) for i in range(2)]
    w_b = [s.sb("w_b%d" % i, [128, 8, CW], BF16) for i in range(2)]
    pt = [s.ps("pt%d" % i, [128, CW], key=("pt", i)) for i in range(4)]
    ot = [s.sb("ot%d" % i, [128, CW], odt) for i in range(4)]
    wv = w.rearrange("(kc p) n -> p kc n", p=128)
    k = 0

    def load_chunk(ch):
        n0 = ch * CW
        cw = min(CW, N - n0)
        b = ch % 2
        s.dma("sp", w_f[b][:, :, :cw], wv[:, :, n0:n0 + cw], writes=[("w_f", b)])
        s.op("pool", lambda e: e.tensor_copy(out=w_b[b][:, :, :cw], in_=w_f[b][:, :, :cw]),
             reads=[("w_f", b)], writes=[("w_b", b)])

    load_chunk(0)
    for ch in range(nch):
        n0 = ch * CW
        cw = min(CW, N - n0)
        b = ch % 2
        if ch + 1 < nch:
            load_chunk(ch + 1)
        for t in range(NT):
            pb = k % 4
            k += 1
            for kc in range(8):
                s.op("pe", lambda e: e.matmul(pt[pb][:, :cw], lhsT=x_b[:, kc, t * 128:(t + 1) * 128],
                                              rhs=w_b[b][:, kc, :cw], start=(kc == 0), stop=(kc == 7)),
                     reads=[("x_b", t // 4), ("w_b", b)], writes=[("pt", pb)])
            if has_bias:
                s.op("dve", lambda e: e.tensor_tensor(out=ot[pb][:, :cw], in0=pt[pb][:, :cw], in1=bias_bc[:, n0:n0 + cw], op=ALU.add),
                     reads=[("pt", pb), "bias_bc"], writes=[("ot", pb)])
            elif pb % 2 == 0:
                s.op("act", lambda e: e.copy(out=ot[pb][:, :cw], in_=pt[pb][:, :cw]),
                     reads=[("pt", pb)], writes=[("ot", pb)])
            else:
                s.op("dve", lambda e: e.tensor_copy(out=ot[pb][:, :cw], in_=pt[pb][:, :cw]),
                     reads=[("pt", pb)], writes=[("ot", pb)])
            s.dma("sp" if pb % 2 else "pool", y[t * 128:(t + 1) * 128, n0:n0 + cw], ot[pb][:, :cw],
                  reads=[("ot", pb)], writes=[("y", t, ch)])
    s.finish([("y", t, ch) for t in range(NT) for ch in range(nch)])
    print("ninst", s.ninst)
    s.close()
    return nc


A_DIL = (1, 4, 16)


def atta_bias_table():
    slopes = (2.0 ** (-8.0 * np.arange(1, 17) / 16)).astype(np.float32)
    k = np.arange(128)[:, None]
    q = np.arange(128)[None, :]
    tab = np.zeros((3, 128, 2, 16, 128), np.float32)
    for g, d in enumerate(A_DIL):
        for ch in range(2):
            dist = (q - k + (128 if ch == 0 else 0))
            valid = (dist >= 0) & (dist <= 128)
            for hh in range(16):
                b = -(slopes[hh] * (dist * d).astype(np.float32))
                tab[g, :, ch, hh, :] = np.where(valid, b, -1.0e30)
    return tab


def build_atta(units, nslot):
    nc = bass.Bass("TRN2", target_bir_lowering=False)
    U = len(units)
    qT = nc.dram_tensor("qT", [U, 64, 16, 128], BF16, kind="ExternalInput").ap()
    kT = nc.dram_tensor("kT", [U, 64, 16, 256], BF16, kind="ExternalInput").ap()
    v = nc.dram_tensor("v", [U, 2, 128, 16, 64], BF16, kind="ExternalInput").ap()
    tab = nc.dram_tensor("tab", [nslot, 128, 2, 16, 128], F32, kind="ExternalInput").ap()
    o = nc.dram_tensor("o", [U, 128, 16 * 65], F32, kind="ExternalOutput").ap()
    s = Sched(nc)
    tb = s.sb("tb", [128, 2, 16, 128], F32)
    q_sb = [s.sb("q_sb%d" % i, [64, 16, 128], BF16) for i in range(2)]
    k_sb = [s.sb("k_sb%d" % i, [64, 16, 256], BF16) for i in range(2)]
    v_sb = [s.sb("v_sb%d" % i, [128, 2, 16, 65], BF16) for i in range(2)]
    tmp = [s.sb("tmp%d" % i, [128, 2, 512], F32) for i in range(2)]
    pT = [s.sb("pT%d" % i, [128, 2, 512], BF16) for i in range(2)]
    o_sb = [s.sb("o_sb%d" % i, [128, 16, 65], F32) for i in range(2)]
    ps_s = [[s.ps("ps_s%d_%d" % (i, c), [128, 512], key=("ps_s", i, c)) for c in range(2)] for i in range(2)]
    ps_o = [s.ps("ps_o%d" % i, [128, 4, 128], key=("ps_o", i)) for i in range(2)]
    for i in range(2):
        s.op("pool", lambda e: e.memset(v_sb[i][:, :, :, 64:65], 1.0), writes=[("v_sb", i)])
    okeys = []
    state = {"cur_g": -1}
    items = [(u, hg) for u in range(U) for hg in range(4)]

    def S(n):
        u, hg = items[n]
        g = units[u]
        b = u % 2
        i = n % 2
        if hg == 0:
            if g != state["cur_g"]:
                s.dma("sp", tb[:, :, :, :], tab[g], writes=["tb"])
                state["cur_g"] = g
            s.dma("sp", q_sb[b][:, :, :], qT[u], writes=[("q_sb", b)])
            s.dma("sp", k_sb[b][:, :, :], kT[u], writes=[("k_sb", b)])
            for c in range(2):
                s.dma("sp", v_sb[b][:, c, :, 0:64], v[u, c], writes=[("v_sb", b)], reads=[("v_sb", b)])
        for c in range(2):
            for hh in range(4):
                hd = hg * 4 + hh
                s.op("pe", lambda e: e.matmul(ps_s[i][c][:, hh * 128:(hh + 1) * 128],
                                              lhsT=k_sb[b][:, hd, c * 128:(c + 1) * 128], rhs=q_sb[b][:, hd, :],
                                              start=True, stop=True),
                     reads=[("k_sb", b), ("q_sb", b)], writes=[("ps_s", i, c)])
            s.op("dve", lambda e: e.scalar_tensor_tensor(
                out=tmp[i][:, c, :], in0=ps_s[i][c][:, :], scalar=0.125,
                in1=tb[:, c, hg * 4:hg * 4 + 4, :].rearrange("p a b -> p (a b)"),
                op0=ALU.mult, op1=ALU.add),
                reads=[("ps_s", i, c), "tb"], writes=[("tmp", i, c)])
            s.op("act", lambda e: e.activation(out=pT[i][:, c, :], in_=tmp[i][:, c, :], func=AF.Exp),
                 reads=[("tmp", i, c)], writes=[("pT", i, c)])

    def PV(n):
        u, hg = items[n]
        b = u % 2
        i = n % 2
        for hh in range(4):
            hd = hg * 4 + hh
            for c in range(2):
                s.op("pe", lambda e: e.matmul(ps_o[i][:, hh, 0:65], lhsT=pT[i][:, c, hh * 128:(hh + 1) * 128],
                                              rhs=v_sb[b][:, c, hd, :], start=(c == 0), stop=(c == 1)),
                     reads=[("pT", i, c), ("v_sb", b)], writes=[("ps_o", i)])
        s.op("dve" if hg % 2 else "act", (lambda e: e.tensor_copy(out=o_sb[b][:, hg * 4:hg * 4 + 4, :], in_=ps_o[i][:, :, 0:65])) if hg % 2
             else (lambda e: e.copy(out=o_sb[b][:, hg * 4:hg * 4 + 4, :], in_=ps_o[i][:, :, 0:65])),
             reads=[("ps_o", i)], writes=[("o_sb", b)])
        if hg == 3:
            s.dma("sp", o[u], o_sb[b][:, :, :].rearrange("p a b -> p (a b)"), reads=[("o_sb", b)], writes=[("o", u)])
            okeys.append(("o", u))

    S(0)
    for n in range(len(items)):
        if n + 1 < len(items):
            S(n + 1)
        PV(n)
    s.finish(okeys)
    print("atta ninst", s.ninst)
    s.close()
    return nc


def atta_host_prep(qkv):
    S = qkv.shape[0]
    x = qkv.reshape(S, 3, 3, 16, 64)
    qTs, kTs, vs, meta = [], [], [], []
    for g, d in enumerate(A_DIL):
        L = S // d
        nb = L // 128
        def perm(a):
            return a.reshape(L, d, 16, 64).transpose(1, 0, 2, 3)
        q = perm(x[:, g, 0]).reshape(d, nb, 128, 16, 64)
        k = perm(x[:, g, 1])
        vv = perm(x[:, g, 2])
        z = np.zeros((d, 128, 16, 64), qkv.dtype)
        kp = np.concatenate([z, k], axis=1).reshape(d, nb + 1, 128, 16, 64)
        vp = np.concatenate([z, vv], axis=1).reshape(d, nb + 1, 128, 16, 64)
        qT = q.transpose(0, 1, 4, 3, 2).reshape(d * nb, 64, 16, 128)
        k2 = np.stack([kp[:, :-1], kp[:, 1:]], axis=2)
        kT = k2.transpose(0, 1, 5, 4, 2, 3).reshape(d * nb, 64, 16, 256)
        v2 = np.stack([vp[:, :-1], vp[:, 1:]], axis=2).reshape(d * nb, 2, 128, 16, 64)
        qTs.append(np.ascontiguousarray(qT))
        kTs.append(np.ascontiguousarray(kT))
        vs.append(np.ascontiguousarray(v2))
        meta.append([(g, (i % nb) == 0) for i in range(d * nb)])
    return qTs, kTs, vs, meta


def atta_host_post(o_groups, S):
    out = []
    for g, d in enumerate(A_DIL):
        L = S // d
        a = o_groups[g].reshape(d, L, 1040).transpose(1, 0, 2).reshape(S, 1040)
        out.append(a)
    return np.stack(out)


ALPHA = 8.0 ** 0.25
LN_EPS = 1e-5
BIG = 1.0e30


def build_post(T, modeA):
    nc = bass.Bass("TRN2", target_bir_lowering=False)
    D = 1024
    NTL = T // 128
    SG = min(1024, T)
    NSG = T // SG
    TPS = SG // 128

    def din(name, shape, dt=F32):
        return nc.dram_tensor(name, shape, dt, kind="ExternalInput").ap()

    h = din("h", [T, D])
    if modeA:
        og = din("og", [3, T, 16 * 65])
    else:
        og = din("og", [3, T, 16 * 65])
        gl = din("gl", [T, 48], BF16)
    wout = din("wout", [D, D])
    lnp = din("lnp", [4, D])
    wr = din("wr", [D, 20])
    br = din("br", [20])
    wgu = din("wgu", [16, D, 512])
    wd = din("wd", [16, 256, D])
    ident = din("ident", [128, 128])
    sel = din("sel", [128, 16 * 128])
    hout = nc.dram_tensor("hout", [T, D], F32, kind="ExternalOutput").ap()

    s = Sched(nc)
    ident_f = s.sb("ident_f", [128, 128], F32)
    ident_b = s.sb("ident_b", [128, 128], BF16)
    sel_f = s.sb("sel_f", [128, 16 * 128], F32)
    lnbc = s.sb("lnbc", [128, 4, D], F32)
    br_bc = s.sb("br_bc", [128, 20], F32)
    wr_f = s.sb("wr_f", [128, 8, 20], F32)
    wout_b = s.sb("wout_b", [128, 8, D], BF16)
    eps_t = s.sb("eps_t", [128, 1], F32)
    stage = [s.sb("stage%d" % i, [128, 4096], F32) for i in range(2)]
    wgu_b = [s.sb("wgu_b%d" % i, [128, 8, 512], BF16) for i in range(2)]
    wd_b = [s.sb("wd_b%d" % i, [128, 2, D], BF16) for i in range(2)]
    nstage = [0]

    def stage_load(src_ap, n_inner, dst_ap, dkey):
        b = nstage[0] % 2
        nstage[0] += 1
        sv = stage[b][:, :src_ap.shape[1] * src_ap.shape[2]].rearrange("p (a b) -> p a b", a=src_ap.shape[1])
        s.dma("sp", sv, src_ap, writes=[("stage", b)])
        s.op("pool", lambda e: e.tensor_copy(out=dst_ap, in_=sv), reads=[("stage", b)], writes=[dkey])

    s.dma("sp", ident_f[:, :], ident, writes=["ident_f"])
    s.op("dve", lambda e: e.tensor_copy(out=ident_b[:, :], in_=ident_f[:, :]), reads=["ident_f"], writes=["ident_b"])
    s.dma("sp", sel_f[:, :], sel, writes=["sel_f"])
    for i in range(4):
        s.dma("sp", lnbc[:, i, :], lnp[i].partition_broadcast(128), writes=[("lnbc", i)])
    s.dma("sp", br_bc[:, :], br.partition_broadcast(128), writes=["br_bc"])
    s.dma("sp", wr_f[:, :, :], wr.rearrange("(kc p) n -> p kc n", p=128), writes=["wr_f"])
    s.op("dve", lambda e: e.memset(eps_t[:, :], LN_EPS), writes=["eps_t"])
    woutv = wout.rearrange("(kc p) n -> p kc n", p=128)
    for i in range(2):
        stage_load(woutv[:, 4 * i:4 * i + 4, :], None, wout_b[:, 4 * i:4 * i + 4, :], ("wout_b", i))

    h_t = s.sb("h_t", [128, D], F32)
    if modeA:
        og_t = s.sb("og_t", [128, 3, 16 * 65], F32)
        acc = s.sb("acc", [128, 16, 65], F32)
        rl = s.sb("rl", [128, 16], F32)
    else:
        og_t = s.sb("og_t", [128, 3, 16 * 65], F32)
        gl_t = s.sb("gl_t", [128, 48], BF16)
        gate = s.sb("gate", [128, 3, 16], F32)
        rl3 = s.sb("rl3", [128, 3, 16], F32)

    o_b = s.sb("o_b", [128, D], BF16)
    oT = s.sb("oT", [128, 8, 128], BF16)
    r = s.sb("r", [128, D], F32)
    xn = s.sb("xn", [128, D], F32)
    tA = r[:, :].rearrange("p (a b) -> p a b", a=16)
    tB = xn[:, :].rearrange("p (a b) -> p a b", a=16)
    h1 = s.sb("h1", [128, D], F32)
    st = s.sb("st", [128, 2, 6], F32)
    mv = s.sb("mv", [128, 2], F32)
    rstd = s.sb("rstd", [128, 1], F32)
    yacc = s.sb("yacc", [128, TPS, D], F32)
    h1T_b = s.sb("h1T_b", [128, 8, SG], BF16)
    h1T_f = s.sb("h1T_f", [128, 8, 128], F32)
    combT = s.sb("combT", [128, SG], F32)
    L = s.sb("L", [128, 20], F32)
    sm = s.sb("sm", [128, 16], F32)
    goh = s.sb("goh", [128, 4], F32)
    em = s.sb("em", [128, 4, 4], F32)
    top8 = s.sb("top8", [128, 8], F32)
    c0 = s.sb("c0", [128, 16], F32)
    c1 = s.sb("c1", [128, 16], F32)
    comb = s.sb("comb", [128, 128], F32)
    s.op("dve", lambda e: e.memset(comb[:, :], 0.0), writes=["comb"])
    sg = [s.sb("sg%d" % i, [128, 512], F32) for i in range(2)]
    tt_ = [s.sb("tt%d" % i, [128, 512], F32) for i in range(2)]
    hid2 = [[s.sb("hid%d_%d" % (p, i), [128, 512], BF16) for i in range(2)] for p in range(2)]
    cb2 = [s.sb("cb2_%d" % p, [128, 512], F32) for p in range(2)]
    gu = [s.ps("gu%d" % i, [128, 512], key=("gu", i)) for i in range(4)]
    pcb = s.ps("pcb", [128, 512])
    py = [s.ps("py%d" % i, [128, 512], key=("py", i)) for i in range(2)]
    pT_b = s.ps("pT_b", [128, 1024], BF16)

    def layer_norm(src, src_key, gi, dst, dst_key):
        for i in range(2):
            s.op("dve", lambda e: e.bn_stats(out=st[:, i, :], in_=src[:, i * 512:(i + 1) * 512]),
                 reads=[src_key], writes=[("st", i)])
        s.op("dve", lambda e: e.bn_aggr(out=mv[:, :], in_=st[:, :, :]), reads=[("st", 0), ("st", 1)], writes=["mv"])
        s.op("act", lambda e: e.activation(out=rstd[:, :], in_=mv[:, 1:2], func=AF.Sqrt, bias=eps_t[:, :], scale=1.0),
             reads=["mv", "eps_t"], writes=["rstd"])
        s.op("dve", lambda e: e.reciprocal(out=rstd[:, :], in_=rstd[:, :]), reads=["rstd"], writes=["rstd"])
        s.op("dve", lambda e: e.tensor_scalar(out=xn[:, :], in0=src[:, :], scalar1=mv[:, 0:1], scalar2=rstd[:, 0:1],
                                              op0=ALU.subtract, op1=ALU.mult),
             reads=[src_key, "mv", "rstd"], writes=["xn"])
        s.op("pool", lambda e: e.tensor_tensor(out=xn[:, :], in0=xn[:, :], in1=lnbc[:, gi, :], op=ALU.mult),
             reads=["xn", ("lnbc", gi)], writes=["xn"])
        s.op("pool", lambda e: e.tensor_tensor(out=dst, in0=xn[:, :], in1=lnbc[:, gi + 1, :], op=ALU.add),
             reads=["xn", ("lnbc", gi + 1)], writes=[dst_key])

    STOP = 99

    class StopBuild(Exception):
        pass

    def ck(n):
        if STOP == n:
            raise StopBuild()

    out_keys = []
    try:
      ck(1)
      for sgi in range(NSG):
          for ti in range(TPS):
              gt = sgi * TPS + ti
              rows = slice(gt * 128, (gt + 1) * 128)
              s.dma("sp", h_t[:, :], h[rows, :], writes=["h_t"])
              if modeA:
                  s.dma("sp", og_t[:, :, :], og[:, rows, :].rearrange("g t c -> t g c"), writes=["og_t"])
                  accf = acc[:, :, :].rearrange("p a b -> p (a b)")
                  s.op("dve", lambda e: e.tensor_tensor(out=accf, in0=og_t[:, 0, :], in1=og_t[:, 1, :], op=ALU.add),
                       reads=["og_t"], writes=["acc"])
                  s.op("dve", lambda e: e.tensor_tensor(out=accf, in0=accf, in1=og_t[:, 2, :], op=ALU.add),
                       reads=["og_t", "acc"], writes=["acc"])
                  s.op("dve", lambda e: e.reciprocal(out=rl[:, :], in_=acc[:, :, 64]), reads=["acc"], writes=["rl"])
                  s.op("dve", lambda e: e.tensor_tensor(out=o_b[:, :].rearrange("p (a b) -> p a b", a=16),
                                                        in0=acc[:, :, 0:64],
                                                        in1=rl[:, :].unsqueeze(2).to_broadcast([128, 16, 64]),
                                                        op=ALU.mult),
                       reads=["acc", "rl"], writes=["o_b"])
              else:
                  s.dma("sp", og_t[:, :, :], og[:, rows, :].rearrange("g t c -> t g c"), writes=["og_t"])
                  s.dma("sp", gl_t[:, :], gl[rows, :], writes=["gl_t"])
                  s.op("act", lambda e: e.activation(out=gate[:, :, :].rearrange("p a b -> p (a b)"), in_=gl_t[:, :], func=AF.Sigmoid),
                       reads=["gl_t"], writes=["gate"])
                  ogv = og_t[:, :, :].rearrange("p g (h c) -> p g h c", c=65)
                  s.op("dve", lambda e: e.tensor_scalar(out=rl3[:, :, :], in0=ogv[:, :, :, 64], scalar1=1.0e-30, scalar2=None, op0=ALU.max),
                       reads=["og_t"], writes=["rl3"])
                  s.op("dve", lambda e: e.reciprocal(out=rl3[:, :, :], in_=rl3[:, :, :]), reads=["rl3"], writes=["rl3"])
                  s.op("dve", lambda e: e.tensor_tensor(out=rl3[:, :, :], in0=rl3[:, :, :], in1=gate[:, :, :], op=ALU.mult),
                       reads=["rl3", "gate"], writes=["rl3"])
                  s.op("dve", lambda e: e.tensor_tensor(out=tA[:, :, :], in0=ogv[:, 0, :, 0:64],
                                                        in1=rl3[:, 0, :].unsqueeze(2).to_broadcast([128, 16, 64]), op=ALU.mult),
                       reads=["og_t", "rl3"], writes=["r"])
                  s.op("dve", lambda e: e.tensor_tensor(out=tB[:, :, :], in0=ogv[:, 1, :, 0:64],
                                                        in1=rl3[:, 1, :].unsqueeze(2).to_broadcast([128, 16, 64]), op=ALU.mult),
                       reads=["og_t", "rl3"], writes=["xn"])
                  s.op("dve", lambda e: e.tensor_tensor(out=tA[:, :, :], in0=tA[:, :, :], in1=tB[:, :, :], op=ALU.add),
                       reads=["r", "xn"], writes=["r"])
                  s.op("dve", lambda e: e.tensor_tensor(out=tB[:, :, :], in0=ogv[:, 2, :, 0:64],
                                                        in1=rl3[:, 2, :].unsqueeze(2).to_broadcast([128, 16, 64]), op=ALU.mult),
                       reads=["og_t", "rl3"], writes=["xn"])
                  s.op("dve", lambda e: e.tensor_tensor(out=o_b[:, :].rearrange("p (a b) -> p a b", a=16), in0=tA[:, :, :], in1=tB[:, :, :], op=ALU.add),
                       reads=["r", "xn"], writes=["o_b"])
              for kc in range(8):
                  s.op("pe", lambda e: e.transpose(out=pT_b[:, kc * 128:(kc + 1) * 128],
                                                   in_=o_b[:, kc * 128:(kc + 1) * 128], identity=ident_b[:, :]),
                       reads=["o_b", "ident_b"], writes=["pT_b"])
              s.op("act", lambda e: e.copy(out=oT[:, :, :].rearrange("p a b -> p (a b)"), in_=pT_b[:, :]),
                   reads=["pT_b"], writes=["oT"])
              for hh in range(2):
                  for kc in range(8):
                      s.op("pe", lambda e: e.matmul(gu[hh][:, :], lhsT=oT[:, kc, :],
                                                    rhs=wout_b[:, kc, hh * 512:(hh + 1) * 512],
                                                    start=(kc == 0), stop=(kc == 7)),
                           reads=["oT", ("wout_b", kc // 4)], writes=[("gu", hh)])
                  s.op("dve", lambda e: e.scalar_tensor_tensor(out=r[:, hh * 512:(hh + 1) * 512],
                                                               in0=h_t[:, hh * 512:(hh + 1) * 512], scalar=ALPHA,
                                                               in1=gu[hh][:, :], op0=ALU.mult, op1=ALU.add),
                       reads=["h_t", ("gu", hh)], writes=["r"])
              ck(2)
              layer_norm(r, "r", 0, h1[:, :], "h1")
              ck(3)
              s.op("act", lambda e: e.mul(out=yacc[:, ti, :], in_=h1[:, :], mul=ALPHA),
                   reads=["h1"], writes=[("yacc", ti)])
              ck(31)
              for q in range(2):
                  for j in range(4):
                      kc = q * 4 + j
                      s.op("pe", lambda e: e.transpose(out=gu[2 + q][:, j * 128:(j + 1) * 128],
                                                       in_=h1[:, kc * 128:(kc + 1) * 128], identity=ident_f[:, :]),
                           reads=["h1", "ident_f"], writes=[("gu", 2 + q)])
                  ck(32)
                  s.op("act", lambda e: e.copy(out=h1T_f[:, q * 4:q * 4 + 4, :],
                                               in_=gu[2 + q][:, :].rearrange("p (a b) -> p a b", a=4)),
                       reads=[("gu", 2 + q)], writes=[("h1T_f", q)])
                  ck(33)
                  s.op("dve", lambda e: e.tensor_copy(out=h1T_b[:, q * 4:q * 4 + 4, ti * 128:(ti + 1) * 128],
                                                      in_=gu[2 + q][:, :].rearrange("p (a b) -> p a b", a=4)),
                       reads=[("gu", 2 + q)], writes=[("h1T_b", ti)])
              ck(4)
              for kc in range(8):
                  s.op("pe", lambda e: e.matmul(pcb[:, 0:20], lhsT=h1T_f[:, kc, :], rhs=wr_f[:, kc, :],
                                                start=(kc == 0), stop=(kc == 7)),
                       reads=[("h1T_f", kc // 4), "wr_f"], writes=["pcb"])
              s.op("dve", lambda e: e.tensor_tensor(out=L[:, :], in0=pcb[:, 0:20], in1=br_bc[:, :], op=ALU.add),
                   reads=["pcb", "br_bc"], writes=["L"])
              ck(5)
              V = lambda eng, fn, rd, wr_: s.op(eng, fn, reads=rd, writes=wr_)
              V("dve", lambda e: e.reduce_max(out=sm[:, 0:1], in_=L[:, 0:4], axis=AX.X), ["L"], ["sm"])
              V("dve", lambda e: e.tensor_scalar(out=sm[:, 1:2], in0=sm[:, 0:1], scalar1=-1.0, scalar2=None, op0=ALU.mult),
                ["sm"], ["sm"])
              V("act", lambda e: e.activation(out=goh[:, :], in_=L[:, 0:4], func=AF.Exp, bias=sm[:, 1:2], scale=1.0),
                ["L", "sm"], ["goh"])
              V("dve", lambda e: e.reduce_sum(out=sm[:, 2:3], in_=goh[:, :], axis=AX.X), ["goh"], ["sm"])
              V("dve", lambda e: e.reciprocal(out=sm[:, 3:4], in_=sm[:, 2:3]), ["sm"], ["sm"])
              V("dve", lambda e: e.tensor_scalar(out=goh[:, :], in0=L[:, 0:4], scalar1=sm[:, 0:1], scalar2=None,
                                                 op0=ALU.is_equal), ["L", "sm", "goh"], ["goh"])
              V("dve", lambda e: e.tensor_scalar(out=goh[:, :], in0=goh[:, :], scalar1=BIG, scalar2=BIG,
                                                 op0=ALU.mult, op1=ALU.subtract), ["goh"], ["goh"])
              V("dve", lambda e: e.tensor_tensor(out=em[:, :, :], in0=L[:, 4:20].rearrange("p (a b) -> p a b", a=4),
                                                 in1=goh[:, :].unsqueeze(2).to_broadcast([128, 4, 4]), op=ALU.add),
                ["L", "goh"], ["em"])
              emf = em[:, :, :].rearrange("p a b -> p (a b)")
              V("dve", lambda e: e.max(out=top8[:, :], in_=emf), ["em"], ["top8"])
              V("dve", lambda e: e.tensor_tensor(out=sm[:, 4:5], in0=top8[:, 1:2], in1=top8[:, 0:1], op=ALU.subtract),
                ["top8", "sm"], ["sm"])
              V("act", lambda e: e.activation(out=sm[:, 5:6], in_=sm[:, 4:5], func=AF.Exp), ["sm"], ["sm"])
              V("dve", lambda e: e.tensor_scalar(out=sm[:, 6:7], in0=sm[:, 5:6], scalar1=1.0, scalar2=None, op0=ALU.add),
                ["sm"], ["sm"])
              V("dve", lambda e: e.reciprocal(out=sm[:, 6:7], in_=sm[:, 6:7]), ["sm"], ["sm"])
              V("dve", lambda e: e.tensor_tensor(out=sm[:, 7:8], in0=sm[:, 6:7], in1=sm[:, 3:4], op=ALU.mult), ["sm"], ["sm"])
              V("dve", lambda e: e.tensor_tensor(out=sm[:, 8:9], in0=sm[:, 7:8], in1=sm[:, 5:6], op=ALU.mult), ["sm"], ["sm"])
              V("dve", lambda e: e.tensor_scalar(out=c0[:, :], in0=emf, scalar1=top8[:, 0:1], scalar2=sm[:, 7:8],
                                                 op0=ALU.is_equal, op1=ALU.mult), ["em", "top8", "sm"], ["c0"])
              V("dve", lambda e: e.tensor_scalar(out=c1[:, :], in0=emf, scalar1=top8[:, 1:2], scalar2=sm[:, 8:9],
                                                 op0=ALU.is_equal, op1=ALU.mult), ["em", "top8", "sm"], ["c1"])
              V("dve", lambda e: e.tensor_tensor(out=comb[:, 0:16], in0=c0[:, :], in1=c1[:, :], op=ALU.add),
                ["c0", "c1"], ["comb"])
              ck(6)
              V("pe", lambda e: e.transpose(out=pcb[:, 128:256], in_=comb[:, :], identity=ident_f[:, :]),
                ["comb", "ident_f"], ["pcb"])
              V("act", lambda e: e.copy(out=combT[:, ti * 128:(ti + 1) * 128], in_=pcb[:, 128:256]),
                ["pcb"], [("combT", ti)])

          ck(7)
          NSUB = SG // 512
          units = [(ex, sub) for ex in range(16) for sub in range(NSUB)]

          def load_w(ex):
              wb = ex % 2
              stage_load(wgu[ex].rearrange("(kc p) n -> p kc n", p=128), None, wgu_b[wb][:, :, :], ("wgu_b", wb))
              stage_load(wd[ex].rearrange("(fc p) n -> p fc n", p=128), None, wd_b[wb][:, :, :], ("wd_b", wb))

          def GU(n, fc):
              ex, sub = units[n]
              wb = ex % 2
              tok = slice(sub * 512, (sub + 1) * 512)
              tkeys = [("h1T_b", sub * 4 + i) for i in range(4)]
              for j in (fc, 2 + fc):
                  for kc in range(8):
                      s.op("pe", lambda e: e.matmul(gu[j][:, :], lhsT=wgu_b[wb][:, kc, j * 128:(j + 1) * 128],
                                                    rhs=h1T_b[:, kc, tok], start=(kc == 0), stop=(kc == 7)),
                           reads=[("wgu_b", wb)] + tkeys, writes=[("gu", j)])

          def CB(n):
              ex, sub = units[n]
              tok = slice(sub * 512, (sub + 1) * 512)
              s.op("pe", lambda e: e.matmul(pcb[:, :], lhsT=sel_f[:, ex * 128:(ex + 1) * 128], rhs=combT[:, tok],
                                            start=True, stop=True),
                   reads=["sel_f"] + [("combT", sub * 4 + i) for i in range(4)], writes=["pcb"])
              s.op("act", lambda e: e.copy(out=cb2[n % 2][:, :], in_=pcb[:, :]), reads=["pcb"], writes=[("cb", n % 2)])

          def EW(n, fc):
              p = n % 2
              s.op("act", lambda e: e.activation(out=sg[fc][:, :], in_=gu[fc][:, :], func=AF.Silu),
                   reads=[("gu", fc)], writes=[("sg", fc)])
              s.op("dve", lambda e: e.tensor_tensor(out=tt_[fc][:, :], in0=gu[2 + fc][:, :], in1=cb2[p][:, :], op=ALU.mult),
                   reads=[("gu", 2 + fc), ("cb", p)], writes=[("tt", fc)])
              s.op("pool", lambda e: e.tensor_tensor(out=hid2[p][fc][:, :], in0=sg[fc][:, :], in1=tt_[fc][:, :], op=ALU.mult),
                   reads=[("sg", fc), ("tt", fc)], writes=[("hid", p, fc)])

          def DOWN(n):
              ex, sub = units[n]
              wb = ex % 2
              p = n % 2
              for t4 in range(4):
                  ti = sub * 4 + t4
                  for hh in range(2):
                      for fc in range(2):
                          s.op("pe", lambda e: e.matmul(py[hh][:, :], lhsT=hid2[p][fc][:, t4 * 128:(t4 + 1) * 128],
                                                        rhs=wd_b[wb][:, fc, hh * 512:(hh + 1) * 512],
                                                        start=(fc == 0), stop=(fc == 1)),
                               reads=[("hid", p, fc), ("wd_b", wb)], writes=[("py", hh)])
                      s.op("dve", lambda e: e.tensor_tensor(out=yacc[:, ti, hh * 512:(hh + 1) * 512],
                                                            in0=yacc[:, ti, hh * 512:(hh + 1) * 512],
                                                            in1=py[hh][:, :], op=ALU.add),
                           reads=[("yacc", ti), ("py", hh)], writes=[("yacc", ti)])

          load_w(0)
          GU(0, 0); CB(0); EW(0, 0); GU(0, 1); EW(0, 1)
          for n in range(len(units)):
              ex, sub = units[n]
              if sub == 0 and ex + 1 < 16:
                  load_w(ex + 1)
              if n + 1 < len(units):
                  GU(n + 1, 0); CB(n + 1); EW(n + 1, 0)
              DOWN(n)
              if n + 1 < len(units):
                  GU(n + 1, 1); EW(n + 1, 1)
          ck(8)
          for ti in range(TPS):
              gt = sgi * TPS + ti
              layer_norm(yacc[:, ti, :], ("yacc", ti), 2, h1[:, :], "h1")
              s.dma("sp", hout[gt * 128:(gt + 1) * 128, :], h1[:, :], reads=["h1"], writes=[("hout", gt)])
              out_keys.append(("hout", gt))
    except StopBuild:
        pass
    s.finish(out_keys)
    print("post ninst", s.ninst)
    s.close()
    return nc


def post_consts():
    ident = np.eye(128, dtype=np.float32)
    sel = np.zeros((128, 16 * 128), np.float32)
    for e in range(16):
        sel[e, e * 128:(e + 1) * 128] = 1.0
    return {"ident": ident, "sel": sel}


def build_cmp():
    nc = bass.Bass("TRN2", target_bir_lowering=False)

    def din(name, shape, dt=F32):
        return nc.dram_tensor(name, shape, dt, kind="ExternalInput").ap()
    xT = din("xT", [2048, 1024], BF16)
    pos = din("pos", [2048])
    w1 = din("w1", [2048, 256])
    b1 = din("b1", [256])
    w2 = din("w2", [256, 64])
    b2 = din("b2", [64])
    outT = nc.dram_tensor("outT", [64, 1024], BF16, kind="ExternalOutput").ap()
    s = Sched(nc)
    x_in = s.sb("x_in", [128, 16, 1024], BF16)
    x_b = s.sb("x_b", [128, 16, 1024], BF16)
    pos_sb = s.sb("pos_sb", [128, 16], F32)
    w1_f = s.sb("w1_f", [128, 16, 256], F32)
    w1_b = s.sb("w1_b", [128, 16, 256], BF16)
    b1_sb = s.sb("b1_sb", [128, 2], F32)
    w2_f = s.sb("w2_f", [128, 2, 64], F32)
    w2_b = s.sb("w2_b", [128, 2, 64], BF16)
    b2_sb = s.sb("b2_sb", [64, 1], F32)
    xh = s.sb("xh", [128, 512], F32)
    x2 = s.sb("x2", [128, 512], F32)
    sg = s.sb("sg", [128, 512], F32)
    hid = s.sb("hid", [128, 2, 1024], BF16)
    o_sb = s.sb("o_sb", [64, 1024], BF16)
    ph = [s.ps("ph%d" % i, [128, 512], key=("ph", i)) for i in range(2)]
    po = s.ps("po", [128, 512], key="po")
    s.dma("sp", x_in[:, :, :], xT.rearrange("(kc p) n -> p kc n", p=128), writes=["x_in"])
    s.dma("sp", pos_sb[:, :], pos.rearrange("(kc p) -> p kc", p=128), writes=["pos_sb"], allow_slow_non_contiguous=True)
    s.dma("sp", w1_f[:, :, :], w1.rearrange("(kc p) n -> p kc n", p=128), writes=["w1_f"])
    s.dma("sp", b1_sb[:, :], b1.rearrange("(fc p) -> p fc", p=128), writes=["b1_sb"], allow_slow_non_contiguous=True)
    s.dma("sp", w2_f[:, :, :], w2.rearrange("(fc p) n -> p fc n", p=128), writes=["w2_f"])
    s.dma("sp", b2_sb[:, :], b2.rearrange("(p o) -> p o", o=1), writes=["b2_sb"])
    s.op("pool", lambda e: e.tensor_copy(out=w1_b[:, :, :], in_=w1_f[:, :, :]), reads=["w1_f"], writes=["w1_b"])
    s.op("pool", lambda e: e.tensor_copy(out=w2_b[:, :, :], in_=w2_f[:, :, :]), reads=["w2_f"], writes=["w2_b"])
    for kc in range(16):
        s.op("dve", lambda e: e.tensor_scalar(out=x_b[:, kc, :], in0=x_in[:, kc, :], scalar1=pos_sb[:, kc:kc + 1], scalar2=None,
                                              op0=ALU.add), reads=["x_in", "pos_sb"], writes=[("x_b", kc)])
    xkeys = [("x_b", kc) for kc in range(16)]
    k = 0
    for nch in range(2):
        ns = slice(nch * 512, (nch + 1) * 512)
        for fc in range(2):
            p = ph[k % 2]
            pk = ("ph", k % 2)
            k += 1
            for kc in range(16):
                s.op("pe", lambda e: e.matmul(p[:, :], lhsT=w1_b[:, kc, fc * 128:(fc + 1) * 128], rhs=x_b[:, kc, ns],
                                              start=(kc == 0), stop=(kc == 15)), reads=["w1_b"] + xkeys, writes=[pk])
            s.op("dve", lambda e: e.tensor_scalar(out=xh[:, :], in0=p[:, :], scalar1=b1_sb[:, fc:fc + 1], scalar2=None, op0=ALU.add),
                 reads=[pk, "b1_sb"], writes=["xh"])
            s.op("pool", lambda e: e.tensor_tensor(out=x2[:, :], in0=xh[:, :], in1=xh[:, :], op=ALU.mult), reads=["xh"], writes=["x2"])
            s.op("dve", lambda e: e.tensor_scalar(out=x2[:, :], in0=x2[:, :], scalar1=0.044715, scalar2=1.0, op0=ALU.mult, op1=ALU.add),
                 reads=["x2"], writes=["x2"])
            s.op("pool", lambda e: e.tensor_tensor(out=x2[:, :], in0=x2[:, :], in1=xh[:, :], op=ALU.mult), reads=["x2", "xh"], writes=["x2"])
            s.op("act", lambda e: e.activation(out=sg[:, :], in_=x2[:, :], func=AF.Sigmoid, scale=1.5957691216057308),
                 reads=["x2"], writes=["sg"])
            s.op("dve", lambda e: e.tensor_tensor(out=hid[:, fc, ns], in0=xh[:, :], in1=sg[:, :], op=ALU.mult),
                 reads=["xh", "sg"], writes=[("hid", fc, nch)])
        for fc in range(2):
            s.op("pe", lambda e: e.matmul(po[0:64, :], lhsT=w2_b[:, fc, :], rhs=hid[:, fc, ns], start=(fc == 0), stop=(fc == 1)),
                 reads=["w2_b", ("hid", fc, nch)], writes=["po"])
        s.op("dve", lambda e: e.tensor_scalar(out=o_sb[:, ns], in0=po[0:64, :], scalar1=b2_sb[:, 0:1], scalar2=None, op0=ALU.add),
             reads=["po", "b2_sb"], writes=[("o_sb", nch)])
    s.dma("sp", outT, o_sb[:, :], reads=[("o_sb", 0), ("o_sb", 1)], writes=["outT"])
    s.finish(["outT"])
    s.close()
    return nc


NEG = -1.0e30
MNEG = -240000.0


def nsa_tables(S, heads4):
    slopes = (2.0 ** (-8.0 * np.arange(1, 17) / 16)).astype(np.float64)
    sl4 = slopes[list(heads4)]
    kl = np.arange(128)[:, None].astype(np.float64)
    ql = np.arange(128)[None, :].astype(np.float64)
    t = {}
    tabW = np.zeros((5, 128, 2, 128), np.float32)
    for dlt in range(5):
        dist = ql - kl + 128 * dlt
        valid = (dist >= 0) & (dist < 512)
        for h in range(2):
            tabW[dlt, :, h, :] = np.where(valid, -sl4[h] * dist, NEG)
    t["tabW"] = tabW
    tabS = np.zeros((2, 128, 2, 128), np.float32)
    for h in range(2):
        tabS[1, :, h, :] = -sl4[h] * (ql - kl)
        tabS[0, :, h, :] = np.where(ql - kl >= 0, -sl4[h] * (ql - kl), NEG)
    t["tabS"] = tabS
    cst = np.zeros((128, 4, 128), np.float32)
    for h in range(4):
        cst[:, h, :] = (-sl4[h] * 128.0 * np.arange(128))[None, :]
    t["cst"] = cst
    tabC = np.zeros((128, 4, 128), np.float32)
    for h in range(4):
        tabC[:, h, :] = -sl4[h] * (ql - 16 * kl - 31)
    t["tabC"] = tabC
    maskC = np.zeros((17, 128, 128), np.float32)
    for dlt in range(17):
        maskC[dlt] = np.where(128 * dlt + ql - 16 * kl - 31 >= 0, 0.0, MNEG)
    t["maskC"] = maskC
    NQB = S // 128
    nblk = S // 64
    keep = np.ones((NQB, 128, nblk), np.float32)
    force = np.zeros((NQB, 128, nblk), np.float32)
    n = np.arange(nblk)[None, :]
    for i in range(NQB):
        cur = (2 * i + (np.arange(128) >= 64).astype(np.int64))[:, None]
        forced = (n == 0) | (n == cur) | (n == cur - 1)
        fut = n > cur
        keep[i] = np.where(forced | fut, 0.0, 1.0)
        force[i] = np.where(fut, -1.0e4, np.where(forced, 1.0e4, 0.0))
    t["keep"] = keep
    t["force"] = force
    t["identb"] = np.eye(128, dtype=np.float32)
    oh = np.zeros((64, S), np.float32)
    for j in range(S // 128):
        for k in range(128):
            oh[2 * (j % 16) + k // 64, j * 128 + k] = 1.0
        oh[32:35, j * 128:(j + 1) * 128] = 1.0
        oh[35:38, j * 128:(j + 1) * 128] = float(j)
        oh[38:41, j * 128:(j + 1) * 128] = np.arange(128, dtype=np.float32)[None, :]
    t["oh"] = oh.astype(ml_dtypes.bfloat16)
    bf = ml_dtypes.bfloat16
    def split3(x):
        x = np.asarray(x, np.float64)
        h1 = x.astype(np.float32).astype(bf).astype(np.float64)
        h2 = (x - h1).astype(np.float32).astype(bf).astype(np.float64)
        h3 = (x - h1 - h2).astype(np.float32).astype(bf).astype(np.float64)
        return [h1, h2, h3]
    augq = np.zeros((NQB, 9, 2, 128), np.float64)
    qlv = np.arange(128, dtype=np.float64)
    for h in range(2):
        A = -sl4[h] * (1024.0 * np.arange(NQB)[:, None] + 8.0 * qlv[None, :])
        for r, comp in enumerate(split3(A)):
            augq[:, r, h, :] = comp
        for r, comp in enumerate(split3(sl4[h] * 1024.0)):
            augq[:, 3 + r, h, :] = comp
        for r, comp in enumerate(split3(sl4[h] * 8.0)):
            augq[:, 6 + r, h, :] = comp
    t["augq"] = augq.astype(np.float32).astype(bf)
    t["tabS"][0] = np.where(ql - kl >= 0, 0.0, NEG)[:, None, :].repeat(2, axis=1)
    return t


def build_nsa(S):
    nc = bass.Bass("TRN2", target_bir_lowering=False)
    NQB = S // 128
    NCH = S // 128
    NBLK = S // 64
    NHALF = (NBLK + 127) // 128
    NCMP = S // 16
    NCC = (NCMP + 127) // 128
    CW = min(128, NCMP)

    def din(name, shape, dt=F32):
        return nc.dram_tensor(name, shape, dt, kind="ExternalInput").ap()

    qT4 = din("qT4", [NQB, 64, 4, 128], BF16)
    kcT = din("kcT", [64, NCC * 128], BF16)
    vc = din("vc", [NCC * 128, 64], BF16)
    ksT = din("ksT", [64, S], BF16)
    vs = din("vs", [S, 64], BF16)
    kwT = din("kwT", [64, S], BF16)
    vw = din("vw", [S, 64], BF16)
    tabW_d = din("tabW", [5, 128, 2, 128])
    tabS_d = din("tabS", [2, 128, 2, 128])
    cst_d = din("cst", [128, 4, 128])
    tabC_d = din("tabC", [128, 4, 128])
    maskC_d = din("maskC", [17, 128, 128])
    keep_d = din("keep", [NQB, 128, NBLK])
    force_d = din("force", [NQB, 128, NBLK])
    ident_d = din("identb", [128, 128])
    oh_d = din("oh", [64, S], BF16)
    qaug_d = din("qaugsrc", [NQB, 128, 2, 128], BF16)
    o3s = nc.dram_tensor("o3s", [NQB, 65, 2, 128], F32, kind="ExternalOutput").ap()
    o3 = nc.dram_tensor("o3", [NQB, 128, 3, 2, 65], F32, kind="ExternalOutput").ap()

    s = Sched(nc)
    stg = s.sb("stg", [128, 17 * 128], F32)
    tabW = s.sb("tabW_s", [128, 5, 256], F32)
    tabS = s.sb("tabS_s", [128, 2, 256], F32)
    cst = s.sb("cst_s", [128, 4, 128], F32)
    tabC = s.sb("tabC_s", [128, 512], F32)
    maskC = s.sb("maskC_s", [128, 17, 128], BF16)
    identb = s.sb("identb_s", [128, 128], BF16)
    kc_sb = s.sb("kc_sb", [64, NCC * 128], BF16)
    vc_sb = s.sb("vc_sb", [128, NCC, 65], BF16)
    ks_sb = s.sb("ks_sb", [128, S], BF16)
    vs_sb = s.sb("vs_sb", [128, NCH, 65], BF16)
    kw_sb = s.sb("kw_sb", [64, S], BF16)
    vw_sb = s.sb("vw_sb", [128, NCH, 65], BF16)
    s.dma("sp", tabW[:, :, :], tabW_d.rearrange("v p h q -> p v (h q)"), writes=["tabW"])
    s.dma("sp", tabS[:, :, :], tabS_d.rearrange("v p h q -> p v (h q)"), writes=["tabS"])
    s.dma("sp", cst[:, :, :], cst_d, writes=["cst"])
    s.dma("sp", tabC[:, :], tabC_d.rearrange("p h q -> p (h q)"), writes=["tabC"])
    s.dma("sp", stg[:, 0:17 * 128].rearrange("p (v q) -> p v q", v=17), maskC_d.rearrange("v p q -> p v q"), writes=["stg"])
    s.op("dve", lambda e: e.tensor_copy(out=maskC[:, :, :], in_=stg[:, 0:17 * 128].rearrange("p (v q) -> p v q", v=17)),
         reads=["stg"], writes=["maskC"])
    s.dma("sp", stg[:, 0:128], ident_d, writes=["stg"])
    s.op("dve", lambda e: e.tensor_copy(out=identb[:, :], in_=stg[:, 0:128]), reads=["stg"], writes=["identb"])
    s.dma("sp", kc_sb[:, :], kcT, writes=["kc_sb"])
    s.dma("sp", ks_sb[0:64, :], ksT, writes=["ks_sb"])
    s.dma("sp", ks_sb[64:128, :], oh_d, writes=["ks_sb"])
    s.dma("sp", kw_sb[:, :], kwT, writes=["kw_sb"])
    for (vsb, vd, nm, n_) in ((vc_sb, vc, "vc_sb", NCC), (vs_sb, vs, "vs_sb", NCH), (vw_sb, vw, "vw_sb", NCH)):
        s.op("pool", lambda e: e.memset(vsb[:, :, 64:65], 1.0), writes=[nm])
        s.dma("sp", vsb[:, :, 0:64], vd.rearrange("(c p) d -> p c d", p=128), writes=[nm])

    NB = 5
    LA = 3
    q_sb = [s.sb("q_sb%d" % i, [64, 4, 128], BF16) for i in range(2)]
    keep_sb = [s.sb("keep_sb%d" % i, [128, NBLK], F32) for i in range(2)]
    force_sb = [s.sb("force_sb%d" % i, [128, NBLK], F32) for i in range(2)]
    tmp = [s.sb("tmp%d" % i, [128, 512], F32) for i in range(NB)]
    pc = s.sb("pc", [128, NCC, 512], BF16)
    pw = s.sb("pw", [128, 5, 256], BF16)
    psl = [s.sb("psl%d" % i, [128, 256], BF16) for i in range(NB)]
    oc_sb = s.sb("oc_sb", [128, 4, 65], F32)
    rl = s.sb("rl", [128, 4], F32)
    imp = s.sb("imp", [128, NCC * 128], F32)
    sc = s.sb("sc", [128, NBLK], F32)
    sc2 = s.sb("sc2", [128, NBLK], F32)
    t8a = s.sb("t8a", [128, 8], F32)
    t8b = s.sb("t8b", [128, 8], F32)
    NV = (NCH + 15) // 16
    selb = s.sb("selb", [128, max(64 + NBLK, 32 * (NV - 1) + 128)], BF16)
    qaug = [s.sb("qaug%d" % i, [128, NV, 2, 128], BF16) for i in range(2)]
    osT = [s.sb("osT%d" % i, [65, 2, 128], F32) for i in range(2)]
    o_sb = [s.sb("o_sb%d" % i, [128, 3, 2, 65], F32) for i in range(2)]
    ps_s = [s.ps("ps_s%d" % i, [128, 512], key=("ps_s", i)) for i in range(NB)]
    ps_os = [s.ps("ps_os%d" % i, [128, 512], key=("ps_os", i)) for i in range(1)]
    ps_o = s.ps("ps_o", [128, 4, 128], key="ps_o")
    ps_t = s.ps("ps_t", [128, 1024], BF16, key="ps_t")
    s.op("dve", lambda e: e.memset(selb[:, :], 0.0), writes=["selb"])
    for i_ in range(2):
        s.op("pool", lambda e: e.memset(o_sb[i_][:, :, :, :], 0.0), writes=[("o_sb", i_, 0), ("o_sb", i_, 2)])

    sidx = [0]
    okeys = []

    def nextbuf():
        k = sidx[0] % NB
        sidx[0] += 1
        return k

    def sel_steps(i):
        b = i % 2
        steps = []

        def ld():
            s.dma("sp", q_sb[b][:, :, :], qT4[i], writes=[("q_sb", b)])
            s.dma("sp", qaug[b][:, :, :, :], qaug_d[i].unsqueeze(1).to_broadcast([128, NV, 2, 128]), writes=[("qaug", b)])
            s.dma("sp", keep_sb[b][:, :], keep_d[i], writes=[("keep", b)])
            s.dma("sp", force_sb[b][:, :], force_d[i], writes=[("force", b)])
        steps.append(ld)
        mlist = [m for m in range(NCC) if 16 * CW * m + 31 <= 128 * i + 127]

        def cmp_chunk(m):
            k = nextbuf()
            dl = i - 16 * m
            if dl <= 16:
                s.op("pe", lambda e: e.matmul(ps_s[k][:, :], lhsT=identb[:, :],
                                              rhs=maskC[:, dl, :].unsqueeze(1).to_broadcast([128, 4, 128]),
                                              start=True, stop=False),
                     reads=["identb", "maskC"], writes=[("ps_s", k)])
            s.op("pe", lambda e: e.matmul(ps_s[k][:, :], lhsT=kc_sb[:, m * 128:(m + 1) * 128],
                                          rhs=q_sb[b][:, :, :].rearrange("p a b -> p (a b)"), start=(dl > 16), stop=True),
                 reads=["kc_sb", ("q_sb", b)], writes=[("ps_s", k)])
            s.op("dve", lambda e: e.scalar_tensor_tensor(out=tmp[k][:, :], in0=ps_s[k][:, :], scalar=0.125,
                                                         in1=tabC[:, :], op0=ALU.mult, op1=ALU.add),
                 reads=[("ps_s", k), "tabC"], writes=[("tmp", k)])
            for h in range(4):
                s.op("act", lambda e: e.activation(out=pc[:, m, h * 128:(h + 1) * 128], in_=tmp[k][:, h * 128:(h + 1) * 128],
                                                   func=AF.Exp, bias=cst[:, h, dl:dl + 1], scale=1.0),
                     reads=[("tmp", k), "cst"], writes=[("pc", m, h)])
        for m in mlist:
            steps.append(lambda m=m: cmp_chunk(m))

        def cmp_pv():
            for h in range(4):
                for mi, m in enumerate(mlist):
                    s.op("pe", lambda e: e.matmul(ps_o[:, h, 0:65], lhsT=pc[:, m, h * 128:(h + 1) * 128], rhs=vc_sb[:, m, :],
                                                  start=(mi == 0), stop=(mi == len(mlist) - 1)),
                         reads=[("pc", m, h), "vc_sb"], writes=["ps_o"])
            s.op("act", lambda e: e.copy(out=oc_sb[:, :, :], in_=ps_o[:, :, 0:65]), reads=["ps_o"], writes=["oc_sb"])
            s.op("pool", lambda e: e.tensor_copy(out=o_sb[b][:, 0, :, :], in_=oc_sb[:, 0:2, :]), reads=["oc_sb"], writes=[("o_sb", b, 0)])
            s.op("dve", lambda e: e.tensor_scalar(out=rl[:, :], in0=oc_sb[:, :, 64], scalar1=1.0e-30, scalar2=None, op0=ALU.max),
                 reads=["oc_sb"], writes=["rl"])
            s.op("dve", lambda e: e.reciprocal(out=rl[:, :], in_=rl[:, :]), reads=["rl"], writes=["rl"])
            s.op("pool", lambda e: e.memset(imp[:, :], 0.0), writes=["imp"])
        steps.append(cmp_pv)

        def imp_head(h):
            for m in mlist:
                s.op("pe", lambda e: e.transpose(out=ps_t[:, m * 128:(m + 1) * 128], in_=pc[:, m, h * 128:(h + 1) * 128],
                                                 identity=identb[:, :]),
                     reads=[("pc", m, h), "identb"], writes=["ps_t"])
            w = len(mlist) * 128
            s.op("dve", lambda e: e.scalar_tensor_tensor(out=imp[:, 0:w], in0=ps_t[:, 0:w], scalar=rl[:, h:h + 1],
                                                         in1=imp[:, 0:w], op0=ALU.mult, op1=ALU.add),
                 reads=["ps_t", "rl", "imp"], writes=["imp"])
        for h in range(4):
            steps.append(lambda h=h: imp_head(h))
        A = imp[:, :].rearrange("p (n f) -> p n f", f=4)
        nb = NBLK

        def score1():
            s.op("dve", lambda e: e.tensor_tensor(out=sc[:, :], in0=A[:, 0:nb, 0], in1=A[:, 0:nb, 1], op=ALU.add), reads=["imp"], writes=["sc"])
            s.op("dve", lambda e: e.tensor_tensor(out=sc[:, :], in0=sc[:, :], in1=A[:, 0:nb, 2], op=ALU.add), reads=["imp", "sc"], writes=["sc"])
            s.op("dve", lambda e: e.scalar_tensor_tensor(out=sc[:, :], in0=sc[:, :], scalar=2.0, in1=A[:, 0:nb, 3],
                                                         op0=ALU.mult, op1=ALU.add), reads=["imp", "sc"], writes=["sc"])
            s.op("dve", lambda e: e.tensor_tensor(out=sc[:, 1:nb], in0=sc[:, 1:nb], in1=A[:, 0:nb - 1, 3], op=ALU.add),
                 reads=["imp", "sc"], writes=["sc"])

        def score2():
            s.op("dve", lambda e: e.tensor_tensor(out=sc[:, :], in0=sc[:, :], in1=keep_sb[b][:, :], op=ALU.mult),
                 reads=["sc", ("keep", b)], writes=["sc"])
            s.op("dve", lambda e: e.tensor_tensor(out=sc[:, :], in0=sc[:, :], in1=force_sb[b][:, :], op=ALU.add),
                 reads=["sc", ("force", b)], writes=["sc"])
            s.op("dve", lambda e: e.max(out=t8a[:, :], in_=sc[:, :]), reads=["sc"], writes=["t8a"])

        def score3():
            s.op("dve", lambda e: e.match_replace(out=sc2[:, :], in_to_replace=t8a[:, :], in_values=sc[:, :], imm_value=-3.0e4),
                 reads=["sc", "t8a"], writes=["sc2"])
            s.op("dve", lambda e: e.max(out=t8b[:, :], in_=sc2[:, :]), reads=["sc2"], writes=["t8b"])
            s.op("dve", lambda e: e.tensor_scalar(out=sc2[:, :], in0=sc[:, :], scalar1=t8b[:, 7:8], scalar2=-MNEG,
                                                  op0=ALU.is_ge, op1=ALU.mult), reads=["sc", "t8b"], writes=["sc2"])
            s.op("dve", lambda e: e.tensor_scalar(out=selb[:, 64:64 + nb], in0=sc2[:, :], scalar1=MNEG, scalar2=None, op0=ALU.add),
                 reads=["sc2"], writes=["selb"])

        def score4():
            for v_ in range(NV):
                s.op("pe", lambda e: e.transpose(out=ps_t[:, v_ * 128:(v_ + 1) * 128], in_=selb[:, 32 * v_:32 * v_ + 128],
                                                 identity=identb[:, :]), reads=["selb", "identb"], writes=["ps_t"])
            s.op("act", lambda e: e.copy(out=qaug[b][64:96, :, :, :],
                                         in_=ps_t[64:96, 0:NV * 128].rearrange("p (v q) -> p v q", v=NV).unsqueeze(2).to_broadcast([32, NV, 2, 128])),
                 reads=["ps_t"], writes=[("qaug", b)])
        steps += [score1, score2, score3, score4]
        return steps

    def s_stage(i, j, k):
        b = i % 2
        v_ = j // 16
        s.op("pe", lambda e: e.matmul(ps_s[k][:, 0:256], lhsT=ks_sb[:, j * 128:(j + 1) * 128],
                                      rhs=qaug[b][:, v_, :, :].rearrange("p a b -> p (a b)"), start=True, stop=True),
             reads=["ks_sb", ("qaug", b)], writes=[("ps_s", k)])
        if j == i:
            s.op("dve", lambda e: e.scalar_tensor_tensor(out=tmp[k][:, 0:256], in0=ps_s[k][:, 0:256], scalar=0.125,
                                                         in1=tabS[:, 0, :], op0=ALU.mult, op1=ALU.add),
                 reads=[("ps_s", k), "tabS"], writes=[("tmp", k)])
            s.op("act", lambda e: e.activation(out=psl[k][:, 0:256], in_=tmp[k][:, 0:256], func=AF.Exp),
                 reads=[("tmp", k)], writes=[("psl", k, 0)])
        else:
            s.op("act", lambda e: e.activation(out=psl[k][:, 0:256], in_=ps_s[k][:, 0:256], func=AF.Exp, scale=0.125),
                 reads=[("ps_s", k)], writes=[("psl", k, 0)])

    def pv_stage(i, j, k):
        s.op("pe", lambda e: e.matmul(ps_os[0][0:65, 0:256], lhsT=vs_sb[:, j, :], rhs=psl[k][:, 0:256],
                                      start=(j == 0), stop=(j == i)),
             reads=[("psl", k, 0), "vs_sb"], writes=[("ps_os", 0)])

    def window(i):
        b = i % 2
        wl = [j for j in range(i - 4, i + 1) if j >= 0]
        for wi, j in enumerate(wl):
            k = nextbuf()
            s.op("pe", lambda e: e.matmul(ps_s[k][:, 0:256], lhsT=kw_sb[:, j * 128:(j + 1) * 128],
                                          rhs=q_sb[b][:, 0:2, :].rearrange("p a b -> p (a b)"), start=True, stop=True),
                 reads=["kw_sb", ("q_sb", b)], writes=[("ps_s", k)])
            s.op("dve", lambda e: e.scalar_tensor_tensor(out=tmp[k][:, 0:256], in0=ps_s[k][:, 0:256], scalar=0.125,
                                                         in1=tabW[:, i - j, :], op0=ALU.mult, op1=ALU.add),
                 reads=[("ps_s", k), "tabW"], writes=[("tmp", k)])
            s.op("act", lambda e: e.activation(out=pw[:, wi, :], in_=tmp[k][:, 0:256], func=AF.Exp),
                 reads=[("tmp", k)], writes=[("pw", wi)])
        for h in range(2):
            for wi, j in enumerate(wl):
                s.op("pe", lambda e: e.matmul(ps_o[:, h, 0:65], lhsT=pw[:, wi, h * 128:(h + 1) * 128], rhs=vw_sb[:, j, :],
                                              start=(wi == 0), stop=(wi == len(wl) - 1)),
                     reads=[("pw", wi), "vw_sb"], writes=["ps_o"])
        s.op("act", lambda e: e.copy(out=o_sb[b][:, 2, :, :], in_=ps_o[:, 0:2, 0:65]), reads=["ps_o"], writes=[("o_sb", b, 2)])

    for st in sel_steps(0):
        st()
    for i in range(NQB):
        b = i % 2
        nxt = sel_steps(i + 1) if i + 1 < NQB else []
        n = i + 1
        bufs = {}
        for step in range(n + LA):
            if step < n:
                bufs[step] = nextbuf()
                s_stage(i, step, bufs[step])
            if step - LA >= 0:
                pv_stage(i, step - LA, bufs[step - LA])
            if nxt and step % 2 == 1:
                nxt.pop(0)()
        s.op("act", lambda e: e.copy(out=osT[b][:, :, :].rearrange("p a b -> p (a b)"), in_=ps_os[0][0:65, 0:256]),
             reads=[("ps_os", 0)], writes=[("osT", b)])
        s.dma("sp", o3s[i], osT[b][:, :, :], reads=[("osT", b)], writes=[("o3s", i)])
        okeys.append(("o3s", i))
        window(i)
        while nxt:
            nxt.pop(0)()
        s.dma("sp", o3[i], o_sb[b][:, :, :, :], reads=[("o_sb", b, 0), ("o_sb", b, 2)], writes=[("o3", i)])
        okeys.append(("o3", i))
    s.finish(okeys)
    print("nsa ninst", s.ninst)
    s.close()
    return nc


import numpy as np

NCORES = 8
S = 16384
TPC = S // NCORES
_cache = {}


def _get(key, fn):
    if key not in _cache:
        _cache[key] = fn()
    return _cache[key]


def run_proj(h, w, out_bf16=True):
    N = w.shape[1]
    nc = _get(("proj", N, out_bf16), lambda: build_proj(TPC // 128, N, out_bf16))
    in_maps = [{"xT": np.ascontiguousarray(h[c * TPC:(c + 1) * TPC].T), "w": w} for c in range(NCORES)]
    res = run_bass_kernel_spmd(nc, in_maps, core_ids=list(range(NCORES)))
    return np.concatenate([np.asarray(r["y"]) for r in res.results], axis=0)


def run_atta(qkv):
    qTs, kTs, vs, meta = atta_host_prep(qkv)
    upc = [len(m) // NCORES for m in meta]
    units = []
    for g in range(3):
        for p in range(upc[g]):
            units.append(3 * g + (0 if p == 0 else (2 if p == 8 else 1)))
    nc = _get(("atta",), lambda: build_atta(units, 9))
    tab = atta_bias_table()
    in_maps = []
    for c in range(NCORES):
        tabs = np.zeros((9, 128, 2, 16, 128), np.float32)
        for g in range(3):
            for slot, p in ((0, 0), (1, 1), (2, 8)):
                t = tab[g].copy()
                if meta[g][c * upc[g] + p][1]:
                    t[:, 0] = -1.0e30
                tabs[3 * g + slot] = t
        sl = [slice(c * upc[g], (c + 1) * upc[g]) for g in range(3)]
        in_maps.append({"qT": np.concatenate([qTs[g][sl[g]] for g in range(3)]),
                        "kT": np.concatenate([kTs[g][sl[g]] for g in range(3)]),
                        "v": np.concatenate([vs[g][sl[g]] for g in range(3)]),
                        "tab": tabs})
    res = run_bass_kernel_spmd(nc, in_maps, core_ids=list(range(NCORES)))
    og = []
    for g in range(3):
        off = sum(upc[:g])
        og.append(np.concatenate([np.asarray(r["o"])[off:off + upc[g]] for r in res.results], axis=0))
    return atta_host_post(og, S)


def run_post(h, o_or_og, modeA, wout, lnp, wr, br, wgu, wd):
    nc = _get(("post", modeA), lambda: build_post(TPC, modeA))
    consts = post_consts()
    in_maps = []
    for c in range(NCORES):
        rows = slice(c * TPC, (c + 1) * TPC)
        m = {"h": np.ascontiguousarray(h[rows]), "wout": wout, "lnp": lnp, "wr": wr, "br": br, "wgu": wgu, "wd": wd}
        m.update(consts)
        if modeA:
            m["og"] = np.ascontiguousarray(o_or_og[:, rows])
        else:
            m["o"] = np.ascontiguousarray(o_or_og[rows])
        in_maps.append(m)
    res = run_bass_kernel_spmd(nc, in_maps, core_ids=list(range(NCORES)))
    return np.concatenate([np.asarray(r["hout"]) for r in res.results], axis=0)


def layer_a(h, l, inp):
    qkv = run_proj(h, np.ascontiguousarray(inp["a_w_in"][l]))
    og = run_atta(qkv)
    lnp = np.stack([inp["ln_mix_g"][l], inp["ln_mix_b"][l], inp["ln_ffn_g"][l], inp["ln_ffn_b"][l]])
    return run_post(h, og, 1, np.ascontiguousarray(inp["a_w_out"][l]), lnp,
                    np.ascontiguousarray(inp["moe_w_router"][l]), np.ascontiguousarray(inp["moe_b_router"][l]),
                    np.ascontiguousarray(inp["moe_w_gate_up"][l]), np.ascontiguousarray(inp["moe_w_down"][l]))


BF = ml_dtypes.bfloat16


def run_proj_b(h, w, bias):
    N = w.shape[1]
    nc = _get(("projb", N), lambda: build_proj(TPC // 128, N, True, True))
    in_maps = [{"xT": np.ascontiguousarray(h[c * TPC:(c + 1) * TPC].T), "w": w, "bias": bias} for c in range(NCORES)]
    res = run_bass_kernel_spmd(nc, in_maps, core_ids=list(range(NCORES)))
    return np.concatenate([np.asarray(r["y"]) for r in res.results], axis=0)


def run_cmp(kv, inp):
    kv6 = kv.reshape(S, 6, 4, 64)
    nc = _get(("cmp",), build_cmp)
    in_maps = []
    nblk = S // 16
    for j in range(2):
        for g in range(4):
            u = kv6[:, j, g, :]
            c = u.reshape(nblk, 16, 64)
            blocks = np.concatenate([c[:-1], c[1:]], axis=1)
            X = np.zeros((nblk, 2048), kv.dtype)
            X[:nblk - 1] = blocks.reshape(nblk - 1, 2048)
            in_maps.append({"xT": np.ascontiguousarray(X.T), "pos": np.ascontiguousarray(inp["b_cmp_pos"][j].reshape(2048)),
                            "w1": np.ascontiguousarray(inp["b_cmp_w1"][j]), "b1": np.ascontiguousarray(inp["b_cmp_b1"][j]),
                            "w2": np.ascontiguousarray(inp["b_cmp_w2"][j]), "b2": np.ascontiguousarray(inp["b_cmp_b2"][j])})
    res = run_bass_kernel_spmd(nc, in_maps, core_ids=list(range(NCORES)))
    outs = [np.asarray(r["outT"]) for r in res.results]
    kcT = [outs[g] for g in range(4)]
    vc = [np.ascontiguousarray(outs[4 + g].T) for g in range(4)]
    return kcT, vc


def run_nsa(proj, kv, kcT, vc):
    nc = _get(("nsa",), lambda: build_nsa(S))
    kv6 = kv.reshape(S, 6, 4, 64)
    q = proj[:, :1024].reshape(S // 128, 128, 16, 64)
    in_maps = []
    for c in range(NCORES):
        g, half = c // 2, c % 2
        own = [4 * g + 2 * half, 4 * g + 2 * half + 1]
        oth = [4 * g + 2 * (1 - half), 4 * g + 2 * (1 - half) + 1]
        heads4 = own + oth
        m = nsa_tables(S, heads4)
        m["qT4"] = np.ascontiguousarray(q[:, :, heads4, :].transpose(0, 3, 2, 1))
        qsrc = np.zeros((S // 128, 128, 2, 128), BF)
        qsrc[:, 0:64] = m["qT4"][:, :, 0:2, :]
        qsrc[:, 96:105] = m.pop("augq")
        m["qaugsrc"] = qsrc
        m["kcT"] = kcT[g]
        m["vc"] = vc[g]
        m["ksT"] = np.ascontiguousarray(kv6[:, 2, g, :].T)
        m["vs"] = np.ascontiguousarray(kv6[:, 3, g, :])
        m["kwT"] = np.ascontiguousarray(kv6[:, 4, g, :].T)
        m["vw"] = np.ascontiguousarray(kv6[:, 5, g, :])
        in_maps.append(m)
    res = run_bass_kernel_spmd(nc, in_maps, core_ids=list(range(NCORES)))
    og = np.zeros((3, S, 16, 65), np.float32)
    for c in range(NCORES):
        g, half = c // 2, c % 2
        o3 = np.asarray(res.results[c]["o3"]).reshape(S, 3, 2, 65).copy()
        o3[:, 1] = np.asarray(res.results[c]["o3s"]).transpose(0, 3, 2, 1).reshape(S, 2, 65)
        for hh in range(2):
            og[:, :, 4 * g + 2 * half + hh, :] = o3[:, :, hh, :].transpose(1, 0, 2)
    return og.reshape(3, S, 1040)


def layer_b(h, i, inp, shared):
    l = 2 + i
    proj = run_proj_b(h, np.ascontiguousarray(inp["b_w_q"][i]), np.ascontiguousarray(inp["b_b_q"][i]))
    kv, kcT, vc = shared
    og = run_nsa(proj, kv, kcT, vc)
    lnp = np.stack([inp["ln_mix_g"][l], inp["ln_mix_b"][l], inp["ln_ffn_g"][l], inp["ln_ffn_b"][l]])
    gl = np.ascontiguousarray(proj[:, 1024:1072])
    nc = _get(("post", 0), lambda: build_post(TPC, 0))
    consts = post_consts()
    in_maps = []
    for c in range(NCORES):
        rows = slice(c * TPC, (c + 1) * TPC)
        m = {"h": np.ascontiguousarray(h[rows]), "wout": np.ascontiguousarray(inp["b_w_out"][i]), "lnp": lnp,
             "wr": np.ascontiguousarray(inp["moe_w_router"][l]), "br": np.ascontiguousarray(inp["moe_b_router"][l]),
             "wgu": np.ascontiguousarray(inp["moe_w_gate_up"][l]), "wd": np.ascontiguousarray(inp["moe_w_down"][l]),
             "og": np.ascontiguousarray(og[:, rows]), "gl": np.ascontiguousarray(gl[rows])}
        m.update(consts)
        in_maps.append(m)
    res = run_bass_kernel_spmd(nc, in_maps, core_ids=list(range(NCORES)))
    return np.concatenate([np.asarray(r["hout"]) for r in res.results], axis=0)


def forward(inp):
    h = np.ascontiguousarray(inp["x"][0])
    for l in range(2):
        h = layer_a(h, l, inp)
    kv = run_proj(h, np.ascontiguousarray(inp["b_w_kv"]))
    kcT, vc = run_cmp(kv, inp)
    for i in range(2):
        h = layer_b(h, i, inp, (kv, kcT, vc))
    return h[None].astype(np.float32)


def kernel(**inputs):
    inp = {k: np.asarray(v) for k, v in inputs.items()}
    return forward(inp)
```
